# Optimizing a Trainium2 kernel written in Bass

```python
import math
import jax
import jax.numpy as jnp
from jax import lax
import numpy as np

D_MODEL = 1024
BATCH = 2
SEQ = 16384
DEPTH = 2

CTX_LEN = 256
GRID_W = 64
N_MOD = 6
NORM_EPS = 1e-6

FOURIER_GROUPS = 4
FOURIER_GD = 64
FOURIER_W = FOURIER_GROUPS * FOURIER_GD
DA_HEADS = 6
DA_DH = 64
DA_VD = 2 * DA_DH
DA_W = DA_HEADS * DA_VD
EVEN_IN = FOURIER_W + 3 * DA_W
EVEN_MIX = FOURIER_W + DA_W
Q_BLOCK = 128
ROPE_BASE = 10000.0
ROPE_AXIS_DIM = DA_DH // 2
SUBLN_EPS = 1e-5

GDN_HK = 8
GDN_HV = 16
GDN_DK = 128
GDN_DV = 128
GDN_QK_W = GDN_HK * GDN_DK
GDN_V_W = GDN_HV * GDN_DV
GDN_QKV_W = 2 * GDN_QK_W + GDN_V_W
GDN_CONV = 5
GDN_CHUNK = 64
ODD_IN = GDN_QKV_W + GDN_V_W + 4 * GDN_HV

N_EXPERTS = 32
TOP_K = 4
D_FF = 1024
SWIGLU_LIMIT = 7.0
SWIGLU_ALPHA = 1.702
MOE_BLOCK = 512

N_EVEN = (DEPTH + 1) // 2
N_ODD = DEPTH // 2

kernel_name = "hybrid_fourier_diffattn_gdn_moe_dit"


def rms_norm(x, g, eps=NORM_EPS):
    xf = x.astype(jnp.float32)
    y = xf * lax.rsqrt(jnp.mean(xf * xf, axis=-1, keepdims=True) + eps)
    return y.astype(x.dtype) * g


def modulate(h, shift, scale):
    return h * (1 + scale) + shift


def l2_normalize(x, eps=1e-6):
    return x * lax.rsqrt(jnp.sum(x * x, axis=-1, keepdims=True) + eps)


def diff_lambda_init(layer_idx):
    return 0.8 - 0.6 * math.exp(-0.3 * layer_idx)


def axial_rope_tables(rows):
    t = jnp.arange(rows * GRID_W, dtype=jnp.int32)
    row = (t // GRID_W).astype(jnp.float32)
    col = (t % GRID_W).astype(jnp.float32)
    inv = ROPE_BASE ** (-jnp.arange(0, ROPE_AXIS_DIM, 2, dtype=jnp.float32) / ROPE_AXIS_DIM)
    ang = jnp.concatenate([row[:, None] * inv, col[:, None] * inv], axis=-1)
    return jnp.cos(ang), jnp.sin(ang)


def rope2d(x, cos, sin):
    half = x.shape[-1] // 2
    xf = x.astype(jnp.float32)
    x1, x2 = xf[..., :half], xf[..., half:]
    cb = cos[None, :, None, None, :]
    sb = sin[None, :, None, None, :]
    return jnp.concatenate([x1 * cb - x2 * sb, x2 * cb + x1 * sb], axis=-1).astype(x.dtype)


def fourier_mix(f):
    ff = jnp.moveaxis(f.astype(jnp.float32), 2, 1)
    out = jnp.real(jnp.fft.fft2(ff, norm='ortho'))
    return jnp.moveaxis(out, 1, 2).astype(f.dtype)


def _diff_attend(q, k, v, lam):
    s = jnp.einsum('bqhmd,bkhmd->bhmqk', q, k, preferred_element_type=jnp.float32) * (DA_DH ** -0.5)
    p = jax.nn.softmax(s, axis=-1)
    a = (p[:, :, 0] - lam * p[:, :, 1]).astype(v.dtype)
    return jnp.einsum('bhqk,bkhe->bqhe', a, v)


def _even_split(p):
    b, n, _ = p.shape
    f = p[..., :FOURIER_W].reshape(b, n, FOURIER_GROUPS, FOURIER_GD)
    q, k, v = jnp.split(p[..., FOURIER_W:], 3, axis=-1)
    q = q.reshape(b, n, DA_HEADS, 2, DA_DH)
    k = k.reshape(b, n, DA_HEADS, 2, DA_DH)
    v = v.reshape(b, n, DA_HEADS, DA_VD)
    return f, q, k, v


def _even_merge(f, o, subln_g, lam_init, w_out):
    b, n = f.shape[:2]
    o = rms_norm(o, subln_g, SUBLN_EPS) * (1.0 - lam_init)
    mixed = jnp.concatenate([fourier_mix(f).reshape(b, n, FOURIER_W), o.reshape(b, n, DA_W)], axis=-1)
    return mixed @ w_out


def fourier_diff_mixer(h, hc, w_in, w_out, lam_p, subln_g, lam_init, cos, sin, need_ctx):
    b, n, _ = h.shape
    f, q, k, v = _even_split(h @ w_in)
    fc, qc, kc, vc = _even_split(hc @ w_in)
    q = rope2d(q, cos, sin)
    k = rope2d(k, cos, sin)
    lp = lam_p.astype(jnp.float32)
    lam = jnp.exp(jnp.sum(lp[0] * lp[1])) - jnp.exp(jnp.sum(lp[2] * lp[3])) + lam_init
    k_all = jnp.concatenate([k, kc], axis=1)
    v_all = jnp.concatenate([v, vc], axis=1)
    nb = n // Q_BLOCK
    qb = jnp.moveaxis(q.reshape(b, nb, Q_BLOCK, DA_HEADS, 2, DA_DH), 1, 0)
    o = lax.map(lambda blk: _diff_attend(blk, k_all, v_all, lam), qb)
    o = jnp.moveaxis(o, 0, 1).reshape(b, n, DA_HEADS, DA_VD)
    y = _even_merge(f, o, subln_g, lam_init, w_out)
    if not need_ctx:
        return y, None
    oc = _diff_attend(qc, kc, vc, lam)
    yc = _even_merge(fc, oc, subln_g, lam_init, w_out)
    return y, yc


def centred_depthwise_conv(x, w):
    width = w.shape[0]
    return lax.conv_general_dilated(
        x, w[:, None, :].astype(x.dtype), window_strides=(1,),
        padding=[((width - 1) // 2, width // 2)],
        dimension_numbers=('NWC', 'WIO', 'NWC'),
        feature_group_count=x.shape[-1])


def chunk_gated_delta(q, k, v, g, beta, s0):
    b, t, nh, _ = q.shape
    n = t // GDN_CHUNK

    def chunks(a):
        a = a.reshape((b, n, GDN_CHUNK, nh) + a.shape[3:])
        return jnp.moveaxis(a, (1, 3), (0, 2))

    qc, kc, vc, gc, bc = chunks(q), chunks(k), chunks(v), chunks(g), chunks(beta)
    G = jnp.cumsum(gc, axis=-1)
    idx = jnp.arange(GDN_CHUNK)
    incl = idx[:, None] >= idx[None, :]
    strict = idx[:, None] > idx[None, :]
    diff = G[..., :, None] - G[..., None, :]
    decay = jnp.where(incl, jnp.exp(jnp.where(incl, diff, 0.0)), 0.0)
    kb = kc * bc[..., None]
    lmat = jnp.where(strict, jnp.einsum('...id,...jd->...ij', kb, kc) * decay, 0.0)
    eye = jnp.broadcast_to(jnp.eye(GDN_CHUNK, dtype=jnp.float32), lmat.shape)
    tmat = lax.linalg.triangular_solve(lmat + eye, eye, left_side=True, lower=True)
    u = tmat @ (vc * bc[..., None])
    w = tmat @ (kb * jnp.exp(G)[..., None])
    qg = qc * jnp.exp(G)[..., None]
    intra = jnp.where(incl, jnp.einsum('...id,...jd->...ij', qc, kc) * decay, 0.0)
    g_end = G[..., -1]
    k_tail = kc * jnp.exp(g_end[..., None] - G)[..., None]

    def step(state, xs):
        qg_i, u_i, w_i, intra_i, kt_i, ge_i = xs
        v_new = u_i - w_i @ state
        o_i = qg_i @ state + intra_i @ v_new
        state = state * jnp.exp(ge_i)[..., None, None] + jnp.swapaxes(kt_i, -1, -2) @ v_new
        return state, o_i

    s_fin, o = lax.scan(step, s0, (qg, u, w, intra, k_tail, g_end))
    o = jnp.moveaxis(o, (0, 2), (1, 3)).reshape(b, t, nh, -1)
    return o, s_fin


def _gdn_prepare(p, conv_w, a_log, dt_bias):
    b, n, _ = p.shape
    qkv = jax.nn.silu(centred_depthwise_conv(p[..., :GDN_QKV_W], conv_w)).astype(jnp.float32)
    q = qkv[..., :GDN_QK_W].reshape(b, n, GDN_HK, GDN_DK)
    k = qkv[..., GDN_QK_W:2 * GDN_QK_W].reshape(b, n, GDN_HK, GDN_DK)
    v = qkv[..., 2 * GDN_QK_W:].reshape(b, n, GDN_HV, GDN_DV)
    rep = GDN_HV // GDN_HK
    q = jnp.repeat(l2_normalize(q) * (GDN_DK ** -0.5), rep, axis=2)
    k = jnp.repeat(l2_normalize(k), rep, axis=2)
    z = p[..., GDN_QKV_W:GDN_QKV_W + GDN_V_W].reshape(b, n, GDN_HV, GDN_DV)
    ab = p[..., GDN_QKV_W + GDN_V_W:].astype(jnp.float32).reshape(b, n, 2, 2, GDN_HV)
    a, bb = ab[:, :, 0], ab[:, :, 1]
    g = -jnp.exp(a_log.astype(jnp.float32)) * jax.nn.softplus(a + dt_bias.astype(jnp.float32))
    beta = jax.nn.sigmoid(bb)
    return q, k, v, z, g, beta


def _gdn_readout(o, z, norm_g, w_out):
    b, n = o.shape[:2]
    y = rms_norm(o, norm_g) * jax.nn.silu(z.astype(jnp.float32))
    return y.astype(z.dtype).reshape(b, n, GDN_V_W) @ w_out


def gated_deltanet_mixer(h, hc, w_in, conv_w, a_log, dt_bias, norm_g, w_out, need_ctx):
    q, k, v, z, g, beta = _gdn_prepare(h @ w_in, conv_w, a_log, dt_bias)
    qc, kc, vc, zc, gc, bc = _gdn_prepare(hc @ w_in, conv_w, a_log, dt_bias)
    s0 = jnp.zeros((h.shape[0], GDN_HV, GDN_DK, GDN_DV), jnp.float32)
    rev = lambda a: jnp.flip(a, axis=1)
    oc_f, sc_f = chunk_gated_delta(qc, kc, vc, gc[:, :, 0], bc[:, :, 0], s0)
    o_f, _ = chunk_gated_delta(q, k, v, g[:, :, 0], beta[:, :, 0], sc_f)
    oc_b, sc_b = chunk_gated_delta(rev(qc), rev(kc), rev(vc), rev(gc[:, :, 1]), rev(bc[:, :, 1]), s0)
    o_b, _ = chunk_gated_delta(rev(q), rev(k), rev(v), rev(g[:, :, 1]), rev(beta[:, :, 1]), sc_b)
    y = _gdn_readout(o_f + rev(o_b), z, norm_g, w_out)
    if not need_ctx:
        return y, None
    yc = _gdn_readout(oc_f + rev(oc_b), zc, norm_g, w_out)
    return y, yc


def moe_ffn(h, w_router, b_router, w1, b1, w2, b2):
    n_tok, d = h.shape
    logits = (h @ w_router + b_router).astype(jnp.float32)
    top_val, top_idx = lax.top_k(logits, TOP_K)
    gates = jax.nn.softmax(top_val, axis=-1).astype(h.dtype)
    n_assign = n_tok * TOP_K
    e_flat = top_idx.reshape(n_assign).astype(jnp.int32)
    order = jnp.argsort(e_flat).astype(jnp.int32)
    e_sorted = e_flat[order]
    tok_sorted = order // TOP_K
    gate_sorted = gates.reshape(n_assign)[order]
    counts = jnp.zeros((N_EXPERTS,), jnp.int32).at[e_flat].add(1)
    starts = jnp.cumsum(counts) - counts
    padded = (counts + MOE_BLOCK - 1) // MOE_BLOCK * MOE_BLOCK
    pad_ends = jnp.cumsum(padded)
    pad_starts = pad_ends - padded
    dest = pad_starts[e_sorted] + jnp.arange(n_assign, dtype=jnp.int32) - starts[e_sorted]
    n_blocks = -(-(n_assign + N_EXPERTS * (MOE_BLOCK - 1)) // MOE_BLOCK)
    n_rows = n_blocks * MOE_BLOCK
    row_tok = jnp.full((n_rows,), n_tok, jnp.int32).at[dest].set(tok_sorted)
    row_gate = jnp.zeros((n_rows,), h.dtype).at[dest].set(gate_sorted)
    block_start = jnp.arange(n_blocks, dtype=jnp.int32) * MOE_BLOCK
    block_expert = jnp.minimum(jnp.searchsorted(pad_ends, block_start, side='right'), N_EXPERTS - 1)
    h_pad = jnp.concatenate([h, jnp.zeros((1, d), h.dtype)], axis=0)
    xb = h_pad[row_tok].reshape(n_blocks, MOE_BLOCK, d)

    def expert_block(args):
        xblk, e = args
        u = xblk @ w1[e] + b1[e]
        glu = jnp.minimum(u[:, 0::2], SWIGLU_LIMIT)
        lin = jnp.clip(u[:, 1::2], -SWIGLU_LIMIT, SWIGLU_LIMIT)
        act = glu * jax.nn.sigmoid(SWIGLU_ALPHA * glu) * (lin + 1)
        return act @ w2[e] + b2[e]

    yb = lax.map(expert_block, (xb, block_expert)).reshape(n_rows, d)
    out = jnp.zeros((n_tok + 1, d), yb.dtype).at[row_tok].add(yb * row_gate[:, None])
    return out[:n_tok]


def setup_inputs(seed: int = 0) -> dict:
    key = jax.random.key(seed)
    ks = iter(jax.random.split(key, 32))
    f32 = jnp.float32

    def normal(shape, scale=1.0):
        return jax.random.normal(next(ks), shape, f32) * scale

    def gain(shape):
        return 1.0 + normal(shape, 0.05)

    def log_uniform_dt(shape):
        dt = jnp.exp(jax.random.uniform(next(ks), shape, f32, math.log(1e-3), math.log(1e-1)))
        return dt + jnp.log(-jnp.expm1(-dt))

    return {
        'x': normal((BATCH, SEQ, D_MODEL)),
        'c': normal((BATCH, D_MODEL)),
        'ctx': normal((BATCH, CTX_LEN, D_MODEL)),
        'c_ctx': normal((D_MODEL,)),
        'norm1_g': gain((DEPTH, D_MODEL)),
        'norm2_g': gain((DEPTH, D_MODEL)),
        'w_mod': normal((DEPTH, D_MODEL, N_MOD * D_MODEL), 0.5 * D_MODEL ** -0.5),
        'b_mod': normal((DEPTH, N_MOD * D_MODEL), 0.02),
        'ev_w_in': normal((N_EVEN, D_MODEL, EVEN_IN), D_MODEL ** -0.5),
        'ev_w_out': normal((N_EVEN, EVEN_MIX, D_MODEL), EVEN_MIX ** -0.5),
        'ev_lam': normal((N_EVEN, 4, DA_DH), 0.1),
        'ev_subln_g': gain((N_EVEN, DA_VD)),
        'od_w_in': normal((N_ODD, D_MODEL, ODD_IN), D_MODEL ** -0.5),
        'od_conv_w': normal((N_ODD, GDN_CONV, GDN_QKV_W), GDN_CONV ** -0.5),
        'od_a_log': jnp.log(jax.random.uniform(next(ks), (N_ODD, 2, GDN_HV), f32, 1.0, 16.0)),
        'od_dt_bias': log_uniform_dt((N_ODD, 2, GDN_HV)),
        'od_norm_g': gain((N_ODD, GDN_DV)),
        'od_w_out': normal((N_ODD, GDN_V_W, D_MODEL), GDN_V_W ** -0.5),
        'moe_w_router': normal((DEPTH, D_MODEL, N_EXPERTS), D_MODEL ** -0.5),
        'moe_b_router': normal((DEPTH, N_EXPERTS), 0.01),
        'moe_w1': normal((DEPTH, N_EXPERTS, D_MODEL, 2 * D_FF), D_MODEL ** -0.5),
        'moe_b1': normal((DEPTH, N_EXPERTS, 2 * D_FF), 0.02),
        'moe_w2': normal((DEPTH, N_EXPERTS, D_FF, D_MODEL), D_FF ** -0.5),
        'moe_b2': normal((DEPTH, N_EXPERTS, D_MODEL), 0.02),
        'final_g': gain((D_MODEL,)),
    }


def reference(x, c, ctx, c_ctx, norm1_g, norm2_g, w_mod, b_mod,
              ev_w_in, ev_w_out, ev_lam, ev_subln_g,
              od_w_in, od_conv_w, od_a_log, od_dt_bias, od_norm_g, od_w_out,
              moe_w_router, moe_b_router, moe_w1, moe_b1, moe_w2, moe_b2, final_g):
    bsz, n_lat, d = x.shape
    n_ctx = ctx.shape[1]
    rows = n_lat // GRID_W
    cos, sin = axial_rope_tables(rows)
    s_lat = jax.nn.silu(c)
    s_ctx = jax.nn.silu(c_ctx)
    xc = ctx
    for l in range(DEPTH):
        last = l == DEPTH - 1
        mod = jnp.split((s_lat @ w_mod[l] + b_mod[l])[:, None, :], N_MOD, axis=-1)
        mod_c = jnp.split((s_ctx @ w_mod[l] + b_mod[l])[None, None, :], N_MOD, axis=-1)
        h = modulate(rms_norm(x, norm1_g[l]), mod[0], mod[1])
        hc = modulate(rms_norm(xc, norm1_g[l]), mod_c[0], mod_c[1])
        i = l // 2
        if l % 2 == 0:
            y, yc = fourier_diff_mixer(h, hc, ev_w_in[i], ev_w_out[i], ev_lam[i], ev_subln_g[i],
                                       diff_lambda_init(l), cos, sin, not last)
        else:
            y, yc = gated_deltanet_mixer(h, hc, od_w_in[i], od_conv_w[i], od_a_log[i], od_dt_bias[i],
                                         od_norm_g[i], od_w_out[i], not last)
        x = x + mod[2] * y
        h2 = modulate(rms_norm(x, norm2_g[l]), mod[3], mod[4]).reshape(bsz * n_lat, d)
        moe_args = (moe_w_router[l], moe_b_router[l], moe_w1[l], moe_b1[l], moe_w2[l], moe_b2[l])
        if last:
            x = x + mod[5] * moe_ffn(h2, *moe_args).reshape(bsz, n_lat, d)
        else:
            xc = xc + mod_c[2] * yc
            h2c = modulate(rms_norm(xc, norm2_g[l]), mod_c[3], mod_c[4]).reshape(bsz * n_ctx, d)
            out = moe_ffn(jnp.concatenate([h2, h2c], axis=0), *moe_args)
            x = x + mod[5] * out[:bsz * n_lat].reshape(bsz, n_lat, d)
            xc = xc + mod_c[5] * out[bsz * n_lat:].reshape(bsz, n_ctx, d)
    return rms_norm(x, final_g)
```

```python
import contextlib
import math
import numpy as np
import concourse.bass as bass
import concourse.mybir as mybir
from concourse.bass_utils import run_bass_kernel_spmd

F32 = mybir.dt.float32
BF16 = mybir.dt.bfloat16
I32 = mybir.dt.int32
AF = mybir.ActivationFunctionType
ALU = mybir.AluOpType
AX = mybir.AxisListType

NCORES = 8
D = 1024


class Buf:
    __slots__ = ("t", "last_w", "reads", "name")

    def __init__(self, t, name=""):
        self.t = t
        self.last_w = None
        self.reads = {}
        self.name = name

    def __getitem__(self, k):
        return self.t[k]


class Ctx:
    NDMA = 12

    def __init__(self, nc):
        self.nc = nc
        self.es = contextlib.ExitStack()
        self.eng = {"pe": nc.tensor, "act": nc.scalar, "dve": nc.vector, "pool": nc.gpsimd, "sp": nc.sync}
        self.sem = {}
        self.cnt = {}
        self.waited = {}
        for e in self.eng:
            self.sem[e] = self.es.enter_context(nc.semaphore("s_" + e))
            self.cnt[e] = 0
            self.waited[e] = {}
        self.dsem = {}
        self.dn = {}
        for q in ("sp", "pool", "act"):
            self.dsem[q] = [self.es.enter_context(nc.semaphore(f"d_{q}{i}")) for i in range(self.NDMA)]
            self.dn[q] = 0
        self.nbuf = 0

    def sb(self, shape, dt, name=None):
        self.nbuf += 1
        name = name or f"sb{self.nbuf}"
        return Buf(self.es.enter_context(self.nc.sbuf_tensor(name, list(shape), dt)), name)

    def ps(self, shape, dt, name=None):
        self.nbuf += 1
        name = name or f"ps{self.nbuf}"
        return Buf(self.es.enter_context(self.nc.psum_tensor(name, list(shape), dt)), name)

    def view(self, buf):
        return Buf(buf.t, buf.name + "_v")

    def push(self):
        self.stack = getattr(self, "stack", [])
        self.stack.append(self.es)
        self.es = contextlib.ExitStack()

    def pop(self):
        self.barrier()
        self.es.close()
        self.es = self.stack.pop()

    def barrier(self):
        evs = []
        for f in ("pe", "act", "dve", "pool"):
            if self.cnt[f]:
                evs.append((f, self.sem[f], self.cnt[f]))
        for q in self.dsem:
            n = self.dn[q]
            for s in range(min(n, self.NDMA)):
                last = ((n - 1 - s) // self.NDMA) * self.NDMA + s
                evs.append((("d", q, s), self.dsem[q][s], 16 * (last // self.NDMA + 1)))
        for e in self.eng:
            for ev in evs:
                if ev[0] != e:
                    self._wait(e, ev)

    def _wait(self, e, ev):
        key, sem, val = ev
        if key == e and e == "pe":
            return
        w = self.waited[e]
        if w.get(key, 0) >= val:
            return
        self.eng[e].wait_ge(sem, val)
        w[key] = val

    def _deps(self, e, reads, writes):
        for b in reads:
            if b.last_w is not None:
                self._wait(e, b.last_w)
        for b in writes:
            if b.last_w is not None:
                self._wait(e, b.last_w)
            for ev in b.reads.values():
                self._wait(e, ev)

    def _mark(self, ev, reads, writes):
        for b in writes:
            b.last_w = ev
            b.reads = {}
        for b in reads:
            if b not in writes:
                b.reads[ev[0]] = ev

    def op(self, e, ins_fn, reads=(), writes=()):
        self._deps(e, reads, writes)
        ins = ins_fn()
        self.cnt[e] += 1
        ins.then_inc(self.sem[e], 1)
        self._mark((e, self.sem[e], self.cnt[e]), reads, writes)

    def dma(self, q, out, in_, reads=(), writes=(), **kw):
        self._deps(q, reads, writes)
        j = self.dn[q]
        P = self.NDMA
        sem = self.dsem[q][j % P]
        key = ("d", q, j % P)
        if j >= P:
            self._wait(q, (key, sem, 16 * (j // P)))
        ins = self.eng[q].dma_start(out=out, in_=in_, **kw)
        ins.then_inc(sem, 16)
        self.dn[q] = j + 1
        self._mark((key, sem, 16 * (j // P + 1)), reads, writes)

    def finish(self):
        self.barrier()
        self.es.close()

    def mm(self, out, out_ap, lhsT, lhsT_ap, rhs, rhs_ap, start=True, stop=True):
        self.op("pe", lambda: self.nc.tensor.matmul(out_ap, lhsT_ap, rhs_ap, start=start, stop=stop),
                reads=[lhsT, rhs], writes=[out])

    def tr(self, out, out_ap, in_, in_ap, ident):
        k = in_ap.shape[0]
        self.op("pe", lambda: self.nc.tensor.transpose(out_ap, in_ap, ident.t[0:k, 0:k]), reads=[in_, ident], writes=[out])

    def act(self, out, out_ap, in_, in_ap, func, bias=None, scale=None, accum=None, eng="act", extra_reads=()):
        kw = {}
        rd = [in_] + list(extra_reads)
        wr = [out]
        if bias is not None:
            kw["bias"] = bias
        if scale is not None:
            kw["scale"] = scale
        if accum is not None:
            kw["accum_out"] = accum[1]
            wr.append(accum[0])
        self.op("act", lambda: self.nc.scalar.activation(out_ap, in_ap, func, **kw), reads=rd, writes=wr)

    def cp(self, e, out, out_ap, in_, in_ap):
        if e == "act":
            self.op("act", lambda: self.nc.scalar.copy(out_ap, in_ap), reads=[in_], writes=[out])
        else:
            self.op(e, lambda: self.eng[e].tensor_copy(out_ap, in_ap), reads=[in_], writes=[out])

    def tt(self, e, out, out_ap, a, a_ap, b, b_ap, op):
        self.op(e, lambda: self.eng[e].tensor_tensor(out_ap, a_ap, b_ap, op), reads=[a, b], writes=[out])

    def ts(self, e, out, out_ap, a, a_ap, s1, s2, op0, op1=None, extra_reads=()):
        if op1 is None:
            f = lambda: self.eng[e].tensor_scalar(out_ap, a_ap, s1, None, op0)
        else:
            f = lambda: self.eng[e].tensor_scalar(out_ap, a_ap, s1, s2, op0, op1)
        self.op(e, f, reads=[a] + list(extra_reads), writes=[out])

    def stt(self, e, out, out_ap, a, a_ap, scalar, b, b_ap, op0, op1, extra_reads=()):
        self.op(e, lambda: self.eng[e].scalar_tensor_tensor(out_ap, a_ap, scalar, b_ap, op0, op1),
                reads=[a, b] + list(extra_reads), writes=[out])


def new_nc():
    return bass.Bass("TRN2", target_bir_lowering=False)


def dram_in(nc, name, shape, dt=F32):
    return nc.dram_tensor(name, list(shape), dt, kind="ExternalInput").ap()


def dram_out(nc, name, shape, dt=F32):
    return nc.dram_tensor(name, list(shape), dt, kind="ExternalOutput").ap()


def run(nc, in_maps):
    res = run_bass_kernel_spmd(nc, in_maps, core_ids=list(range(NCORES)))
    return res.results


def load_weight_bf16(c, w_ap, K, N, name, nchunk_cols=None):
    kc_n = K // 128
    wb = c.sb([128, kc_n, N], BF16, name)
    cw = min(N, 2048)
    st = [c.sb([128, cw], F32, f"{name}_st{i}") for i in range(2)]
    i = 0
    engs = ["act", "dve", "pool"]
    for kc in range(kc_n):
        for n0 in range(0, N, cw):
            n1 = min(N, n0 + cw)
            s = st[i % 2]
            c.dma("sp", s[:, 0:n1 - n0], w_ap[kc * 128:(kc + 1) * 128, n0:n1], writes=[s])
            c.cp(engs[i % 3], wb, wb[:, kc, n0:n1], s, s[:, 0:n1 - n0])
            i += 1
    return wb


def load_ident(c, ident_ap):
    idf = c.sb([128, 128], F32, "identf")
    idb = c.sb([128, 128], BF16, "identb")
    c.dma("sp", idf[:], ident_ap[:, :], writes=[idf])
    c.cp("dve", idb, idb[:], idf, idf[:])
    return idf, idb


def make_mod_tiles(c, g_ap, mod_ap, seg, i_shift, i_scale, name):
    gb = c.sb([128, D], F32, name + "_G")
    sh = c.sb([128, D], F32, name + "_S")
    tmp = c.sb([128, D], F32, name + "_t")
    c.dma("sp", tmp[:], g_ap.partition_broadcast(128), writes=[tmp])
    c.dma("sp", gb[:], mod_ap[seg, i_scale:i_scale + 1, :].partition_broadcast(128), writes=[gb])
    c.dma("sp", sh[:], mod_ap[seg, i_shift:i_shift + 1, :].partition_broadcast(128), writes=[sh])
    c.stt("dve", gb, gb[:], gb, gb[:], 1.0, tmp, tmp[:], ALU.add, ALU.mult)
    return gb, sh


def build_mod():
    nc = new_nc()
    cT = dram_in(nc, "cT", [D, 3])
    wm = dram_in(nc, "wm", [2, D, 768])
    bm = dram_in(nc, "bm", [2, 768])
    out = dram_out(nc, "mod", [2, 3, 768])
    c = Ctx(nc)
    sT = c.sb([128, 8, 3], F32, "sT")
    c.dma("sp", sT[:], cT.rearrange("(kc p) s -> p kc s", p=128), writes=[sT])
    c.act(sT, sT[:], sT, sT[:], AF.Silu)
    wt = [c.sb([128, 768], F32, f"wt{i}") for i in range(3)]
    pt = [c.ps([128, 512], F32, f"pm{i}") for i in range(4)]
    bt = c.sb([3, 2, 768], F32, "bt")
    for l in range(2):
        c.dma("sp", bt[:, l, :], bm[l:l + 1, :].partition_broadcast(3), writes=[bt])
    ot = c.sb([3, 2, 768], F32, "ot")
    i = 0
    for l in range(2):
        pa, pb = pt[2 * l], pt[2 * l + 1]
        for kc in range(8):
            w = wt[i % 3]
            i += 1
            c.dma("sp", w[:], wm[l, kc * 128:(kc + 1) * 128, :], writes=[w])
            c.mm(pa, pa[0:3, 0:512], sT, sT[:, kc, :], w, w[:, 0:512], start=(kc == 0), stop=(kc == 7))
            c.mm(pb, pb[0:3, 0:256], sT, sT[:, kc, :], w, w[:, 512:768], start=(kc == 0), stop=(kc == 7))
        c.tt("dve", ot, ot[:, l, 0:512], pa, pa[0:3, 0:512], bt, bt[:, l, 0:512], ALU.add)
        c.tt("dve", ot, ot[:, l, 512:768], pb, pb[0:3, 0:256], bt, bt[:, l, 512:768], ALU.add)
    c.dma("sp", out.rearrange("l s n -> s l n"), ot[:], reads=[ot])
    c.finish()
    return nc


def run_mod(inp):
    cT = np.ascontiguousarray(np.concatenate([inp["c"], inp["c_ctx"][None]], 0).T)
    maps = []
    for k in range(NCORES):
        maps.append({"cT": cT,
                     "wm": np.ascontiguousarray(inp["w_mod"][:, :, 768 * k:768 * (k + 1)]),
                     "bm": np.ascontiguousarray(inp["b_mod"][:, 768 * k:768 * (k + 1)])})
    res = run(build_mod(), maps)
    return np.concatenate([r["mod"] for r in res], axis=2)


class NormT:
    def __init__(self, c, idb, eps=1e-6):
        self.c = c
        self.idb = idb
        self.eps = eps
        self.xt = [c.sb([128, D], F32, f"nx{i}") for i in range(2)]
        self.sq = c.sb([128, D], F32, "nsq")
        self.ss = [c.sb([128, 1], F32, f"nss{i}") for i in range(2)]
        self.tmp = c.sb([128, D], F32, "ntmp")
        self.hb = [c.sb([128, D], BF16, f"nhb{i}") for i in range(2)]
        self.pT = [c.ps([128, 8, 128], BF16, f"npT{i}") for i in range(2)]
        self.hT = [c.sb([128, 8, 128], BF16, f"nhT{i}") for i in range(2)]
        self.i = 0

    def run(self, x_ap, G, Sh, xt=None, hf=None):
        c = self.c
        i = self.i
        self.i += 1
        if xt is None:
            xt = self.xt[i % 2]
            c.dma("sp", xt[:], x_ap, writes=[xt])
        ss = self.ss[i % 2]
        c.act(self.sq, self.sq[:], xt, xt[:], AF.Square, accum=(ss, ss[:, 0:1]))
        c.ts("dve", ss, ss[:], ss, ss[:], 1.0 / D, self.eps, ALU.mult, ALU.add)
        c.op("act", lambda: c.nc.scalar.sqrt(ss[:], ss[:]), reads=[ss], writes=[ss])
        c.op("dve", lambda: c.nc.vector.reciprocal(ss[:], ss[:]), reads=[ss], writes=[ss])
        c.stt("dve", self.tmp, self.tmp[:], xt, xt[:], ss[:, 0:1], G, G[:], ALU.mult, ALU.mult, extra_reads=[ss])
        hb = self.hb[i % 2]
        if hf is None:
            c.tt("pool", hb, hb[:], self.tmp, self.tmp[:], Sh, Sh[:], ALU.add)
        else:
            c.tt("pool", hf, hf[:], self.tmp, self.tmp[:], Sh, Sh[:], ALU.add)
            c.cp("pool", hb, hb[:], hf, hf[:])
        pT = self.pT[i % 2]
        for kc in range(8):
            c.tr(pT, pT[:, kc, :], hb, hb[:, kc * 128:(kc + 1) * 128], self.idb)
        hT = self.hT[i % 2]
        c.cp("act", hT, hT[:], pT, pT[:])
        return hT


L0_T = 4224
EVEN_IN = 2560


def build_l0proj(N=EVEN_IN, rope=True):
    nc = new_nc()
    x = dram_in(nc, "x", [L0_T, D])
    g = dram_in(nc, "g", [D])
    mod = dram_in(nc, "mod", [2, 2, D])
    w = dram_in(nc, "w", [D, N])
    if rope:
        cs = dram_in(nc, "cs", [L0_T, 2, 32])
    ident = dram_in(nc, "ident", [128, 128])
    out = dram_out(nc, "p", [L0_T, N])
    c = Ctx(nc)
    idf, idb = load_ident(c, ident)
    wb = load_weight_bf16(c, w, D, N, "wb")
    mods = []
    for s_ in range(2):
        gb = bcast_tile(c, mod[s_, 1:2, :], f"mG{s_}")
        gg = bcast_tile(c, g, f"mg{s_}") if s_ == 0 else mods[0][2]
        sh = bcast_tile(c, mod[s_, 0:1, :], f"mS{s_}")
        c.stt("dve", gb, gb[:], gb, gb[:], 1.0, gg, gg[:], ALU.add, ALU.mult)
        mods.append((gb, sh, gg))
    nt = NormT(c, idb)
    po = [c.ps([128, 512], F32, f"po{i}") for i in range(4)]
    ot = [c.sb([128, N], F32, f"ot{i}") for i in range(2 if N <= 4096 else 1)]
    if rope:
        cst = [c.sb([128, 2, 32], F32, f"cs{i}") for i in range(2)]
        r1 = c.sb([128, 24, 32], F32, "r1")
        r2 = c.sb([128, 24, 32], F32, "r2")
        r3 = c.sb([128, 24, 32], F32, "r3")
        r4 = c.sb([128, 24, 32], F32, "r4")
    nt_tiles = L0_T // 128
    j = 0
    for t in range(nt_tiles):
        G, Sh, _ = mods[0] if t < 32 else mods[1]
        hT = nt.run(x[t * 128:(t + 1) * 128, :], G, Sh)
        o = ot[t % len(ot)]
        if rope:
            cs_t = cst[t % 2]
            c.dma("pool", cs_t[:], cs[t * 128:(t + 1) * 128, :, :], writes=[cs_t])
        for n, n0 in enumerate(range(0, N, 512)):
            n1 = min(N, n0 + 512)
            p = po[j % 4]
            j += 1
            for kc in range(8):
                c.mm(p, p[:, 0:n1 - n0], hT, hT[:, kc, :], wb, wb[:, kc, n0:n1], start=(kc == 0), stop=(kc == 7))
            c.cp("act" if n % 2 == 0 else "dve", o, o[:, n0:n1], p, p[:, 0:n1 - n0])
        if not rope:
            c.dma("sp", out[t * 128:(t + 1) * 128, :], o[:], reads=[o])
            continue
        qk = o[:, 256:1792].rearrange("p (a h e) -> p a h e", a=24, h=2)
        x1 = qk[:, :, 0, :]
        x2 = qk[:, :, 1, :]
        cb = cs_t[:, 0:1, :].broadcast_to([128, 24, 32])
        sb_ = cs_t[:, 1:2, :].broadcast_to([128, 24, 32])
        c.tt("dve", r1, r1[:], o, x1, cs_t, cb, ALU.mult)
        c.tt("pool", r2, r2[:], o, x2, cs_t, sb_, ALU.mult)
        c.tt("dve", r3, r3[:], o, x2, cs_t, cb, ALU.mult)
        c.tt("pool", r4, r4[:], o, x1, cs_t, sb_, ALU.mult)
        c.tt("dve", o, x1, r1, r1[:], r2, r2[:], ALU.subtract)
        c.tt("pool", o, x2, r3, r3[:], r4, r4[:], ALU.add)
        c.dma("sp", out[t * 128:(t + 1) * 128, :], o[:], reads=[o])
    c.finish()
    return nc


def rope_tables():
    t = np.arange(16384)
    row = (t // 64).astype(np.float32)
    col = (t % 64).astype(np.float32)
    inv = (np.float32(10000.0) ** (-np.arange(0, 32, 2, dtype=np.float32) / np.float32(32))).astype(np.float32)
    ang = np.concatenate([row[:, None] * inv, col[:, None] * inv], axis=-1).astype(np.float32)
    return np.cos(ang).astype(np.float32), np.sin(ang).astype(np.float32)


def run_l0proj(inp, mod):
    cos, sin = rope_tables()
    ident = np.eye(128, dtype=np.float32)
    maps = []
    for k in range(NCORES):
        b, r = k // 4, k % 4
        xs = np.zeros((L0_T, D), np.float32)
        xs[:4096] = inp["x"][b, r * 4096:(r + 1) * 4096]
        xs[4096:4160] = inp["ctx"][b, r * 64:(r + 1) * 64]
        cs = np.zeros((L0_T, 2, 32), np.float32)
        cs[:4096, 0] = cos[r * 4096:(r + 1) * 4096]
        cs[:4096, 1] = sin[r * 4096:(r + 1) * 4096]
        cs[4096:, 0] = 1.0
        m = np.stack([mod[0, b].reshape(6, D)[0:2], mod[0, 2].reshape(6, D)[0:2]])
        maps.append({"x": xs, "g": inp["norm1_g"][0], "mod": np.ascontiguousarray(m), "w": inp["ev_w_in"][0],
                     "cs": cs, "ident": ident})
    res = run(build_l0proj(), maps)
    p_lat = np.zeros((2, 16384, EVEN_IN), np.float32)
    p_ctx = np.zeros((2, 256, EVEN_IN), np.float32)
    for k in range(NCORES):
        b, r = k // 4, k % 4
        p_lat[b, r * 4096:(r + 1) * 4096] = res[k]["p"][:4096]
        p_ctx[b, r * 64:(r + 1) * 64] = res[k]["p"][4096:4160]
    return p_lat, p_ctx


NKEY = 16640
NKT = NKEY // 128
NQ = 4160


def lam_tile(c, lam_ap, lam_init, neg=True):
    lt = c.sb([128, 4, 64], F32, "lam_in")
    c.dma("sp", lt[:], lam_ap.rearrange("a d -> (a d)").partition_broadcast(128), writes=[lt])
    pr = c.sb([128, 2, 64], F32, "lam_pr")
    c.tt("dve", pr, pr[:, 0, :], lt, lt[:, 0, :], lt, lt[:, 1, :], ALU.mult)
    c.tt("dve", pr, pr[:, 1, :], lt, lt[:, 2, :], lt, lt[:, 3, :], ALU.mult)
    sm = c.sb([128, 2], F32, "lam_sm")
    c.op("dve", lambda: c.nc.vector.reduce_sum(sm[:], pr[:], AX.X), reads=[pr], writes=[sm])
    c.act(sm, sm[:], sm, sm[:], AF.Exp)
    lam = c.sb([128, 1], F32, "lam_sb")
    c.tt("dve", lam, lam[:], sm, sm[:, 0:1], sm, sm[:, 1:2], ALU.subtract)
    if neg:
        c.ts("dve", lam, lam[:], lam, lam[:], lam_init, -1.0, ALU.add, ALU.mult)
    else:
        c.ts("dve", lam, lam[:], lam, lam[:], lam_init, None, ALU.add)
    return lam


def build_attn(nheads=6, nq=NQ, nkt=NKT, lam_init=0.2, dbg=False):
    nc = new_nc()
    if dbg:
        dbg_out = dram_out(nc, "dbg", [6, 128, 512])
    nkey = nkt * 128
    qT = dram_in(nc, "qT", [nheads, 128, nq])
    kT = dram_in(nc, "kT", [nheads, 128, nkey])
    v = dram_in(nc, "v", [nkey, nheads, 128])
    lam_ap = dram_in(nc, "lam", [4, 64])
    out = dram_out(nc, "oT", [nheads, 128, nq])
    c = Ctx(nc)
    nlam = lam_tile(c, lam_ap, lam_init)
    ones = c.sb([128, 128], BF16, "ones")
    c.op("dve", lambda: nc.vector.memset(ones[:], 1.0), writes=[ones])
    onesf = c.sb([1, 128], F32, "onesf")
    c.op("dve", lambda: nc.vector.memset(onesf[:], 1.0), writes=[onesf])
    KT = c.sb([128, nkey], BF16, "KT")
    V = c.sb([128, nkt, 128], BF16, "V")
    QT = c.sb([128, nq], BF16, "QT")
    CH = 2048
    st = [c.sb([128, CH], F32, f"st{i}") for i in range(2)]
    S = [[c.ps([128, 512], F32, f"S{m}{i}") for i in range(2)] for m in range(2)]
    OT = [c.ps([128, 512], F32, f"OT{m}") for m in range(2)]
    SUM = [c.ps([1, 512], F32, f"SUM{m}") for m in range(2)]
    P = [[c.sb([128, 512], BF16, f"P{m}{i}") for i in range(2)] for m in range(2)]
    rc = [c.sb([1, 512], F32, f"rc{m}") for m in range(2)]
    o0 = c.sb([128, 512], F32, "o0")
    o1 = c.sb([128, 512], F32, "o1")
    ob = [c.sb([128, 512], F32, f"ob{i}") for i in range(2)]
    si = 0
    nblk = 0
    vv = v.rearrange("(t p) h e -> p t h e", p=128)
    for h in range(nheads):
        for (dst, src, n) in ((KT, kT, nkey), (QT, qT, nq)):
            for c0 in range(0, n, CH):
                c1 = min(n, c0 + CH)
                s = st[si % 2]
                c.dma("sp", s[:, 0:c1 - c0], src[h, :, c0:c1], writes=[s])
                c.cp(("dve", "pool")[si % 2], dst, dst[:, c0:c1], s, s[:, 0:c1 - c0])
                si += 1
        for t0 in range(0, nkt, 16):
            t1 = min(nkt, t0 + 16)
            s = st[si % 2]
            sv = s[:, 0:(t1 - t0) * 128].rearrange("p (t e) -> p t e", e=128)
            c.dma("sp", sv, vv[:, t0:t1, h, :], writes=[s])
            c.cp(("dve", "pool")[si % 2], V, V[:, t0:t1, :], s, sv)
            si += 1
        blocks = [(q0, min(512, nq - 64 - q0), 0, nkt) for q0 in range(0, nq - 64, 512)]
        blocks.append((nq - 64, 64, nkt - 2, nkt))
        for (q0, n, kt0, kt1) in blocks:
            for kt in range(kt0, kt1):
                for m in range(2):
                    s_ = S[m][kt % 2]
                    p_ = P[m][kt % 2]
                    c.mm(s_, s_[:, 0:n], KT, KT[m * 64:(m + 1) * 64, kt * 128:(kt + 1) * 128],
                         QT, QT[m * 64:(m + 1) * 64, q0:q0 + n])
                    c.act(p_, p_[:, 0:n], s_, s_[:, 0:n], AF.Exp, scale=0.125)
                    c.mm(OT[m], OT[m][:, 0:n], V, V[:, kt, :], p_, p_[:, 0:n], start=(kt == kt0), stop=(kt == kt1 - 1))
                    c.mm(SUM[m], SUM[m][0:1, 0:n], ones, ones[:, 0:1], p_, p_[:, 0:n], start=(kt == kt0), stop=(kt == kt1 - 1))
            for m in range(2):
                c.op("dve", lambda m=m: nc.vector.reciprocal(rc[m][0:1, 0:n], SUM[m][0:1, 0:n]), reads=[SUM[m]], writes=[rc[m]])
                bc = S[m][0]
                c.mm(bc, bc[:, 0:n], onesf, onesf[0:1, :], rc[m], rc[m][0:1, 0:n])
                dst = o0 if m == 0 else o1
                c.cp("act", dst, dst[:, 0:n], OT[m], OT[m][:, 0:n])
                c.tt("dve", dst, dst[:, 0:n], dst, dst[:, 0:n], bc, bc[:, 0:n], ALU.mult)
            o = ob[nblk % 2]
            nblk += 1
            c.stt("dve", o, o[:, 0:n], o1, o1[:, 0:n], nlam[:, 0:1], o0, o0[:, 0:n], ALU.mult, ALU.add, extra_reads=[nlam])
            c.dma("pool", out[h, :, q0:q0 + n], o[:, 0:n], reads=[o])
    if dbg:
        d = c.sb([128, 512], F32, "dbgt")
        for i, (src, np_) in enumerate(((P[0][0], 128), (P[1][1], 128), (o0, 128), (o1, 128), (rc[0], 1), (nlam, 128))):
            w = 1 if src is nlam else 512
            c.cp("dve", d, d[0:np_, 0:w], src, src[0:np_, 0:w])
            c.dma("sp", dbg_out[i, 0:np_, 0:w], d[0:np_, 0:w], reads=[d], allow_slow_non_contiguous=True)
    c.finish()
    return nc


def lam_init_of(l):
    return 0.8 - 0.6 * math.exp(-0.3 * l)


def run_attn(inp, p_lat, p_ctx):
    maps = []
    for k in range(NCORES):
        b, r = k // 4, k % 4
        pk = np.concatenate([p_lat[b], p_ctx[b]], axis=0)
        pq = np.concatenate([p_lat[b, r * 4096:(r + 1) * 4096], p_ctx[b, r * 64:(r + 1) * 64]], axis=0)
        qT = np.ascontiguousarray(pq[:, 256:1024].reshape(NQ, 6, 128).transpose(1, 2, 0))
        kT = np.ascontiguousarray(pk[:, 1024:1792].reshape(NKEY, 6, 128).transpose(1, 2, 0))
        v = np.ascontiguousarray(pk[:, 1792:2560].reshape(NKEY, 6, 128))
        maps.append({"qT": qT, "kT": kT, "v": v, "lam": inp["ev_lam"][0]})
    res = run(build_attn(lam_init=lam_init_of(0)), maps)
    o_lat = np.zeros((2, 16384, 6, 128), np.float32)
    o_ctx = np.zeros((2, 256, 6, 128), np.float32)
    for k in range(NCORES):
        b, r = k // 4, k % 4
        o = res[k]["oT"].transpose(2, 0, 1)
        o_lat[b, r * 4096:(r + 1) * 4096] = o[:4096]
        o_ctx[b, r * 64:(r + 1) * 64] = o[4096:]
    return o_lat, o_ctx


def fourier_tables():
    c64 = np.arange(64)
    a64 = 2 * np.pi * np.outer(c64, c64) / 64
    cs64 = np.concatenate([np.cos(a64), -np.sin(a64)], 1).astype(np.float32)
    n = np.arange(128)
    a = 2 * np.pi * np.outer(n, n) / 128
    cs128 = np.concatenate([np.cos(a), -np.sin(a)], 1).astype(np.float32)
    sc128 = np.concatenate([np.sin(a), np.cos(a)], 1).astype(np.float32)
    tw = 2 * np.pi * np.outer(n, n) / 16384.0
    twt = np.stack([np.cos(tw), np.sin(tw)], 0).astype(np.float32)
    m = np.arange(256)
    a256 = 2 * np.pi * np.outer(m, m) / 256
    cs256 = np.stack([np.cos(a256), np.sin(a256)], 0).astype(np.float32)
    return {"cs64": cs64, "cs128": cs128, "sc128": sc128, "tw": twt, "cs256": cs256}


def build_fourier():
    nc = new_nc()
    fT = dram_in(nc, "fT", [64, 16384])
    fcT = dram_in(nc, "fcT", [64, 256])
    cs64 = dram_in(nc, "cs64", [64, 128])
    cs128 = dram_in(nc, "cs128", [128, 256])
    sc128 = dram_in(nc, "sc128", [128, 256])
    tw = dram_in(nc, "tw", [2, 128, 128])
    cs256 = dram_in(nc, "cs256", [2, 256, 256])
    out = dram_out(nc, "fo", [128, 64, 128])
    outc = dram_out(nc, "foc", [256, 64])
    c = Ctx(nc)

    def ld_bf(ap, shape, name, q="sp"):
        f = c.sb(shape, F32, name + "_f")
        b = c.sb(shape, BF16, name)
        c.dma(q, f[:], ap, writes=[f])
        c.cp("dve", b, b[:], f, f[:])
        return b
    FT = c.sb([64, 16384], BF16, "FT")
    stg = [c.sb([64, 4096], F32, f"fst{i}") for i in range(2)]
    for i in range(4):
        s = stg[i % 2]
        c.dma("sp", s[:], fT[:, i * 4096:(i + 1) * 4096], writes=[s])
        c.cp(("dve", "pool")[i % 2], FT, FT[:, i * 4096:(i + 1) * 4096], s, s[:])
    CS64 = ld_bf(cs64[:, :], [64, 128], "CS64")
    CS128 = ld_bf(cs128[:, :], [128, 256], "CS128")
    SC128 = ld_bf(sc128[:, :], [128, 256], "SC128")
    TW = c.sb([128, 2, 128], F32, "TW")
    c.dma("sp", TW[:], tw.rearrange("a p k -> p a k"), writes=[TW])
    ps = [c.ps([128, 512], F32, f"fp{i}") for i in range(4)]
    G = c.sb([128, 128, 128], BF16, "G")
    FTv = FT[:, :].rearrange("c (a b) -> c a b", b=128)
    for j in range(32):
        p = ps[j % 4]
        for u in range(4):
            t2 = j * 4 + u
            c.mm(p, p[:, u * 128:(u + 1) * 128], FT, FTv[:, :, t2], CS64, CS64[:, :])
        c.cp(("act", "dve")[j % 2], G, G[:, j * 4:(j + 1) * 4, :].rearrange("p a b -> p (a b)"), p, p[:])
    Br = c.sb([128, 64, 128], BF16, "Br")
    Bi = c.sb([128, 64, 128], BF16, "Bi")
    tmp = [c.sb([128, 2, 256], F32, f"ftmp{i}") for i in range(2)]
    t1_ = [c.sb([128, 2, 128], F32, f"ft1{i}") for i in range(2)]
    t2_ = [c.sb([128, 2, 128], F32, f"ft2{i}") for i in range(2)]
    for j in range(32):
        p = ps[j % 4]
        for u in range(2):
            cp_ = j * 2 + u
            c.mm(p, p[:, u * 256:(u + 1) * 256], G, G[:, :, cp_], CS128, CS128[:, :], start=True, stop=False)
            c.mm(p, p[:, u * 256:(u + 1) * 256], G, G[:, :, 64 + cp_], SC128, SC128[:, :], start=False, stop=True)
        A = tmp[j % 2]
        c.cp("act", A, A[:].rearrange("p a b -> p (a b)"), p, p[:])
        Ar = A[:, :, 0:128]
        Ai = A[:, :, 128:256]
        cw = TW[:, 0:1, :].broadcast_to([128, 2, 128])
        sw = TW[:, 1:2, :].broadcast_to([128, 2, 128])
        a1, a2 = t1_[j % 2], t2_[j % 2]
        c.tt("dve", a1, a1[:], A, Ar, TW, cw, ALU.mult)
        c.tt("pool", a2, a2[:], A, Ai, TW, sw, ALU.mult)
        c.tt("dve", Br, Br[:, j * 2:(j + 1) * 2, :], a1, a1[:], a2, a2[:], ALU.add)
        c.tt("pool", a2, a2[:], A, Ai, TW, cw, ALU.mult)
        c.tt("dve", a1, a1[:], A, Ar, TW, sw, ALU.mult)
        c.tt("pool", Bi, Bi[:, j * 2:(j + 1) * 2, :], a2, a2[:], a1, a1[:], ALU.subtract)
    ot = [c.sb([128, 512], F32, f"fot{i}") for i in range(2)]
    Brv = Br[:].rearrange("p a b -> p (a b)")
    Biv = Bi[:].rearrange("p a b -> p (a b)")
    outv = out.rearrange("k c j -> k (c j)")
    for j in range(16):
        p = ps[j % 4]
        c.mm(p, p[:], CS128, CS128[:, 0:128], Br, Brv[:, j * 512:(j + 1) * 512], start=True, stop=False)
        c.mm(p, p[:], SC128, SC128[:, 0:128], Bi, Biv[:, j * 512:(j + 1) * 512], start=False, stop=True)
        o = ot[j % 2]
        c.act(o, o[:], p, p[:], AF.Copy, scale=1.0 / 1024.0)
        c.dma("sp", outv[:, j * 512:(j + 1) * 512], o[:], reads=[o])
    FC = ld_bf(fcT[:, :], [64, 256], "FC")
    C256 = ld_bf(cs256.rearrange("a (kc p) k -> p a kc k", p=128), [128, 2, 2, 256], "C256")
    Gc = c.sb([128, 2, 128], BF16, "Gc")
    p = ps[0]
    for tcn in range(2):
        c.mm(p, p[:, tcn * 128:(tcn + 1) * 128], FC, FC[:, tcn * 128:(tcn + 1) * 128], CS64, CS64[:, :])
    c.cp("act", Gc, Gc[:].rearrange("p a b -> p (a b)"), p, p[:, 0:256])
    oc = c.sb([128, 2, 64], F32, "foc_t")
    p = ps[1]
    for kc in range(2):
        n = 0
        for tcn in range(2):
            c.mm(p, p[:, kc * 64:(kc + 1) * 64], C256, C256[:, 0, tcn, kc * 128:(kc + 1) * 128], Gc, Gc[:, tcn, 0:64], start=(n == 0), stop=False)
            n += 1
            c.mm(p, p[:, kc * 64:(kc + 1) * 64], C256, C256[:, 1, tcn, kc * 128:(kc + 1) * 128], Gc, Gc[:, tcn, 64:128], start=False, stop=(tcn == 1))
    c.act(oc, oc[:].rearrange("p a b -> p (a b)"), p, p[:, 0:128], AF.Copy, scale=1.0 / 128.0)
    c.dma("sp", outc.rearrange("(kc p) c -> p kc c", p=128), oc[:], reads=[oc])
    c.finish()
    return nc


def run_fourier(p_lat, p_ctx):
    tabs = fourier_tables()
    maps = []
    for k in range(NCORES):
        b, g = k // 4, k % 4
        m = dict(tabs)
        m["fT"] = np.ascontiguousarray(p_lat[b, :, g * 64:(g + 1) * 64].T)
        m["fcT"] = np.ascontiguousarray(p_ctx[b, :, g * 64:(g + 1) * 64].T)
        maps.append(m)
    res = run(build_fourier(), maps)
    fl = np.zeros((2, 16384, 256), np.float32)
    fc = np.zeros((2, 256, 256), np.float32)
    for k in range(NCORES):
        b, g = k // 4, k % 4
        fo = res[k]["fo"]
        fl[b, :, g * 64:(g + 1) * 64] = fo.transpose(0, 2, 1).reshape(16384, 64)
        fc[b, :, g * 64:(g + 1) * 64] = res[k]["foc"]
    return fl, fc


class DB:
    def __init__(self):
        self.last_w = None
        self.reads = {}


def bcast_tile(c, ap_row, name, n=D, q="sp"):
    t = c.sb([128, n], F32, name)
    c.dma(q, t[:], ap_row.partition_broadcast(128), writes=[t])
    return t


def build_post(layer, ntiles, nlat, last, passes, lam_init=0.2):
    nc = new_nc()
    T = ntiles * 128
    Kmix = 1024 if layer == 0 else 2048
    KC = Kmix // 128
    x = dram_in(nc, "x", [T, D])
    if layer == 0:
        o_in = dram_in(nc, "o", [T, 768])
        f_in = dram_in(nc, "fo", [T, 256])
    else:
        of_in = dram_in(nc, "of", [T, 2048])
        ob_in = dram_in(nc, "ob", [T, 2048])
        z_in = dram_in(nc, "z", [T, 2048])
    sg = dram_in(nc, "sg", [128])
    g2 = dram_in(nc, "g2", [D])
    mod = dram_in(nc, "mod", [2, 6, D])
    wout = dram_in(nc, "wout", [Kmix, D])
    wr = dram_in(nc, "wr", [D, 32])
    br = dram_in(nc, "br", [32])
    w1 = dram_in(nc, "w1", [32, D, 2048])
    b1 = dram_in(nc, "b1", [32, 2048])
    w2 = dram_in(nc, "w2", [32, D, D])
    b2 = dram_in(nc, "b2", [32, D])
    ident = dram_in(nc, "ident", [128, 128])
    if last:
        fg = dram_in(nc, "fg", [D])
    xo = dram_out(nc, "xo", [T, D])
    h2s = nc.dram_tensor("h2s", [128, 8, T], BF16, kind="Internal").ap()
    c = Ctx(nc)
    idf, idb = load_ident(c, ident)
    gates = c.sb([128, ntiles, 32], F32, "gates")
    xo_db = [DB() for _ in range(ntiles)]
    h2_db = [DB() for _ in range(ntiles)]
    nseg = 1 if nlat == ntiles else 2

    c.push()
    woutb = load_weight_bf16(c, wout, Kmix, D, "woutb")
    wrf = c.sb([128, 8, 32], F32, "wrf")
    c.dma("sp", wrf[:], wr.rearrange("(kc p) n -> p kc n", p=128), writes=[wrf])
    brt = bcast_tile(c, br, "brt", 32)
    sgt = bcast_tile(c, sg, "sgt", 128)
    if layer == 0:
        c.ts("dve", sgt, sgt[:], sgt, sgt[:], 1.0 - lam_init, None, ALU.mult)
    mods2 = [make_mod_tiles(c, g2, mod, s_, 3, 4, f"m2{s_}") for s_ in range(nseg)]
    gate_m = [bcast_tile(c, mod[s_, 2:3, :], f"gm{s_}") for s_ in range(nseg)]
    nt = NormT(c, idb)
    mixp = [c.ps([128, 8, 128], BF16, "mixp0")]
    yp = [c.ps([128, 512], F32, f"yp{i}") for i in range(2)]
    hfp = [c.ps([128, 4, 128], F32, f"hfp{i}") for i in range(2)]
    lgp = c.ps([128, 32], F32, "lgp")
    mixed = c.sb([128, Kmix], BF16, "mixed")
    mixT = c.sb([128, KC, 128], BF16, "mixT")
    x1 = [c.sb([128, D], F32, f"x1_{i}") for i in range(2)]
    hf = c.sb([128, D], F32, "hf")
    hTf = c.sb([128, 8, 128], F32, "hTf")
    lg = c.sb([128, 32], F32, "lg")
    mx8 = c.sb([128, 8], F32, "mx8")
    msk = c.sb([128, 32], F32, "msk")
    ex = c.sb([128, 32], F32, "ex")
    sm = c.sb([128, 1], F32, "gsm")
    ytmp = c.sb([128, D], F32, "ytmp")
    if layer == 0:
        ot = [c.sb([128, 768], F32, f"o_t{i}") for i in range(2)]
        ft = [c.sb([128, 256], F32, f"f_t{i}") for i in range(2)]
        osq = c.sb([128, 6, 128], F32, "osq")
        oss = c.sb([128, 6], F32, "oss")
        nh_, eps_h = 6, 1e-5
    else:
        oft = [c.sb([128, 2048], F32, f"of_t{i}") for i in range(2)]
        obt = [c.sb([128, 2048], F32, f"ob_t{i}") for i in range(2)]
        zt = [c.sb([128, 2048], F32, f"z_t{i}") for i in range(2)]
        osq = c.sb([128, 16, 128], F32, "osq")
        oss = c.sb([128, 16], F32, "oss")
        nh_, eps_h = 16, 1e-6
    for t in range(ntiles):
        seg = 0 if t < nlat else 1
        rows = slice(t * 128, (t + 1) * 128)
        xt = nt.xt[t % 2]
        c.dma("sp", xt[:], x[rows, :], writes=[xt])
        if layer == 0:
            o_ = ot[t % 2]
            f_ = ft[t % 2]
            c.dma("pool", o_[:], o_in[rows, :], writes=[o_])
            c.dma("pool", f_[:], f_in[rows, :], writes=[f_])
            src = o_
            c.cp("act", mixed, mixed[:, 0:256], f_, f_[:])
            moff = 256
        else:
            a_, b_, z_ = oft[t % 2], obt[t % 2], zt[t % 2]
            c.dma("pool", a_[:], of_in[rows, :], writes=[a_])
            c.dma("pool", b_[:], ob_in[rows, :], writes=[b_])
            c.dma("sp", z_[:], z_in[rows, :], writes=[z_])
            c.tt("pool", a_, a_[:], a_, a_[:], b_, b_[:], ALU.add)
            c.act(z_, z_[:], z_, z_[:], AF.Silu)
            src = a_
            moff = 0
        sv_ = src[:, :].rearrange("p (h e) -> p h e", e=128)
        c.tt("pool", osq, osq[:], src, sv_, src, sv_, ALU.mult)
        c.op("dve", lambda: nc.vector.reduce_sum(oss[:], osq[:], AX.X), reads=[osq], writes=[oss])
        c.ts("dve", oss, oss[:], oss, oss[:], 1.0 / 128, eps_h, ALU.mult, ALU.add)
        c.op("act", lambda: nc.scalar.sqrt(oss[:], oss[:]), reads=[oss], writes=[oss])
        c.op("dve", lambda: nc.vector.reciprocal(oss[:], oss[:]), reads=[oss], writes=[oss])
        c.tt("dve", osq, osq[:], src, sv_, oss, oss[:].unsqueeze(2).broadcast_to([128, nh_, 128]), ALU.mult)
        mv = mixed[:, moff:Kmix].rearrange("p (h e) -> p h e", e=128)
        sgb = sgt[:].unsqueeze(1).broadcast_to([128, nh_, 128])
        if layer == 0:
            c.tt("pool", mixed, mv, osq, osq[:], sgt, sgb, ALU.mult)
        else:
            c.tt("pool", osq, osq[:], osq, osq[:], sgt, sgb, ALU.mult)
            c.tt("dve", mixed, mv, osq, osq[:], z_, z_[:, :].rearrange("p (h e) -> p h e", e=128), ALU.mult)
        for i_ in range(KC // 8):
            mp = mixp[0]
            for kc in range(i_ * 8, (i_ + 1) * 8):
                c.tr(mp, mp[:, kc % 8, :], mixed, mixed[:, kc * 128:(kc + 1) * 128], idb)
            c.cp(("act", "dve")[i_ % 2], mixT, mixT[:, i_ * 8:(i_ + 1) * 8, :], mp, mp[:])
        x1t = x1[t % 2]
        for n in range(2):
            for kc in range(KC):
                c.mm(yp[n], yp[n][:], mixT, mixT[:, kc, :], woutb, woutb[:, kc, n * 512:(n + 1) * 512],
                     start=(kc == 0), stop=(kc == KC - 1))
            cs_ = slice(n * 512, (n + 1) * 512)
            c.tt("dve", ytmp, ytmp[:, cs_], yp[n], yp[n][:], gate_m[seg], gate_m[seg][:, cs_], ALU.mult)
            c.tt("pool", x1t, x1t[:, cs_], ytmp, ytmp[:, cs_], xt, xt[:, cs_], ALU.add)
        c.dma("sp", xo[rows, :], x1t[:], reads=[x1t], writes=[xo_db[t]])
        G2, S2 = mods2[seg]
        hT = nt.run(None, G2, S2, xt=x1t, hf=hf)
        c.dma("pool", h2s[:, :, rows], hT[:], reads=[hT], writes=[h2_db[t]])
        for kc in range(8):
            hp = hfp[kc // 4]
            c.tr(hp, hp[:, kc % 4, :], hf, hf[:, kc * 128:(kc + 1) * 128], idf)
        for i_ in range(2):
            c.cp(("act", "dve")[i_], hTf, hTf[:, i_ * 4:(i_ + 1) * 4, :], hfp[i_], hfp[i_][:])
        for kc in range(8):
            c.mm(lgp, lgp[:], hTf, hTf[:, kc, :], wrf, wrf[:, kc, :], start=(kc == 0), stop=(kc == 7))
        c.tt("dve", lg, lg[:], lgp, lgp[:], brt, brt[:], ALU.add)
        c.op("dve", lambda: nc.vector.max(out=mx8[:], in_=lg[:]), reads=[lg], writes=[mx8])
        c.ts("dve", msk, msk[:], lg, lg[:], mx8[:, 3:4], None, ALU.is_ge, extra_reads=[mx8])
        c.ts("dve", mx8, mx8[:, 0:1], mx8, mx8[:, 0:1], -1.0, None, ALU.mult)
        c.act(ex, ex[:], lg, lg[:], AF.Exp, bias=mx8[:, 0:1], extra_reads=[mx8])
        c.tt("dve", ex, ex[:], ex, ex[:], msk, msk[:], ALU.mult)
        c.op("dve", lambda: nc.vector.reduce_sum(sm[:], ex[:], AX.X), reads=[ex], writes=[sm])
        c.op("dve", lambda: nc.vector.reciprocal(sm[:], sm[:]), reads=[sm], writes=[sm])
        c.ts("dve", gates, gates[:, t, :], ex, ex[:], sm[:, 0:1], None, ALU.mult, extra_reads=[sm])
    c.pop()

    c.push()
    maxp = max(passes)
    H2T = c.sb([128, 8, maxp * 128], BF16, "H2T")
    acc = c.sb([128, maxp, D], F32, "acc")
    w1g = c.sb([128, 8, 1024], BF16, "w1g")
    w1l = c.sb([128, 8, 1024], BF16, "w1l")
    w2b = [c.sb([128, 8, 1024], BF16, f"w2b{i}") for i in range(2)]
    stg = [c.sb([128, 2048], F32, f"wst{i}") for i in range(2)]
    b1t = [c.sb([128, 8, 2], F32, f"b1t{i}") for i in range(2)]
    b2f = c.sb([1, D], F32, "b2f")
    b2b = [c.sb([1, D], BF16, f"b2b{i}") for i in range(2)]
    onesb = c.sb([1, 128], BF16, "onesb")
    c.op("dve", lambda: nc.vector.memset(onesb[:], 1.0), writes=[onesb])
    aT = c.sb([128, 8, maxp * 128], BF16, "aT")
    glu = [c.sb([128, 512], F32, f"glu{i}") for i in range(2)]
    sig = [c.sb([128, 512], F32, "sig0")] * 2
    lin = [c.sb([128, 512], F32, f"lin{i}") for i in range(2)]
    ugp = [c.ps([128, 512], F32, f"ugp{i}") for i in range(2)]
    ulp = [c.ps([128, 512], F32, f"ulp{i}") for i in range(2)]
    ypp = [c.ps([128, 512], F32, f"ypp{i}") for i in range(4)]
    gate5 = [bcast_tile(c, mod[s_, 5:6, :], f"g5{s_}") for s_ in range(nseg)]
    xr = [c.sb([128, D], F32, "xr0")] * 2
    if last:
        fgt = bcast_tile(c, fg, "fgt")
        fsq = c.sb([128, D], F32, "fsq")
        fss = c.sb([128, 1], F32, "fss")
    si = 0
    jj = 0
    yi = 0
    ei = 0
    tbase = 0
    for npass in passes:
        tiles = list(range(tbase, tbase + npass))
        ntok = npass * 128
        c.dma("sp", H2T[:, :, 0:ntok], h2s[:, :, tbase * 128:tbase * 128 + ntok], reads=[h2_db[t] for t in tiles], writes=[H2T])
        c.op("pool", lambda: nc.gpsimd.memset(acc[:], 0.0), writes=[acc])
        for e in range(32):
            w2e = w2b[ei % 2]
            b1e = b1t[ei % 2]
            b2e = b2b[ei % 2]
            ei += 1
            for kc in range(8):
                s_ = stg[si % 2]
                c.dma(("sp", "pool")[si % 2], s_[:], w1[e, kc * 128:(kc + 1) * 128, :], writes=[s_])
                sv2 = s_[:, :].rearrange("p (n two) -> p n two", two=2)
                c.cp("act", w1g, w1g[:, kc, :], s_, sv2[:, :, 0])
                c.cp("pool", w1l, w1l[:, kc, :], s_, sv2[:, :, 1])
                si += 1
            c.dma("sp", b1e[:], b1[e].rearrange("(c p two) -> p c two", p=128, two=2), writes=[b1e])
            c.dma("sp", b2f[:], b2[e:e + 1, :], writes=[b2f])
            c.cp("pool", b2e, b2e[:], b2f, b2f[:])
            for kc2 in range(4):
                s_ = stg[si % 2]
                c.dma(("sp", "pool")[si % 2], s_[:].rearrange("p (a n) -> p a n", a=2),
                      w2[e, kc2 * 256:(kc2 + 1) * 256, :].rearrange("(a p) n -> p a n", p=128), writes=[s_])
                c.cp(("act", "pool")[kc2 % 2], w2e, w2e[:, kc2 * 2:kc2 * 2 + 2, :].rearrange("p a n -> p (a n)"), s_, s_[:])
                si += 1
            for blk0 in range(0, npass, 4):
                nb = min(4, npass - blk0)
                n = nb * 128
                tok0 = blk0 * 128
                for j in range(8):
                    ug, ul = ugp[jj % 2], ulp[jj % 2]
                    gl, sg_, ln = glu[jj % 2], sig[jj % 2], lin[jj % 2]
                    jj += 1
                    for kc in range(8):
                        c.mm(ug, ug[:, 0:n], w1g, w1g[:, kc, j * 128:(j + 1) * 128], H2T, H2T[:, kc, tok0:tok0 + n],
                             start=(kc == 0), stop=(kc == 7))
                    for kc in range(8):
                        c.mm(ul, ul[:, 0:n], w1l, w1l[:, kc, j * 128:(j + 1) * 128], H2T, H2T[:, kc, tok0:tok0 + n],
                             start=(kc == 0), stop=(kc == 7))
                    c.ts("dve", gl, gl[:, 0:n], ug, ug[:, 0:n], b1e[:, j, 0:1], 7.0, ALU.add, ALU.min, extra_reads=[b1e])
                    c.act(sg_, sg_[:, 0:n], gl, gl[:, 0:n], AF.Sigmoid, scale=1.702)
                    c.ts("dve", ln, ln[:, 0:n], ul, ul[:, 0:n], b1e[:, j, 1:2], 7.0, ALU.add, ALU.min, extra_reads=[b1e])
                    c.ts("pool", ln, ln[:, 0:n], ln, ln[:, 0:n], -7.0, 1.0, ALU.max, ALU.add)
                    c.tt("pool", gl, gl[:, 0:n], gl, gl[:, 0:n], sg_, sg_[:, 0:n], ALU.mult)
                    c.tt("pool", aT, aT[:, j, tok0:tok0 + n], gl, gl[:, 0:n], ln, ln[:, 0:n], ALU.mult)
            for tl in range(npass):
                for nh in range(2):
                    y_ = ypp[yi % 4]
                    yi += 1
                    cs_ = slice(nh * 512, (nh + 1) * 512)
                    c.mm(y_, y_[:], onesb, onesb[0:1, :], b2e, b2e[0:1, cs_], start=True, stop=False)
                    for j in range(8):
                        c.mm(y_, y_[:], aT, aT[:, j, tl * 128:(tl + 1) * 128], w2e, w2e[:, j, cs_],
                             start=False, stop=(j == 7))
                    c.stt("dve", acc, acc[:, tl, cs_], y_, y_[:], gates[:, tbase + tl, e:e + 1], acc, acc[:, tl, cs_],
                          ALU.mult, ALU.add, extra_reads=[gates])
        for tl, t in enumerate(tiles):
            seg = 0 if t < nlat else 1
            rows = slice(t * 128, (t + 1) * 128)
            xr_ = xr[t % 2]
            c.dma("sp", xr_[:], xo[rows, :], reads=[xo_db[t]], writes=[xr_])
            c.tt("pool", acc, acc[:, tl, :], acc, acc[:, tl, :], gate5[seg], gate5[seg][:], ALU.mult)
            c.tt("dve", xr_, xr_[:], xr_, xr_[:], acc, acc[:, tl, :], ALU.add)
            if last:
                c.act(fsq, fsq[:], xr_, xr_[:], AF.Square, accum=(fss, fss[:, 0:1]))
                c.ts("dve", fss, fss[:], fss, fss[:], 1.0 / D, 1e-6, ALU.mult, ALU.add)
                c.op("act", lambda: nc.scalar.sqrt(fss[:], fss[:]), reads=[fss], writes=[fss])
                c.op("dve", lambda: nc.vector.reciprocal(fss[:], fss[:]), reads=[fss], writes=[fss])
                c.stt("dve", xr_, xr_[:], xr_, xr_[:], fss[:, 0:1], fgt, fgt[:], ALU.mult, ALU.mult, extra_reads=[fss])
            c.dma("sp", xo[rows, :], xr_[:], reads=[xr_], writes=[xo_db[t]])
        tbase += npass
    c.pop()
    c.finish()
    return nc


CH = 64


def gdn_consts():
    i = np.arange(64)
    tri = (i[:, None] <= i[None, :]).astype(np.float32)
    sel = np.zeros((64, 128), np.float32)
    sel[63, :] = 1.0
    mincl = (i[:, None] >= i[None, :]).astype(np.float32)
    negm = (mincl - 1.0) * 30000.0
    mstrict = (i[:, None] > i[None, :]).astype(np.float32)
    return {"tri": tri, "sel": sel, "mincl": mincl, "negm": negm.astype(np.float32), "mstrict": mstrict,
            "identf": np.eye(128, dtype=np.float32)}


def build_gdn(nlat_groups=32, gch=8, nseq=8):
    nc = new_nc()
    nctx_ch = 4
    nchunks = nctx_ch + nlat_groups * gch
    ntok = nchunks * CH
    nlat = nlat_groups * gch * CH
    TP = 2 + 256 + 2 + 2 + nlat + 2
    NX = nchunks * nseq
    nqk = max(2, nseq // 2) if nseq > 1 else 1
    qk_pre = dram_in(nc, "qk_pre", [nqk, 2, 128, TP])
    v_pre = dram_in(nc, "v_pre", [nseq, 128, TP])
    cw_qk = dram_in(nc, "cw_qk", [nqk, 2, 128, 5])
    cw_v = dram_in(nc, "cw_v", [nseq, 128, 5])
    a_x = dram_in(nc, "a_x", [64, nchunks, nseq])
    b_x = dram_in(nc, "b_x", [64, nchunks, nseq])
    alog_x = dram_in(nc, "alog_x", [64, nseq])
    dtb_x = dram_in(nc, "dtb_x", [64, nseq])
    cst = {k: dram_in(nc, k, list(v.shape)) for k, v in gdn_consts().items()}
    o_out = dram_out(nc, "o", [nseq, ntok, 128])
    gts = nc.dram_tensor("gts", [NX, 64], F32, kind="Internal").ap()
    gts_db = DB()
    c = Ctx(nc)

    def ldc(name, shape):
        t = c.sb(shape, F32, name + "_c")
        c.dma("sp", t[:], cst[name][:, :], writes=[t])
        return t
    tri = ldc("tri", [64, 64])
    sel = ldc("sel", [64, 128])
    mincl = ldc("mincl", [64, 64])
    negm = ldc("negm", [64, 64])
    mstrict = ldc("mstrict", [64, 64])
    idf = ldc("identf", [128, 128])
    ones = c.sb([128, 128], F32, "ones")
    c.op("dve", lambda: nc.vector.memset(ones[:], 1.0), writes=[ones])
    pb = [c.ps([128, 512], F32, f"pb{i}") for i in range(8)]
    pi = [0]

    def nextp():
        p = pb[pi[0] % 8]
        pi[0] += 1
        return p

    c.push()
    ax = c.sb([64, NX], F32, "ax")
    bx = c.sb([64, NX], F32, "bx")
    c.dma("sp", ax[:], a_x.rearrange("p c s -> p (c s)"), writes=[ax])
    c.dma("sp", bx[:], b_x.rearrange("p c s -> p (c s)"), writes=[bx])
    al = c.sb([64, nseq], F32, "al")
    dtb = c.sb([64, nseq], F32, "dtb")
    c.dma("sp", al[:], alog_x[:, :], writes=[al])
    c.dma("sp", dtb[:], dtb_x[:, :], writes=[dtb])
    c.act(al, al[:], al, al[:], AF.Exp)
    c.ts("dve", al, al[:], al, al[:], -1.0, None, ALU.mult)

    def xv(t):
        return t[:, :].rearrange("p (c s) -> p c s", s=nseq)
    bc8 = lambda t: t[:, :].unsqueeze(1).broadcast_to([64, nchunks, nseq])
    c.tt("dve", ax, xv(ax), ax, xv(ax), dtb, bc8(dtb), ALU.add)
    c.act(ax, ax[:], ax, ax[:], AF.Exp)
    c.act(ax, ax[:], ax, ax[:], AF.Ln, bias=1.0)
    c.tt("dve", ax, xv(ax), ax, xv(ax), al, bc8(al), ALU.mult)
    c.pop()
    beta = c.sb([64, NX], F32, "beta")
    G = c.sb([64, NX], F32, "G")
    eG = c.sb([64, NX], F32, "eG")
    bk = c.sb([64, NX], F32, "bk")
    kts = c.sb([64, NX], F32, "kts")
    eGe = c.sb([128, NX], F32, "eGe")
    c.push()
    gx = c.sb([64, NX], F32, "gx")
    bx2 = c.sb([64, NX], F32, "bx2")
    c.dma("sp", gx[:], a_x.rearrange("p c s -> p (c s)"), writes=[gx])
    c.dma("sp", bx2[:], b_x.rearrange("p c s -> p (c s)"), writes=[bx2])
    al2 = c.sb([64, nseq], F32, "al2")
    dtb2 = c.sb([64, nseq], F32, "dtb2")
    c.dma("sp", al2[:], alog_x[:, :], writes=[al2])
    c.dma("sp", dtb2[:], dtb_x[:, :], writes=[dtb2])
    c.act(al2, al2[:], al2, al2[:], AF.Exp)
    c.ts("dve", al2, al2[:], al2, al2[:], -1.0, None, ALU.mult)
    c.tt("dve", gx, xv(gx), gx, xv(gx), dtb2, bc8(dtb2), ALU.add)
    c.act(gx, gx[:], gx, gx[:], AF.Exp)
    c.act(gx, gx[:], gx, gx[:], AF.Ln, bias=1.0)
    c.tt("dve", gx, xv(gx), gx, xv(gx), al2, bc8(al2), ALU.mult)
    c.act(beta, beta[:], bx2, bx2[:], AF.Sigmoid)
    Ge = c.sb([128, NX], F32, "Ge")
    for n0 in range(0, NX, 512):
        n1 = min(NX, n0 + 512)
        p = nextp()
        c.mm(p, p[0:64, 0:n1 - n0], tri, tri[:, :], gx, gx[:, n0:n1])
        c.cp("act", G, G[:, n0:n1], p, p[0:64, 0:n1 - n0])
        p2 = nextp()
        c.mm(p2, p2[:, 0:n1 - n0], sel, sel[:, :], G, G[:, n0:n1])
        c.cp("dve", Ge, Ge[:, n0:n1], p2, p2[:, 0:n1 - n0])
    c.act(eG, eG[:], G, G[:], AF.Exp)
    c.act(eGe, eGe[:], Ge, Ge[:], AF.Exp)
    c.tt("dve", kts, kts[:], Ge, Ge[0:64, :], G, G[:], ALU.subtract)
    c.act(kts, kts[:], kts, kts[:], AF.Exp)
    c.tt("dve", bk, bk[:], beta, beta[:], eG, eG[:], ALU.mult)
    gtt = [c.sb([128, 64], F32, f"gtt{i}") for i in range(2)]
    for i_, n0 in enumerate(range(0, NX, 128)):
        n1 = min(NX, n0 + 128)
        p = nextp()
        c.tr(p, p[0:n1 - n0, 0:64], G, G[:, n0:n1], idf_v := c.view(idf))
        g_ = gtt[i_ % 2]
        c.cp("act", g_, g_[0:n1 - n0, :], p, p[0:n1 - n0, 0:64])
        c.dma("sp", gts[n0:n1, :], g_[0:n1 - n0, :], reads=[g_], writes=[gts_db])
    c.pop()

    NT = gch * CH
    S = [c.sb([128, 128], F32, f"S{s}") for s in range(nseq)]
    for s in range(nseq):
        c.op("pool", lambda s=s: nc.gpsimd.memset(S[s][:], 0.0), writes=[S[s]])
    NSLOT = 2

    def mkslot(k):
        B = {}
        B["xin"] = [c.sb([128, NT + 4], F32, f"xin{k}_{i}") for i in range(2)]
        B["cwt"] = c.sb([128, 3, 5], F32, f"cwt{k}")
        for nm in ("qT", "kT", "vT", "sq", "rs"):
            B[nm] = c.sb([128, NT], F32, f"{nm}{k}")
        B["ktail"] = c.sb([64, gch, 128], F32, f"ktail{k}")
        B["R"] = c.sb([64, gch, 256], F32, f"R{k}")
        for nm in ("L", "LT", "L2", "LT2", "intraT", "dec", "tmp64"):
            B[nm] = c.sb([64, gch, 64], F32, f"{nm}{k}")
        B["wT"] = c.sb([128, gch, 64], F32, f"wT{k}")
        B["grow"] = c.sb([1, NT], F32, f"grow{k}")
        B["ngrow"] = c.sb([1, NT], F32, f"ngrow{k}")
        B["vnew"] = [c.sb([64, 128], F32, f"vnew{k}_{i}") for i in range(2)]
        B["o2s"] = [c.sb([64, 128], F32, f"o2s{k}_{i}") for i in range(2)]
        B["osb"] = [c.sb([64, gch, 128], F32, f"osb{k}_{i}") for i in range(2)]
        B["gi"] = 0
        B["xi"] = 0
        return B
    slots = [mkslot(k) for k in range(NSLOT)]
    gts_v = gts.rearrange("(c s) j -> s c j", s=nseq)
    groups = [(0, nctx_ch, 0)] + [(nctx_ch + g * gch, gch, 260 + g * NT) for g in range(nlat_groups)]

    def seq_body(s, c0, ncg, col0, SL):
        n = ncg * CH
        xin, cwt, qT, kT, vT, sq, rs = SL['xin'], SL['cwt'], SL['qT'], SL['kT'], SL['vT'], SL['sq'], SL['rs']
        ktail, R, L, LT, L2, LT2 = SL['ktail'], SL['R'], SL['L'], SL['LT'], SL['L2'], SL['LT2']
        intraT, dec, tmp64, wT, grow, ngrow = SL['intraT'], SL['dec'], SL['tmp64'], SL['wT'], SL['grow'], SL['ngrow']
        vnew, o2s, osb = SL['vnew'], SL['o2s'], SL['osb']
        qki = s // 4 * 2 + (s % 2) if nseq > 1 else 0
        c.dma("pool", cwt[:, 0, :], cw_qk[qki, 0, :, :], writes=[cwt])
        c.dma("pool", cwt[:, 1, :], cw_qk[qki, 1, :, :], writes=[cwt])
        c.dma("pool", cwt[:, 2, :], cw_v[s, :, :], writes=[cwt])
        for wi, (src_ap, dst) in enumerate(((qk_pre[qki, 0], qT), (qk_pre[qki, 1], kT), (v_pre[s], vT))):
            xi_ = xin[SL['xi'] % 2]
            SL['xi'] += 1
            c.dma("sp", xi_[:, 0:n + 4], src_ap[:, col0:col0 + n + 4], writes=[xi_])
            c.ts("dve", dst, dst[:, 0:n], xi_, xi_[:, 0:n], cwt[:, wi, 0:1], None, ALU.mult, extra_reads=[cwt])
            for j in range(1, 5):
                c.stt("dve", dst, dst[:, 0:n], xi_, xi_[:, j:j + n], cwt[:, wi, j:j + 1], dst, dst[:, 0:n],
                      ALU.mult, ALU.add, extra_reads=[cwt])
            c.act(dst, dst[:, 0:n], dst, dst[:, 0:n], AF.Silu)
            yield
            if wi < 2:
                c.tt("pool", sq, sq[:, 0:n], dst, dst[:, 0:n], dst, dst[:, 0:n], ALU.mult)
                for n0 in range(0, n, 512):
                    n1 = min(n, n0 + 512)
                    p = nextp()
                    c.mm(p, p[:, 0:n1 - n0], ones, ones[:, :], sq, sq[:, n0:n1])
                    c.ts("dve", rs, rs[:, n0:n1], p, p[:, 0:n1 - n0], 1e-6, None, ALU.add)
                c.op("act", lambda: nc.scalar.sqrt(rs[:, 0:n], rs[:, 0:n]), reads=[rs], writes=[rs])
                c.op("dve", lambda: nc.vector.reciprocal(rs[:, 0:n], rs[:, 0:n]), reads=[rs], writes=[rs])
                if wi == 0:
                    c.stt("dve", dst, dst[:, 0:n], dst, dst[:, 0:n], 128.0 ** -0.5, rs, rs[:, 0:n], ALU.mult, ALU.mult)
                else:
                    c.tt("dve", dst, dst[:, 0:n], dst, dst[:, 0:n], rs, rs[:, 0:n], ALU.mult)
        def X(t):
            return t[:, :].rearrange("p (c s) -> p c s", s=nseq)[:, c0:c0 + ncg, s]

        def Xb(t, w):
            return X(t).unsqueeze(2).broadcast_to([64, ncg, w])
        for cc0 in range(0, ncg, 4):
            pk = nextp()
            pv = nextp()
            for u in range(4):
                cc = cc0 + u
                c.tr(pk, pk[0:64, u * 128:(u + 1) * 128], kT, kT[:, cc * 64:(cc + 1) * 64], idf)
                c.tr(pv, pv[0:64, u * 128:(u + 1) * 128], vT, vT[:, cc * 64:(cc + 1) * 64], idf)
            pk3 = pk[0:64, :].rearrange("p (a b) -> p a b", b=128)
            pv3 = pv[0:64, :].rearrange("p (a b) -> p a b", b=128)
            sl = slice(cc0, cc0 + 4)
            c.tt("dve", R, R[:, sl, 0:128], pv, pv3, beta, Xb(beta, 128)[:, sl, :], ALU.mult)
            c.tt("dve", R, R[:, sl, 128:256], pk, pk3, bk, Xb(bk, 128)[:, sl, :], ALU.mult)
            c.tt("dve", ktail, ktail[:, sl, :], pk, pk3, kts, Xb(kts, 128)[:, sl, :], ALU.mult)
            yield
        c.dma("pool", grow[0:1, 0:n].rearrange("p (c j) -> p c j", j=64), gts_v[s:s + 1, c0:c0 + ncg, :],
              reads=[gts_db], writes=[grow])
        c.ts("dve", ngrow, ngrow[0:1, 0:n], grow, grow[0:1, 0:n], -1.0, None, ALU.mult)
        bm = lambda t: t[:, :].unsqueeze(1).broadcast_to([64, 8, 64])
        for cc0 in range(0, ncg, 8):
            nb = min(8, ncg - cc0)
            sl = slice(cc0, cc0 + nb)
            pd = nextp()
            pkk = nextp()
            pqk = nextp()
            for u in range(nb):
                cc = cc0 + u
                cs_ = slice(cc * 64, (cc + 1) * 64)
                us_ = slice(u * 64, (u + 1) * 64)
                c.mm(pd, pd[0:64, us_], grow, grow[0:1, cs_], ones, ones[0:1, 0:64], start=True, stop=False)
                c.mm(pd, pd[0:64, us_], ones, ones[0:1, 0:64], ngrow, ngrow[0:1, cs_], start=False, stop=True)
                c.mm(pkk, pkk[0:64, us_], kT, kT[:, cs_], kT, kT[:, cs_])
                c.mm(pqk, pqk[0:64, us_], qT, qT[:, cs_], kT, kT[:, cs_])
            v3 = lambda p: p[0:64, 0:nb * 64].rearrange("p (a b) -> p a b", b=64)
            mb = lambda t: t[:, :].unsqueeze(1).broadcast_to([64, nb, 64])
            c.tt("dve", dec, dec[:, sl, :], pd, v3(pd), mincl, mb(mincl), ALU.mult)
            c.tt("pool", dec, dec[:, sl, :], dec, dec[:, sl, :], negm, mb(negm), ALU.add)
            c.act(dec, dec[:, sl, :], dec, dec[:, sl, :], AF.Exp)
            c.tt("dve", L, L[:, sl, :], pkk, v3(pkk), dec, dec[:, sl, :], ALU.mult)
            c.tt("pool", L, L[:, sl, :], L, L[:, sl, :], mstrict, mb(mstrict), ALU.mult)
            c.tt("pool", L, L[:, sl, :], L, L[:, sl, :], beta, Xb(beta, 64)[:, sl, :], ALU.mult)
            c.tt("dve", tmp64, tmp64[:, sl, :], pqk, v3(pqk), dec, dec[:, sl, :], ALU.mult)
            pl_ = nextp()
            pi_ = nextp()
            for u in range(nb):
                cc = cc0 + u
                us_ = slice(u * 64, (u + 1) * 64)
                c.tr(pl_, pl_[0:64, us_], L, L[:, cc, :], idf_v)
                c.tr(pi_, pi_[0:64, us_], tmp64, tmp64[:, cc, :], idf_v)
            c.cp("act", LT, LT[:, sl, :], pl_, v3(pl_))
            c.cp("act", intraT, intraT[:, sl, :], pi_, v3(pi_))
            yield
        A, AT, B, BT = L, LT, L2, LT2
        for lev in range(6):
            for cc0 in range(0, ncg, 2):
                p = nextp()
                for u in range(2):
                    cc = cc0 + u
                    c.mm(p, p[0:64, u * 256:(u + 1) * 256], AT, AT[:, cc, :], R, R[:, cc, :])
                p3 = p[0:64, :].rearrange("p (a b) -> p a b", b=256)
                sl = slice(cc0, cc0 + 2)
                c.tt(("dve", "pool")[0], R, R[:, sl, :], R, R[:, sl, :], p, p3, ALU.subtract if lev == 0 else ALU.add)
            yield
            if lev < 5:
                for cc0 in range(0, ncg, 8):
                    nb = min(8, ncg - cc0)
                    sl = slice(cc0, cc0 + nb)
                    pa = nextp()
                    pt = nextp()
                    for u in range(nb):
                        cc = cc0 + u
                        us_ = slice(u * 64, (u + 1) * 64)
                        c.mm(pa, pa[0:64, us_], AT, AT[:, cc, :], A, A[:, cc, :])
                        c.mm(pt, pt[0:64, us_], A, A[:, cc, :], AT, AT[:, cc, :])
                    v3 = lambda p: p[0:64, 0:nb * 64].rearrange("p (a b) -> p a b", b=64)
                    c.cp("act", B, B[:, sl, :], pa, v3(pa))
                    c.cp("act", BT, BT[:, sl, :], pt, v3(pt))
                yield
                A, AT, B, BT = B, BT, A, AT
        for cc0 in range(0, ncg, 8):
            nb = min(8, ncg - cc0)
            p = nextp()
            for u in range(nb):
                cc = cc0 + u
                c.tr(p, p[:, u * 64:(u + 1) * 64], R, R[:, cc, 128:256], idf_v)
            c.cp("act", wT, wT[:, cc0:cc0 + nb, :], p, p[:, 0:nb * 64].rearrange("p (a b) -> p a b", b=64))
        ob_ = osb[SL['gi'] % 2]
        SL['gi'] += 1
        St = S[s]
        for cc in range(ncg):
            col = (c0 + cc) * nseq + s
            cs_ = slice(cc * 64, (cc + 1) * 64)
            pv_ = nextp()
            c.mm(pv_, pv_[0:64, 0:128], wT, wT[:, cc, :], St, St[:, :])
            vn = vnew[cc % 2]
            c.tt("dve", vn, vn[:], R, R[:, cc, 0:128], pv_, pv_[0:64, 0:128], ALU.subtract)
            po1 = nextp()
            c.mm(po1, po1[0:64, 0:128], qT, qT[:, cs_], St, St[:, :])
            po2 = nextp()
            c.mm(po2, po2[0:64, 0:128], intraT, intraT[:, cc, :], vn, vn[:])
            ps_ = nextp()
            c.mm(ps_, ps_[:, 0:128], ktail, ktail[:, cc, :], vn, vn[:])
            o2 = o2s[cc % 2]
            c.cp("act", o2, o2[:], po2, po2[0:64, 0:128])
            c.stt("dve", ob_, ob_[:, cc, :], po1, po1[0:64, 0:128], eG[:, col:col + 1], o2, o2[:], ALU.mult, ALU.add,
                  extra_reads=[eG])
            c.stt("dve", St, St[:, :], St, St[:, :], eGe[:, col:col + 1], ps_, ps_[:, 0:128], ALU.mult, ALU.add,
                  extra_reads=[eGe])
            yield
        c.dma("sp", o_out[s, c0 * 64:(c0 + ncg) * 64, :].rearrange("(c p) e -> p c e", p=64), ob_[:, 0:ncg, :], reads=[ob_])


    for (c0, ncg, col0) in groups:
        for s0 in range(0, nseq, NSLOT):
            gens = [seq_body(s0 + i, c0, ncg, col0, slots[i]) for i in range(NSLOT) if s0 + i < nseq]
            while gens:
                for g_ in list(gens):
                    try:
                        next(g_)
                    except StopIteration:
                        gens.remove(g_)
    c.finish()
    return nc


ODD_IN = 6208


def run_l1proj(inp, mod, x_lat, x_ctx):
    ident = np.eye(128, dtype=np.float32)
    maps = []
    for k in range(NCORES):
        b, r = k // 4, k % 4
        xs = np.zeros((L0_T, D), np.float32)
        xs[:4096] = x_lat[b, r * 4096:(r + 1) * 4096]
        xs[4096:4160] = x_ctx[b, r * 64:(r + 1) * 64]
        m = np.stack([mod[1, b].reshape(6, D)[0:2], mod[1, 2].reshape(6, D)[0:2]])
        maps.append({"x": xs, "g": inp["norm1_g"][1], "mod": np.ascontiguousarray(m), "w": inp["od_w_in"][0], "ident": ident})
    res = run(build_l0proj(N=ODD_IN, rope=False), maps)
    p_lat = np.zeros((2, 16384, ODD_IN), np.float32)
    p_ctx = np.zeros((2, 256, ODD_IN), np.float32)
    for k in range(NCORES):
        b, r = k // 4, k % 4
        p_lat[b, r * 4096:(r + 1) * 4096] = res[k]["p"][:4096]
        p_ctx[b, r * 64:(r + 1) * 64] = res[k]["p"][4096:4160]
    return p_lat, p_ctx


def run_post0(inp, mod, o_lat, o_ctx, f_lat, f_ctx):
    ident = np.eye(128, dtype=np.float32)
    maps = []
    for k in range(NCORES):
        b, r = k // 4, k % 4

        def sh(lat, ctx, w):
            a = np.zeros((L0_T, w), np.float32)
            a[:4096] = lat[b, r * 4096:(r + 1) * 4096].reshape(4096, w)
            a[4096:4160] = ctx[b, r * 64:(r + 1) * 64].reshape(64, w)
            return a
        m = np.stack([mod[0, b].reshape(6, D), mod[0, 2].reshape(6, D)])
        maps.append({"x": sh(inp["x"], inp["ctx"], D), "o": sh(o_lat, o_ctx, 768), "fo": sh(f_lat, f_ctx, 256),
                     "sg": inp["ev_subln_g"][0], "g2": inp["norm2_g"][0], "mod": np.ascontiguousarray(m),
                     "wout": inp["ev_w_out"][0], "wr": inp["moe_w_router"][0], "br": inp["moe_b_router"][0],
                     "w1": inp["moe_w1"][0], "b1": inp["moe_b1"][0], "w2": inp["moe_w2"][0], "b2": inp["moe_b2"][0],
                     "ident": ident})
    res = run(build_post(0, 33, 32, False, [11, 11, 11], lam_init=lam_init_of(0)), maps)
    x_lat = np.zeros((2, 16384, D), np.float32)
    x_ctx = np.zeros((2, 256, D), np.float32)
    for k in range(NCORES):
        b, r = k // 4, k % 4
        x_lat[b, r * 4096:(r + 1) * 4096] = res[k]["xo"][:4096]
        x_ctx[b, r * 64:(r + 1) * 64] = res[k]["xo"][4096:4160]
    return x_lat, x_ctx


def run_gdn(inp, p_lat, p_ctx):
    consts = gdn_consts()
    conv_w = inp["od_conv_w"][0]
    TP = 2 + 256 + 2 + 2 + 16384 + 2
    maps = []
    for k in range(NCORES):
        b, r = k // 4, k % 4

        def seq(cols, d):
            c_ = p_ctx[b][:, cols]
            l_ = p_lat[b][:, cols]
            if d == 1:
                c_ = c_[::-1]
                l_ = l_[::-1]
            out = np.zeros((cols.stop - cols.start, TP), np.float32)
            out[:, 2:258] = c_.T
            out[:, 262:262 + 16384] = l_.T
            return out

        def taps(cols, d):
            w = conv_w[:, cols].T
            return np.ascontiguousarray(w[:, ::-1] if d == 1 else w)
        qk_pre = np.zeros((4, 2, 128, TP), np.float32)
        cw_qk = np.zeros((4, 2, 128, 5), np.float32)
        for khl in range(2):
            kh = 2 * r + khl
            for d in range(2):
                for j, base in enumerate((0, 1024)):
                    cols = slice(base + kh * 128, base + (kh + 1) * 128)
                    qk_pre[khl * 2 + d, j] = seq(cols, d)
                    cw_qk[khl * 2 + d, j] = taps(cols, d)
        v_pre = np.zeros((8, 128, TP), np.float32)
        cw_v = np.zeros((8, 128, 5), np.float32)
        a_x = np.zeros((64, 260, 8), np.float32)
        b_x = np.zeros((64, 260, 8), np.float32)
        alog_x = np.zeros((64, 8), np.float32)
        dtb_x = np.zeros((64, 8), np.float32)
        for hvl in range(4):
            hv = 4 * r + hvl
            for d in range(2):
                s = hvl * 2 + d
                cols = slice(2048 + hv * 128, 2048 + (hv + 1) * 128)
                v_pre[s] = seq(cols, d)
                cw_v[s] = taps(cols, d)
                for arr, ab in ((a_x, 0), (b_x, 1)):
                    col = 6144 + ab * 32 + d * 16 + hv
                    c_ = p_ctx[b][:, col]
                    l_ = p_lat[b][:, col]
                    if d == 1:
                        c_ = c_[::-1]
                        l_ = l_[::-1]
                    arr[:, :, s] = np.concatenate([c_, l_]).reshape(260, 64).T
                alog_x[:, s] = inp["od_a_log"][0, d, hv]
                dtb_x[:, s] = inp["od_dt_bias"][0, d, hv]
        m = {"qk_pre": qk_pre, "v_pre": v_pre, "cw_qk": cw_qk, "cw_v": cw_v, "a_x": a_x, "b_x": b_x,
             "alog_x": alog_x, "dtb_x": dtb_x}
        m.update(consts)
        maps.append(m)
    res = run(build_gdn(), maps)
    o_f = np.zeros((2, 16384, 16, 128), np.float32)
    o_b = np.zeros((2, 16384, 16, 128), np.float32)
    for k in range(NCORES):
        b, r = k // 4, k % 4
        o = res[k]["o"]
        for hvl in range(4):
            hv = 4 * r + hvl
            o_f[b, :, hv] = o[hvl * 2, 256:]
            o_b[b, :, hv] = o[hvl * 2 + 1, 256:][::-1]
    return o_f.reshape(2, 16384, 2048), o_b.reshape(2, 16384, 2048)


def run_post1(inp, mod, x_lat, o_f, o_b, p_lat):
    ident = np.eye(128, dtype=np.float32)
    maps = []
    for k in range(NCORES):
        b, r = k // 4, k % 4
        rs = slice(r * 4096, (r + 1) * 4096)
        m = np.stack([mod[1, b].reshape(6, D), mod[1, 2].reshape(6, D)])
        maps.append({"x": np.ascontiguousarray(x_lat[b, rs]), "of": np.ascontiguousarray(o_f[b, rs]),
                     "ob": np.ascontiguousarray(o_b[b, rs]), "z": np.ascontiguousarray(p_lat[b, rs, 4096:6144]),
                     "sg": inp["od_norm_g"][0], "g2": inp["norm2_g"][1], "mod": np.ascontiguousarray(m),
                     "wout": inp["od_w_out"][0], "wr": inp["moe_w_router"][1], "br": inp["moe_b_router"][1],
                     "w1": inp["moe_w1"][1], "b1": inp["moe_b1"][1], "w2": inp["moe_w2"][1], "b2": inp["moe_b2"][1],
                     "ident": ident, "fg": inp["final_g"]})
    res = run(build_post(1, 32, 32, True, [11, 11, 10]), maps)
    out = np.zeros((2, 16384, D), np.float32)
    for k in range(NCORES):
        b, r = k // 4, k % 4
        out[b, r * 4096:(r + 1) * 4096] = res[k]["xo"]
    return out


def kernel(**inputs):
    inp = {k: np.ascontiguousarray(np.asarray(v, dtype=np.float32)) for k, v in inputs.items()}
    mod = run_mod(inp)
    p_lat, p_ctx = run_l0proj(inp, mod)
    o_lat, o_ctx = run_attn(inp, p_lat, p_ctx)
    f_lat, f_ctx = run_fourier(p_lat, p_ctx)
    del p_lat, p_ctx
    x_lat, x_ctx = run_post0(inp, mod, o_lat, o_ctx, f_lat, f_ctx)
    del o_lat, o_ctx, f_lat, f_ctx
    p1_lat, p1_ctx = run_l1proj(inp, mod, x_lat, x_ctx)
    o_f, o_b = run_gdn(inp, p1_lat, p1_ctx)
    out = run_post1(inp, mod, x_lat, o_f, o_b, p1_lat)
    return out
```

```python
import contextlib
import math
import numpy as np
import concourse.bass as bass
import concourse.mybir as mybir
from concourse.bass_utils import run_bass_kernel_spmd

F32 = mybir.dt.float32
BF16 = mybir.dt.bfloat16
I32 = mybir.dt.int32
AF = mybir.ActivationFunctionType
ALU = mybir.AluOpType
AX = mybir.AxisListType

NCORES = 8
D = 1024


class Buf:
    __slots__ = ("t", "last_w", "reads", "name")

    def __init__(self, t, name=""):
        self.t = t
        self.last_w = None
        self.reads = {}
        self.name = name

    def __getitem__(self, k):
        return self.t[k]


class Ctx:
    NDMA = 12

    def __init__(self, nc):
        self.nc = nc
        self.es = contextlib.ExitStack()
        self.eng = {"pe": nc.tensor, "act": nc.scalar, "dve": nc.vector, "pool": nc.gpsimd, "sp": nc.sync}
        self.sem = {}
        self.cnt = {}
        self.waited = {}
        for e in self.eng:
            self.sem[e] = self.es.enter_context(nc.semaphore("s_" + e))
            self.cnt[e] = 0
            self.waited[e] = {}
        self.dsem = {}
        self.dn = {}
        for q in ("sp", "pool", "act"):
            self.dsem[q] = [self.es.enter_context(nc.semaphore(f"d_{q}{i}")) for i in range(self.NDMA)]
            self.dn[q] = 0
        self.nbuf = 0

    def sb(self, shape, dt, name=None):
        self.nbuf += 1
        name = name or f"sb{self.nbuf}"
        return Buf(self.es.enter_context(self.nc.sbuf_tensor(name, list(shape), dt)), name)

    def ps(self, shape, dt, name=None):
        self.nbuf += 1
        name = name or f"ps{self.nbuf}"
        return Buf(self.es.enter_context(self.nc.psum_tensor(name, list(shape), dt)), name)

    def view(self, buf):
        return Buf(buf.t, buf.name + "_v")

    def push(self):
        self.stack = getattr(self, "stack", [])
        self.stack.append(self.es)
        self.es = contextlib.ExitStack()

    def pop(self):
        self.barrier()
        self.es.close()
        self.es = self.stack.pop()

    def barrier(self):
        evs = []
        for f in ("pe", "act", "dve", "pool"):
            if self.cnt[f]:
                evs.append((f, self.sem[f], self.cnt[f]))
        for q in self.dsem:
            n = self.dn[q]
            for s in range(min(n, self.NDMA)):
                last = ((n - 1 - s) // self.NDMA) * self.NDMA + s
                evs.append((("d", q, s), self.dsem[q][s], 16 * (last // self.NDMA + 1)))
        for e in self.eng:
            for ev in evs:
                if ev[0] != e:
                    self._wait(e, ev)

    def _wait(self, e, ev):
        key, sem, val = ev
        if key == e and e == "pe":
            return
        w = self.waited[e]
        if w.get(key, 0) >= val:
            return
        self.eng[e].wait_ge(sem, val)
        w[key] = val

    def _deps(self, e, reads, writes):
        for b in reads:
            if b.last_w is not None:
                self._wait(e, b.last_w)
        for b in writes:
            if b.last_w is not None:
                self._wait(e, b.last_w)
            for ev in b.reads.values():
                self._wait(e, ev)

    def _mark(self, ev, reads, writes):
        for b in writes:
            b.last_w = ev
            b.reads = {}
        for b in reads:
            if b not in writes:
                b.reads[ev[0]] = ev

    def op(self, e, ins_fn, reads=(), writes=()):
        self._deps(e, reads, writes)
        ins = ins_fn()
        self.cnt[e] += 1
        ins.then_inc(self.sem[e], 1)
        self._mark((e, self.sem[e], self.cnt[e]), reads, writes)

    def dma(self, q, out, in_, reads=(), writes=(), **kw):
        self._deps(q, reads, writes)
        j = self.dn[q]
        P = self.NDMA
        sem = self.dsem[q][j % P]
        key = ("d", q, j % P)
        if j >= P:
            self._wait(q, (key, sem, 16 * (j // P)))
        ins = self.eng[q].dma_start(out=out, in_=in_, **kw)
        ins.then_inc(sem, 16)
        self.dn[q] = j + 1
        self._mark((key, sem, 16 * (j // P + 1)), reads, writes)

    def finish(self):
        self.barrier()
        self.es.close()

    F32R = False

    def mm(self, out, out_ap, lhsT, lhsT_ap, rhs, rhs_ap, start=True, stop=True):
        if self.F32R and lhsT_ap.dtype == F32 and rhs_ap.dtype == F32:
            lhsT_ap = lhsT_ap.bitcast(mybir.dt.float32r)
            rhs_ap = rhs_ap.bitcast(mybir.dt.float32r)
        self.op("pe", lambda: self.nc.tensor.matmul(out_ap, lhsT_ap, rhs_ap, start=start, stop=stop),
                reads=[lhsT, rhs], writes=[out])

    def tr(self, out, out_ap, in_, in_ap, ident):
        k = in_ap.shape[0]
        self.op("pe", lambda: self.nc.tensor.transpose(out_ap, in_ap, ident.t[0:k, 0:k]), reads=[in_, ident], writes=[out])

    def act(self, out, out_ap, in_, in_ap, func, bias=None, scale=None, accum=None, eng="act", extra_reads=()):
        kw = {}
        rd = [in_] + list(extra_reads)
        wr = [out]
        if bias is not None:
            kw["bias"] = bias
        if scale is not None:
            kw["scale"] = scale
        if accum is not None:
            kw["accum_out"] = accum[1]
            wr.append(accum[0])
        self.op("act", lambda: self.nc.scalar.activation(out_ap, in_ap, func, **kw), reads=rd, writes=wr)

    def cp(self, e, out, out_ap, in_, in_ap):
        if e == "act":
            self.op("act", lambda: self.nc.scalar.copy(out_ap, in_ap), reads=[in_], writes=[out])
        else:
            self.op(e, lambda: self.eng[e].tensor_copy(out_ap, in_ap), reads=[in_], writes=[out])

    def tt(self, e, out, out_ap, a, a_ap, b, b_ap, op):
        self.op(e, lambda: self.eng[e].tensor_tensor(out_ap, a_ap, b_ap, op), reads=[a, b], writes=[out])

    def ts(self, e, out, out_ap, a, a_ap, s1, s2, op0, op1=None, extra_reads=()):
        if op1 is None:
            f = lambda: self.eng[e].tensor_scalar(out_ap, a_ap, s1, None, op0)
        else:
            f = lambda: self.eng[e].tensor_scalar(out_ap, a_ap, s1, s2, op0, op1)
        self.op(e, f, reads=[a] + list(extra_reads), writes=[out])

    def stt(self, e, out, out_ap, a, a_ap, scalar, b, b_ap, op0, op1, extra_reads=()):
        self.op(e, lambda: self.eng[e].scalar_tensor_tensor(out_ap, a_ap, scalar, b_ap, op0, op1),
                reads=[a, b] + list(extra_reads), writes=[out])


def new_nc():
    return bass.Bass("TRN2", target_bir_lowering=False)


def dram_in(nc, name, shape, dt=F32):
    return nc.dram_tensor(name, list(shape), dt, kind="ExternalInput").ap()


def dram_out(nc, name, shape, dt=F32):
    return nc.dram_tensor(name, list(shape), dt, kind="ExternalOutput").ap()


def run(nc, in_maps):
    res = run_bass_kernel_spmd(nc, in_maps, core_ids=list(range(NCORES)))
    return res.results


def load_weight_bf16(c, w_ap, K, N, name, nchunk_cols=None):
    kc_n = K // 128
    wb = c.sb([128, kc_n, N], BF16, name)
    cw = min(N, 2048)
    st = [c.sb([128, cw], F32, f"{name}_st{i}") for i in range(2)]
    i = 0
    engs = ["act", "dve", "pool"]
    for kc in range(kc_n):
        for n0 in range(0, N, cw):
            n1 = min(N, n0 + cw)
            s = st[i % 2]
            c.dma("sp", s[:, 0:n1 - n0], w_ap[kc * 128:(kc + 1) * 128, n0:n1], writes=[s])
            c.cp(engs[i % 3], wb, wb[:, kc, n0:n1], s, s[:, 0:n1 - n0])
            i += 1
    return wb


def load_ident(c, ident_ap):
    idf = c.sb([128, 128], F32, "identf")
    idb = c.sb([128, 128], BF16, "identb")
    c.dma("sp", idf[:], ident_ap[:, :], writes=[idf])
    c.cp("dve", idb, idb[:], idf, idf[:])
    return idf, idb


def make_mod_tiles(c, g_ap, mod_ap, seg, i_shift, i_scale, name):
    gb = c.sb([128, D], F32, name + "_G")
    sh = c.sb([128, D], F32, name + "_S")
    tmp = c.sb([128, D], F32, name + "_t")
    c.dma("sp", tmp[:], g_ap.partition_broadcast(128), writes=[tmp])
    c.dma("sp", gb[:], mod_ap[seg, i_scale:i_scale + 1, :].partition_broadcast(128), writes=[gb])
    c.dma("sp", sh[:], mod_ap[seg, i_shift:i_shift + 1, :].partition_broadcast(128), writes=[sh])
    c.stt("dve", gb, gb[:], gb, gb[:], 1.0, tmp, tmp[:], ALU.add, ALU.mult)
    return gb, sh


def build_mod():
    nc = new_nc()
    cT = dram_in(nc, "cT", [D, 3])
    wm = dram_in(nc, "wm", [2, D, 768])
    bm = dram_in(nc, "bm", [2, 768])
    out = dram_out(nc, "mod", [2, 3, 768])
    c = Ctx(nc)
    sT = c.sb([128, 8, 3], F32, "sT")
    c.dma("sp", sT[:], cT.rearrange("(kc p) s -> p kc s", p=128), writes=[sT])
    c.act(sT, sT[:], sT, sT[:], AF.Silu)
    wt = [c.sb([128, 768], F32, f"wt{i}") for i in range(3)]
    pt = [c.ps([128, 512], F32, f"pm{i}") for i in range(4)]
    bt = c.sb([3, 2, 768], F32, "bt")
    for l in range(2):
        c.dma("sp", bt[:, l, :], bm[l:l + 1, :].partition_broadcast(3), writes=[bt])
    ot = c.sb([3, 2, 768], F32, "ot")
    i = 0
    for l in range(2):
        pa, pb = pt[2 * l], pt[2 * l + 1]
        for kc in range(8):
            w = wt[i % 3]
            i += 1
            c.dma("sp", w[:], wm[l, kc * 128:(kc + 1) * 128, :], writes=[w])
            c.mm(pa, pa[0:3, 0:512], sT, sT[:, kc, :], w, w[:, 0:512], start=(kc == 0), stop=(kc == 7))
            c.mm(pb, pb[0:3, 0:256], sT, sT[:, kc, :], w, w[:, 512:768], start=(kc == 0), stop=(kc == 7))
        c.tt("dve", ot, ot[:, l, 0:512], pa, pa[0:3, 0:512], bt, bt[:, l, 0:512], ALU.add)
        c.tt("dve", ot, ot[:, l, 512:768], pb, pb[0:3, 0:256], bt, bt[:, l, 512:768], ALU.add)
    c.dma("sp", out.rearrange("l s n -> s l n"), ot[:], reads=[ot])
    c.finish()
    return nc


def run_mod(inp):
    cT = np.ascontiguousarray(np.concatenate([inp["c"], inp["c_ctx"][None]], 0).T)
    maps = []
    for k in range(NCORES):
        maps.append({"cT": cT,
                     "wm": np.ascontiguousarray(inp["w_mod"][:, :, 768 * k:768 * (k + 1)]),
                     "bm": np.ascontiguousarray(inp["b_mod"][:, 768 * k:768 * (k + 1)])})
    res = run(build_mod(), maps)
    return np.concatenate([r["mod"] for r in res], axis=2)


class NormT:
    def __init__(self, c, idb, eps=1e-6):
        self.c = c
        self.idb = idb
        self.eps = eps
        self.xt = [c.sb([128, D], F32, f"nx{i}") for i in range(2)]
        self.sq = c.sb([128, D], F32, "nsq")
        self.ss = [c.sb([128, 1], F32, f"nss{i}") for i in range(2)]
        self.tmp = c.sb([128, D], F32, "ntmp")
        self.hb = [c.sb([128, D], BF16, f"nhb{i}") for i in range(2)]
        self.pT = [c.ps([128, 8, 128], BF16, f"npT{i}") for i in range(2)]
        self.hT = [c.sb([128, 8, 128], BF16, f"nhT{i}") for i in range(2)]
        self.i = 0

    def run(self, x_ap, G, Sh, xt=None, hf=None):
        c = self.c
        i = self.i
        self.i += 1
        if xt is None:
            xt = self.xt[i % 2]
            c.dma("sp", xt[:], x_ap, writes=[xt])
        ss = self.ss[i % 2]
        c.act(self.sq, self.sq[:], xt, xt[:], AF.Square, accum=(ss, ss[:, 0:1]))
        c.ts("dve", ss, ss[:], ss, ss[:], 1.0 / D, self.eps, ALU.mult, ALU.add)
        c.op("act", lambda: c.nc.scalar.sqrt(ss[:], ss[:]), reads=[ss], writes=[ss])
        c.op("dve", lambda: c.nc.vector.reciprocal(ss[:], ss[:]), reads=[ss], writes=[ss])
        c.stt("dve", self.tmp, self.tmp[:], xt, xt[:], ss[:, 0:1], G, G[:], ALU.mult, ALU.mult, extra_reads=[ss])
        hb = self.hb[i % 2]
        if hf is None:
            c.tt("pool", hb, hb[:], self.tmp, self.tmp[:], Sh, Sh[:], ALU.add)
        else:
            c.tt("pool", hf, hf[:], self.tmp, self.tmp[:], Sh, Sh[:], ALU.add)
            c.cp("pool", hb, hb[:], hf, hf[:])
        pT = self.pT[i % 2]
        for kc in range(8):
            c.tr(pT, pT[:, kc, :], hb, hb[:, kc * 128:(kc + 1) * 128], self.idb)
        hT = self.hT[i % 2]
        c.cp("act", hT, hT[:], pT, pT[:])
        return hT


L0_T = 4224
EVEN_IN = 2560


def build_l0proj(N=EVEN_IN, rope=True):
    nc = new_nc()
    x = dram_in(nc, "x", [L0_T, D])
    g = dram_in(nc, "g", [D])
    mod = dram_in(nc, "mod", [2, 2, D])
    w = dram_in(nc, "w", [D, N])
    if rope:
        cs = dram_in(nc, "cs", [L0_T, 2, 32])
    ident = dram_in(nc, "ident", [128, 128])
    out = dram_out(nc, "p", [L0_T, N])
    c = Ctx(nc)
    idf, idb = load_ident(c, ident)
    wb = load_weight_bf16(c, w, D, N, "wb")
    mods = []
    for s_ in range(2):
        gb = bcast_tile(c, mod[s_, 1:2, :], f"mG{s_}")
        gg = bcast_tile(c, g, f"mg{s_}") if s_ == 0 else mods[0][2]
        sh = bcast_tile(c, mod[s_, 0:1, :], f"mS{s_}")
        c.stt("dve", gb, gb[:], gb, gb[:], 1.0, gg, gg[:], ALU.add, ALU.mult)
        mods.append((gb, sh, gg))
    nt = NormT(c, idb)
    po = [c.ps([128, 512], F32, f"po{i}") for i in range(4)]
    ot = [c.sb([128, N], F32, f"ot{i}") for i in range(2 if N <= 4096 else 1)]
    if rope:
        cst = [c.sb([128, 2, 32], F32, f"cs{i}") for i in range(2)]
        r1 = c.sb([128, 24, 32], F32, "r1")
        r2 = c.sb([128, 24, 32], F32, "r2")
        r3 = c.sb([128, 24, 32], F32, "r3")
        r4 = c.sb([128, 24, 32], F32, "r4")
    nt_tiles = L0_T // 128
    j = 0
    for t in range(nt_tiles):
        G, Sh, _ = mods[0] if t < 32 else mods[1]
        hT = nt.run(x[t * 128:(t + 1) * 128, :], G, Sh)
        o = ot[t % len(ot)]
        if rope:
            cs_t = cst[t % 2]
            c.dma("pool", cs_t[:], cs[t * 128:(t + 1) * 128, :, :], writes=[cs_t])
        for n, n0 in enumerate(range(0, N, 512)):
            n1 = min(N, n0 + 512)
            p = po[j % 4]
            j += 1
            for kc in range(8):
                c.mm(p, p[:, 0:n1 - n0], hT, hT[:, kc, :], wb, wb[:, kc, n0:n1], start=(kc == 0), stop=(kc == 7))
            c.cp("act" if n % 2 == 0 else "dve", o, o[:, n0:n1], p, p[:, 0:n1 - n0])
        if not rope:
            c.dma("sp", out[t * 128:(t + 1) * 128, :], o[:], reads=[o])
            continue
        qk = o[:, 256:1792].rearrange("p (a h e) -> p a h e", a=24, h=2)
        x1 = qk[:, :, 0, :]
        x2 = qk[:, :, 1, :]
        cb = cs_t[:, 0:1, :].broadcast_to([128, 24, 32])
        sb_ = cs_t[:, 1:2, :].broadcast_to([128, 24, 32])
        c.tt("dve", r1, r1[:], o, x1, cs_t, cb, ALU.mult)
        c.tt("pool", r2, r2[:], o, x2, cs_t, sb_, ALU.mult)
        c.tt("dve", r3, r3[:], o, x2, cs_t, cb, ALU.mult)
        c.tt("pool", r4, r4[:], o, x1, cs_t, sb_, ALU.mult)
        c.tt("dve", o, x1, r1, r1[:], r2, r2[:], ALU.subtract)
        c.tt("pool", o, x2, r3, r3[:], r4, r4[:], ALU.add)
        c.dma("sp", out[t * 128:(t + 1) * 128, :], o[:], reads=[o])
    c.finish()
    return nc


def rope_tables():
    t = np.arange(16384)
    row = (t // 64).astype(np.float32)
    col = (t % 64).astype(np.float32)
    inv = (np.float32(10000.0) ** (-np.arange(0, 32, 2, dtype=np.float32) / np.float32(32))).astype(np.float32)
    ang = np.concatenate([row[:, None] * inv, col[:, None] * inv], axis=-1).astype(np.float32)
    return np.cos(ang).astype(np.float32), np.sin(ang).astype(np.float32)


def run_l0proj(inp, mod):
    cos, sin = rope_tables()
    ident = np.eye(128, dtype=np.float32)
    maps = []
    for k in range(NCORES):
        b, r = k // 4, k % 4
        xs = np.zeros((L0_T, D), np.float32)
        xs[:4096] = inp["x"][b, r * 4096:(r + 1) * 4096]
        xs[4096:4160] = inp["ctx"][b, r * 64:(r + 1) * 64]
        cs = np.zeros((L0_T, 2, 32), np.float32)
        cs[:4096, 0] = cos[r * 4096:(r + 1) * 4096]
        cs[:4096, 1] = sin[r * 4096:(r + 1) * 4096]
        cs[4096:, 0] = 1.0
        m = np.stack([mod[0, b].reshape(6, D)[0:2], mod[0, 2].reshape(6, D)[0:2]])
        maps.append({"x": xs, "g": inp["norm1_g"][0], "mod": np.ascontiguousarray(m), "w": inp["ev_w_in"][0],
                     "cs": cs, "ident": ident})
    res = run(build_l0proj(), maps)
    p_lat = np.zeros((2, 16384, EVEN_IN), np.float32)
    p_ctx = np.zeros((2, 256, EVEN_IN), np.float32)
    for k in range(NCORES):
        b, r = k // 4, k % 4
        p_lat[b, r * 4096:(r + 1) * 4096] = res[k]["p"][:4096]
        p_ctx[b, r * 64:(r + 1) * 64] = res[k]["p"][4096:4160]
    return p_lat, p_ctx


NKEY = 16640
NKT = NKEY // 128
NQ = 4160


def lam_tile(c, lam_ap, lam_init, neg=True):
    lt = c.sb([128, 4, 64], F32, "lam_in")
    c.dma("sp", lt[:], lam_ap.rearrange("a d -> (a d)").partition_broadcast(128), writes=[lt])
    pr = c.sb([128, 2, 64], F32, "lam_pr")
    c.tt("dve", pr, pr[:, 0, :], lt, lt[:, 0, :], lt, lt[:, 1, :], ALU.mult)
    c.tt("dve", pr, pr[:, 1, :], lt, lt[:, 2, :], lt, lt[:, 3, :], ALU.mult)
    sm = c.sb([128, 2], F32, "lam_sm")
    c.op("dve", lambda: c.nc.vector.reduce_sum(sm[:], pr[:], AX.X), reads=[pr], writes=[sm])
    c.act(sm, sm[:], sm, sm[:], AF.Exp)
    lam = c.sb([128, 1], F32, "lam_sb")
    c.tt("dve", lam, lam[:], sm, sm[:, 0:1], sm, sm[:, 1:2], ALU.subtract)
    if neg:
        c.ts("dve", lam, lam[:], lam, lam[:], lam_init, -1.0, ALU.add, ALU.mult)
    else:
        c.ts("dve", lam, lam[:], lam, lam[:], lam_init, None, ALU.add)
    return lam


def build_attn(nheads=6, nq=NQ, nkt=NKT, lam_init=0.2, dbg=False):
    nc = new_nc()
    if dbg:
        dbg_out = dram_out(nc, "dbg", [6, 128, 512])
    nkey = nkt * 128
    qT = dram_in(nc, "qT", [nheads, 128, nq])
    kT = dram_in(nc, "kT", [nheads, 128, nkey])
    v = dram_in(nc, "v", [nkey, nheads, 128])
    lam_ap = dram_in(nc, "lam", [4, 64])
    out = dram_out(nc, "oT", [nheads, 128, nq])
    c = Ctx(nc)
    nlam = lam_tile(c, lam_ap, lam_init)
    ones = c.sb([128, 128], BF16, "ones")
    c.op("dve", lambda: nc.vector.memset(ones[:], 1.0), writes=[ones])
    onesf = c.sb([1, 128], F32, "onesf")
    c.op("dve", lambda: nc.vector.memset(onesf[:], 1.0), writes=[onesf])
    KT = c.sb([128, nkey], BF16, "KT")
    V = c.sb([128, nkt, 128], BF16, "V")
    QT = c.sb([128, nq], BF16, "QT")
    CH = 2048
    st = [c.sb([128, CH], F32, f"st{i}") for i in range(2)]
    S = [[c.ps([128, 512], F32, f"S{m}{i}") for i in range(2)] for m in range(2)]
    OT = [c.ps([128, 512], F32, f"OT{m}") for m in range(2)]
    SUM = [c.ps([1, 512], F32, f"SUM{m}") for m in range(2)]
    P = [[c.sb([128, 512], BF16, f"P{m}{i}") for i in range(2)] for m in range(2)]
    rc = [c.sb([1, 512], F32, f"rc{m}") for m in range(2)]
    o0 = c.sb([128, 512], F32, "o0")
    o1 = c.sb([128, 512], F32, "o1")
    ob = [c.sb([128, 512], F32, f"ob{i}") for i in range(2)]
    si = 0
    nblk = 0
    vv = v.rearrange("(t p) h e -> p t h e", p=128)
    for h in range(nheads):
        for (dst, src, n) in ((KT, kT, nkey), (QT, qT, nq)):
            for c0 in range(0, n, CH):
                c1 = min(n, c0 + CH)
                s = st[si % 2]
                c.dma("sp", s[:, 0:c1 - c0], src[h, :, c0:c1], writes=[s])
                c.cp(("dve", "pool")[si % 2], dst, dst[:, c0:c1], s, s[:, 0:c1 - c0])
                si += 1
        for t0 in range(0, nkt, 16):
            t1 = min(nkt, t0 + 16)
            s = st[si % 2]
            sv = s[:, 0:(t1 - t0) * 128].rearrange("p (t e) -> p t e", e=128)
            c.dma("sp", sv, vv[:, t0:t1, h, :], writes=[s])
            c.cp(("dve", "pool")[si % 2], V, V[:, t0:t1, :], s, sv)
            si += 1
        blocks = [(q0, min(512, nq - 64 - q0), 0, nkt) for q0 in range(0, nq - 64, 512)]
        blocks.append((nq - 64, 64, nkt - 2, nkt))
        for (q0, n, kt0, kt1) in blocks:
            for kt in range(kt0, kt1):
                for m in range(2):
                    s_ = S[m][kt % 2]
                    p_ = P[m][kt % 2]
                    c.mm(s_, s_[:, 0:n], KT, KT[m * 64:(m + 1) * 64, kt * 128:(kt + 1) * 128],
                         QT, QT[m * 64:(m + 1) * 64, q0:q0 + n])
                    c.act(p_, p_[:, 0:n], s_, s_[:, 0:n], AF.Exp, scale=0.125)
                    c.mm(OT[m], OT[m][:, 0:n], V, V[:, kt, :], p_, p_[:, 0:n], start=(kt == kt0), stop=(kt == kt1 - 1))
                    c.mm(SUM[m], SUM[m][0:1, 0:n], ones, ones[:, 0:1], p_, p_[:, 0:n], start=(kt == kt0), stop=(kt == kt1 - 1))
            for m in range(2):
                c.op("dve", lambda m=m: nc.vector.reciprocal(rc[m][0:1, 0:n], SUM[m][0:1, 0:n]), reads=[SUM[m]], writes=[rc[m]])
                bc = S[m][0]
                c.mm(bc, bc[:, 0:n], onesf, onesf[0:1, :], rc[m], rc[m][0:1, 0:n])
                dst = o0 if m == 0 else o1
                c.cp("act", dst, dst[:, 0:n], OT[m], OT[m][:, 0:n])
                c.tt("dve", dst, dst[:, 0:n], dst, dst[:, 0:n], bc, bc[:, 0:n], ALU.mult)
            o = ob[nblk % 2]
            nblk += 1
            c.stt("dve", o, o[:, 0:n], o1, o1[:, 0:n], nlam[:, 0:1], o0, o0[:, 0:n], ALU.mult, ALU.add, extra_reads=[nlam])
            c.dma("pool", out[h, :, q0:q0 + n], o[:, 0:n], reads=[o])
    if dbg:
        d = c.sb([128, 512], F32, "dbgt")
        for i, (src, np_) in enumerate(((P[0][0], 128), (P[1][1], 128), (o0, 128), (o1, 128), (rc[0], 1), (nlam, 128))):
            w = 1 if src is nlam else 512
            c.cp("dve", d, d[0:np_, 0:w], src, src[0:np_, 0:w])
            c.dma("sp", dbg_out[i, 0:np_, 0:w], d[0:np_, 0:w], reads=[d], allow_slow_non_contiguous=True)
    c.finish()
    return nc


def lam_init_of(l):
    return 0.8 - 0.6 * math.exp(-0.3 * l)


def run_attn(inp, p_lat, p_ctx):
    maps = []
    for k in range(NCORES):
        b, r = k // 4, k % 4
        pk = np.concatenate([p_lat[b], p_ctx[b]], axis=0)
        pq = np.concatenate([p_lat[b, r * 4096:(r + 1) * 4096], p_ctx[b, r * 64:(r + 1) * 64]], axis=0)
        qT = np.ascontiguousarray(pq[:, 256:1024].reshape(NQ, 6, 128).transpose(1, 2, 0))
        kT = np.ascontiguousarray(pk[:, 1024:1792].reshape(NKEY, 6, 128).transpose(1, 2, 0))
        v = np.ascontiguousarray(pk[:, 1792:2560].reshape(NKEY, 6, 128))
        maps.append({"qT": qT, "kT": kT, "v": v, "lam": inp["ev_lam"][0]})
    res = run(build_attn(lam_init=lam_init_of(0)), maps)
    o_lat = np.zeros((2, 16384, 6, 128), np.float32)
    o_ctx = np.zeros((2, 256, 6, 128), np.float32)
    for k in range(NCORES):
        b, r = k // 4, k % 4
        o = res[k]["oT"].transpose(2, 0, 1)
        o_lat[b, r * 4096:(r + 1) * 4096] = o[:4096]
        o_ctx[b, r * 64:(r + 1) * 64] = o[4096:]
    return o_lat, o_ctx


def fourier_tables():
    c64 = np.arange(64)
    a64 = 2 * np.pi * np.outer(c64, c64) / 64
    cs64 = np.concatenate([np.cos(a64), -np.sin(a64)], 1).astype(np.float32)
    n = np.arange(128)
    a = 2 * np.pi * np.outer(n, n) / 128
    cs128 = np.concatenate([np.cos(a), -np.sin(a)], 1).astype(np.float32)
    sc128 = np.concatenate([np.sin(a), np.cos(a)], 1).astype(np.float32)
    tw = 2 * np.pi * np.outer(n, n) / 16384.0
    twt = np.stack([np.cos(tw), np.sin(tw)], 0).astype(np.float32)
    m = np.arange(256)
    a256 = 2 * np.pi * np.outer(m, m) / 256
    cs256 = np.stack([np.cos(a256), np.sin(a256)], 0).astype(np.float32)
    return {"cs64": cs64, "cs128": cs128, "sc128": sc128, "tw": twt, "cs256": cs256}


def build_fourier():
    nc = new_nc()
    fT = dram_in(nc, "fT", [64, 16384])
    fcT = dram_in(nc, "fcT", [64, 256])
    cs64 = dram_in(nc, "cs64", [64, 128])
    cs128 = dram_in(nc, "cs128", [128, 256])
    sc128 = dram_in(nc, "sc128", [128, 256])
    tw = dram_in(nc, "tw", [2, 128, 128])
    cs256 = dram_in(nc, "cs256", [2, 256, 256])
    out = dram_out(nc, "fo", [128, 64, 128])
    outc = dram_out(nc, "foc", [256, 64])
    c = Ctx(nc)

    def ld_bf(ap, shape, name, q="sp"):
        f = c.sb(shape, F32, name + "_f")
        b = c.sb(shape, BF16, name)
        c.dma(q, f[:], ap, writes=[f])
        c.cp("dve", b, b[:], f, f[:])
        return b
    FT = c.sb([64, 16384], BF16, "FT")
    stg = [c.sb([64, 4096], F32, f"fst{i}") for i in range(2)]
    for i in range(4):
        s = stg[i % 2]
        c.dma("sp", s[:], fT[:, i * 4096:(i + 1) * 4096], writes=[s])
        c.cp(("dve", "pool")[i % 2], FT, FT[:, i * 4096:(i + 1) * 4096], s, s[:])
    CS64 = ld_bf(cs64[:, :], [64, 128], "CS64")
    CS128 = ld_bf(cs128[:, :], [128, 256], "CS128")
    SC128 = ld_bf(sc128[:, :], [128, 256], "SC128")
    TW = c.sb([128, 2, 128], F32, "TW")
    c.dma("sp", TW[:], tw.rearrange("a p k -> p a k"), writes=[TW])
    ps = [c.ps([128, 512], F32, f"fp{i}") for i in range(4)]
    G = c.sb([128, 128, 128], BF16, "G")
    FTv = FT[:, :].rearrange("c (a b) -> c a b", b=128)
    for j in range(32):
        p = ps[j % 4]
        for u in range(4):
            t2 = j * 4 + u
            c.mm(p, p[:, u * 128:(u + 1) * 128], FT, FTv[:, :, t2], CS64, CS64[:, :])
        c.cp(("act", "dve")[j % 2], G, G[:, j * 4:(j + 1) * 4, :].rearrange("p a b -> p (a b)"), p, p[:])
    Br = c.sb([128, 64, 128], BF16, "Br")
    Bi = c.sb([128, 64, 128], BF16, "Bi")
    tmp = [c.sb([128, 2, 256], F32, f"ftmp{i}") for i in range(2)]
    t1_ = [c.sb([128, 2, 128], F32, f"ft1{i}") for i in range(2)]
    t2_ = [c.sb([128, 2, 128], F32, f"ft2{i}") for i in range(2)]
    for j in range(32):
        p = ps[j % 4]
        for u in range(2):
            cp_ = j * 2 + u
            c.mm(p, p[:, u * 256:(u + 1) * 256], G, G[:, :, cp_], CS128, CS128[:, :], start=True, stop=False)
            c.mm(p, p[:, u * 256:(u + 1) * 256], G, G[:, :, 64 + cp_], SC128, SC128[:, :], start=False, stop=True)
        A = tmp[j % 2]
        c.cp("act", A, A[:].rearrange("p a b -> p (a b)"), p, p[:])
        Ar = A[:, :, 0:128]
        Ai = A[:, :, 128:256]
        cw = TW[:, 0:1, :].broadcast_to([128, 2, 128])
        sw = TW[:, 1:2, :].broadcast_to([128, 2, 128])
        a1, a2 = t1_[j % 2], t2_[j % 2]
        c.tt("dve", a1, a1[:], A, Ar, TW, cw, ALU.mult)
        c.tt("pool", a2, a2[:], A, Ai, TW, sw, ALU.mult)
        c.tt("dve", Br, Br[:, j * 2:(j + 1) * 2, :], a1, a1[:], a2, a2[:], ALU.add)
        c.tt("pool", a2, a2[:], A, Ai, TW, cw, ALU.mult)
        c.tt("dve", a1, a1[:], A, Ar, TW, sw, ALU.mult)
        c.tt("pool", Bi, Bi[:, j * 2:(j + 1) * 2, :], a2, a2[:], a1, a1[:], ALU.subtract)
    ot = [c.sb([128, 512], F32, f"fot{i}") for i in range(2)]
    Brv = Br[:].rearrange("p a b -> p (a b)")
    Biv = Bi[:].rearrange("p a b -> p (a b)")
    outv = out.rearrange("k c j -> k (c j)")
    for j in range(16):
        p = ps[j % 4]
        c.mm(p, p[:], CS128, CS128[:, 0:128], Br, Brv[:, j * 512:(j + 1) * 512], start=True, stop=False)
        c.mm(p, p[:], SC128, SC128[:, 0:128], Bi, Biv[:, j * 512:(j + 1) * 512], start=False, stop=True)
        o = ot[j % 2]
        c.act(o, o[:], p, p[:], AF.Copy, scale=1.0 / 1024.0)
        c.dma("sp", outv[:, j * 512:(j + 1) * 512], o[:], reads=[o])
    FC = ld_bf(fcT[:, :], [64, 256], "FC")
    C256 = ld_bf(cs256.rearrange("a (kc p) k -> p a kc k", p=128), [128, 2, 2, 256], "C256")
    Gc = c.sb([128, 2, 128], BF16, "Gc")
    p = ps[0]
    for tcn in range(2):
        c.mm(p, p[:, tcn * 128:(tcn + 1) * 128], FC, FC[:, tcn * 128:(tcn + 1) * 128], CS64, CS64[:, :])
    c.cp("act", Gc, Gc[:].rearrange("p a b -> p (a b)"), p, p[:, 0:256])
    oc = c.sb([128, 2, 64], F32, "foc_t")
    p = ps[1]
    for kc in range(2):
        n = 0
        for tcn in range(2):
            c.mm(p, p[:, kc * 64:(kc + 1) * 64], C256, C256[:, 0, tcn, kc * 128:(kc + 1) * 128], Gc, Gc[:, tcn, 0:64], start=(n == 0), stop=False)
            n += 1
            c.mm(p, p[:, kc * 64:(kc + 1) * 64], C256, C256[:, 1, tcn, kc * 128:(kc + 1) * 128], Gc, Gc[:, tcn, 64:128], start=False, stop=(tcn == 1))
    c.act(oc, oc[:].rearrange("p a b -> p (a b)"), p, p[:, 0:128], AF.Copy, scale=1.0 / 128.0)
    c.dma("sp", outc.rearrange("(kc p) c -> p kc c", p=128), oc[:], reads=[oc])
    c.finish()
    return nc


def run_fourier(p_lat, p_ctx):
    tabs = fourier_tables()
    maps = []
    for k in range(NCORES):
        b, g = k // 4, k % 4
        m = dict(tabs)
        m["fT"] = np.ascontiguousarray(p_lat[b, :, g * 64:(g + 1) * 64].T)
        m["fcT"] = np.ascontiguousarray(p_ctx[b, :, g * 64:(g + 1) * 64].T)
        maps.append(m)
    res = run(build_fourier(), maps)
    fl = np.zeros((2, 16384, 256), np.float32)
    fc = np.zeros((2, 256, 256), np.float32)
    for k in range(NCORES):
        b, g = k // 4, k % 4
        fo = res[k]["fo"]
        fl[b, :, g * 64:(g + 1) * 64] = fo.transpose(0, 2, 1).reshape(16384, 64)
        fc[b, :, g * 64:(g + 1) * 64] = res[k]["foc"]
    return fl, fc


class DB:
    def __init__(self):
        self.last_w = None
        self.reads = {}


def bcast_tile(c, ap_row, name, n=D, q="sp"):
    t = c.sb([128, n], F32, name)
    c.dma(q, t[:], ap_row.partition_broadcast(128), writes=[t])
    return t


def build_post(layer, ntiles, nlat, last, passes, lam_init=0.2):
    nc = new_nc()
    T = ntiles * 128
    Kmix = 1024 if layer == 0 else 2048
    KC = Kmix // 128
    x = dram_in(nc, "x", [T, D])
    if layer == 0:
        o_in = dram_in(nc, "o", [T, 768])
        f_in = dram_in(nc, "fo", [T, 256])
    else:
        of_in = dram_in(nc, "of", [T, 2048])
        ob_in = dram_in(nc, "ob", [T, 2048])
        z_in = dram_in(nc, "z", [T, 2048])
    sg = dram_in(nc, "sg", [128])
    g2 = dram_in(nc, "g2", [D])
    mod = dram_in(nc, "mod", [2, 6, D])
    wout = dram_in(nc, "wout", [Kmix, D])
    wr = dram_in(nc, "wr", [D, 32])
    br = dram_in(nc, "br", [32])
    w1 = dram_in(nc, "w1", [32, D, 2048])
    b1 = dram_in(nc, "b1", [32, 2048])
    w2 = dram_in(nc, "w2", [32, D, D])
    b2 = dram_in(nc, "b2", [32, D])
    ident = dram_in(nc, "ident", [128, 128])
    if last:
        fg = dram_in(nc, "fg", [D])
    xo = dram_out(nc, "xo", [T, D])
    h2s = nc.dram_tensor("h2s", [128, 8, T], BF16, kind="Internal").ap()
    c = Ctx(nc)
    idf, idb = load_ident(c, ident)
    gates = c.sb([128, ntiles, 32], F32, "gates")
    xo_db = [DB() for _ in range(ntiles)]
    h2_db = [DB() for _ in range(ntiles)]
    nseg = 1 if nlat == ntiles else 2

    c.push()
    woutb = load_weight_bf16(c, wout, Kmix, D, "woutb")
    wrf = c.sb([128, 8, 32], F32, "wrf")
    c.dma("sp", wrf[:], wr.rearrange("(kc p) n -> p kc n", p=128), writes=[wrf])
    brt = bcast_tile(c, br, "brt", 32)
    sgt = bcast_tile(c, sg, "sgt", 128)
    if layer == 0:
        c.ts("dve", sgt, sgt[:], sgt, sgt[:], 1.0 - lam_init, None, ALU.mult)
    mods2 = [make_mod_tiles(c, g2, mod, s_, 3, 4, f"m2{s_}") for s_ in range(nseg)]
    gate_m = [bcast_tile(c, mod[s_, 2:3, :], f"gm{s_}") for s_ in range(nseg)]
    nt = NormT(c, idb)
    mixp = [c.ps([128, 8, 128], BF16, "mixp0")]
    yp = [c.ps([128, 512], F32, f"yp{i}") for i in range(2)]
    hfp = [c.ps([128, 4, 128], F32, f"hfp{i}") for i in range(2)]
    lgp = c.ps([128, 32], F32, "lgp")
    mixed = c.sb([128, Kmix], BF16, "mixed")
    mixT = c.sb([128, KC, 128], BF16, "mixT")
    x1 = [c.sb([128, D], F32, f"x1_{i}") for i in range(2)]
    hf = c.sb([128, D], F32, "hf")
    hTf = c.sb([128, 8, 128], F32, "hTf")
    lg = c.sb([128, 32], F32, "lg")
    mx8 = c.sb([128, 8], F32, "mx8")
    msk = c.sb([128, 32], F32, "msk")
    ex = c.sb([128, 32], F32, "ex")
    sm = c.sb([128, 1], F32, "gsm")
    ytmp = c.sb([128, D], F32, "ytmp")
    if layer == 0:
        ot = [c.sb([128, 768], F32, f"o_t{i}") for i in range(2)]
        ft = [c.sb([128, 256], F32, f"f_t{i}") for i in range(2)]
        osq = c.sb([128, 6, 128], F32, "osq")
        oss = c.sb([128, 6], F32, "oss")
        nh_, eps_h = 6, 1e-5
    else:
        oft = [c.sb([128, 2048], F32, f"of_t{i}") for i in range(2)]
        obt = [c.sb([128, 2048], F32, f"ob_t{i}") for i in range(2)]
        zt = [c.sb([128, 2048], F32, f"z_t{i}") for i in range(2)]
        osq = c.sb([128, 16, 128], F32, "osq")
        oss = c.sb([128, 16], F32, "oss")
        nh_, eps_h = 16, 1e-6
    for t in range(ntiles):
        seg = 0 if t < nlat else 1
        rows = slice(t * 128, (t + 1) * 128)
        xt = nt.xt[t % 2]
        c.dma("sp", xt[:], x[rows, :], writes=[xt])
        if layer == 0:
            o_ = ot[t % 2]
            f_ = ft[t % 2]
            c.dma("pool", o_[:], o_in[rows, :], writes=[o_])
            c.dma("pool", f_[:], f_in[rows, :], writes=[f_])
            src = o_
            c.cp("act", mixed, mixed[:, 0:256], f_, f_[:])
            moff = 256
        else:
            a_, b_, z_ = oft[t % 2], obt[t % 2], zt[t % 2]
            c.dma("pool", a_[:], of_in[rows, :], writes=[a_])
            c.dma("pool", b_[:], ob_in[rows, :], writes=[b_])
            c.dma("sp", z_[:], z_in[rows, :], writes=[z_])
            c.tt("pool", a_, a_[:], a_, a_[:], b_, b_[:], ALU.add)
            c.act(z_, z_[:], z_, z_[:], AF.Silu)
            src = a_
            moff = 0
        sv_ = src[:, :].rearrange("p (h e) -> p h e", e=128)
        c.tt("pool", osq, osq[:], src, sv_, src, sv_, ALU.mult)
        c.op("dve", lambda: nc.vector.reduce_sum(oss[:], osq[:], AX.X), reads=[osq], writes=[oss])
        c.ts("dve", oss, oss[:], oss, oss[:], 1.0 / 128, eps_h, ALU.mult, ALU.add)
        c.op("act", lambda: nc.scalar.sqrt(oss[:], oss[:]), reads=[oss], writes=[oss])
        c.op("dve", lambda: nc.vector.reciprocal(oss[:], oss[:]), reads=[oss], writes=[oss])
        c.tt("dve", osq, osq[:], src, sv_, oss, oss[:].unsqueeze(2).broadcast_to([128, nh_, 128]), ALU.mult)
        mv = mixed[:, moff:Kmix].rearrange("p (h e) -> p h e", e=128)
        sgb = sgt[:].unsqueeze(1).broadcast_to([128, nh_, 128])
        if layer == 0:
            c.tt("pool", mixed, mv, osq, osq[:], sgt, sgb, ALU.mult)
        else:
            c.tt("pool", osq, osq[:], osq, osq[:], sgt, sgb, ALU.mult)
            c.tt("dve", mixed, mv, osq, osq[:], z_, z_[:, :].rearrange("p (h e) -> p h e", e=128), ALU.mult)
        for i_ in range(KC // 8):
            mp = mixp[0]
            for kc in range(i_ * 8, (i_ + 1) * 8):
                c.tr(mp, mp[:, kc % 8, :], mixed, mixed[:, kc * 128:(kc + 1) * 128], idb)
            c.cp(("act", "dve")[i_ % 2], mixT, mixT[:, i_ * 8:(i_ + 1) * 8, :], mp, mp[:])
        x1t = x1[t % 2]
        for n in range(2):
            for kc in range(KC):
                c.mm(yp[n], yp[n][:], mixT, mixT[:, kc, :], woutb, woutb[:, kc, n * 512:(n + 1) * 512],
                     start=(kc == 0), stop=(kc == KC - 1))
            cs_ = slice(n * 512, (n + 1) * 512)
            c.tt("dve", ytmp, ytmp[:, cs_], yp[n], yp[n][:], gate_m[seg], gate_m[seg][:, cs_], ALU.mult)
            c.tt("pool", x1t, x1t[:, cs_], ytmp, ytmp[:, cs_], xt, xt[:, cs_], ALU.add)
        c.dma("sp", xo[rows, :], x1t[:], reads=[x1t], writes=[xo_db[t]])
        G2, S2 = mods2[seg]
        hT = nt.run(None, G2, S2, xt=x1t, hf=hf)
        c.dma("pool", h2s[:, :, rows], hT[:], reads=[hT], writes=[h2_db[t]])
        for kc in range(8):
            hp = hfp[kc // 4]
            c.tr(hp, hp[:, kc % 4, :], hf, hf[:, kc * 128:(kc + 1) * 128], idf)
        for i_ in range(2):
            c.cp(("act", "dve")[i_], hTf, hTf[:, i_ * 4:(i_ + 1) * 4, :], hfp[i_], hfp[i_][:])
        for kc in range(8):
            c.mm(lgp, lgp[:], hTf, hTf[:, kc, :], wrf, wrf[:, kc, :], start=(kc == 0), stop=(kc == 7))
        c.tt("dve", lg, lg[:], lgp, lgp[:], brt, brt[:], ALU.add)
        c.op("dve", lambda: nc.vector.max(out=mx8[:], in_=lg[:]), reads=[lg], writes=[mx8])
        c.ts("dve", msk, msk[:], lg, lg[:], mx8[:, 3:4], None, ALU.is_ge, extra_reads=[mx8])
        c.ts("dve", mx8, mx8[:, 0:1], mx8, mx8[:, 0:1], -1.0, None, ALU.mult)
        c.act(ex, ex[:], lg, lg[:], AF.Exp, bias=mx8[:, 0:1], extra_reads=[mx8])
        c.tt("dve", ex, ex[:], ex, ex[:], msk, msk[:], ALU.mult)
        c.op("dve", lambda: nc.vector.reduce_sum(sm[:], ex[:], AX.X), reads=[ex], writes=[sm])
        c.op("dve", lambda: nc.vector.reciprocal(sm[:], sm[:]), reads=[sm], writes=[sm])
        c.ts("dve", gates, gates[:, t, :], ex, ex[:], sm[:, 0:1], None, ALU.mult, extra_reads=[sm])
    c.pop()

    c.push()
    maxp = max(passes)
    H2T = c.sb([128, 8, maxp * 128], BF16, "H2T")
    acc = c.sb([128, maxp, D], F32, "acc")
    w1g = c.sb([128, 8, 1024], BF16, "w1g")
    w1l = c.sb([128, 8, 1024], BF16, "w1l")
    w2b = [c.sb([128, 8, 1024], BF16, f"w2b{i}") for i in range(2)]
    stg = [c.sb([128, 2048], F32, f"wst{i}") for i in range(2)]
    b1t = [c.sb([128, 8, 2], F32, f"b1t{i}") for i in range(2)]
    b2f = c.sb([1, D], F32, "b2f")
    b2b = [c.sb([1, D], BF16, f"b2b{i}") for i in range(2)]
    onesb = c.sb([1, 128], BF16, "onesb")
    c.op("dve", lambda: nc.vector.memset(onesb[:], 1.0), writes=[onesb])
    aT = c.sb([128, 8, maxp * 128], BF16, "aT")
    glu = [c.sb([128, 512], F32, f"glu{i}") for i in range(2)]
    sig = [c.sb([128, 512], F32, "sig0")] * 2
    lin = [c.sb([128, 512], F32, f"lin{i}") for i in range(2)]
    ugp = [c.ps([128, 512], F32, f"ugp{i}") for i in range(2)]
    ulp = [c.ps([128, 512], F32, f"ulp{i}") for i in range(2)]
    ypp = [c.ps([128, 512], F32, f"ypp{i}") for i in range(4)]
    gate5 = [bcast_tile(c, mod[s_, 5:6, :], f"g5{s_}") for s_ in range(nseg)]
    xr = [c.sb([128, D], F32, "xr0")] * 2
    if last:
        fgt = bcast_tile(c, fg, "fgt")
        fsq = c.sb([128, D], F32, "fsq")
        fss = c.sb([128, 1], F32, "fss")
    si = 0
    jj = 0
    yi = 0
    ei = 0
    tbase = 0
    for npass in passes:
        tiles = list(range(tbase, tbase + npass))
        ntok = npass * 128
        c.dma("sp", H2T[:, :, 0:ntok], h2s[:, :, tbase * 128:tbase * 128 + ntok], reads=[h2_db[t] for t in tiles], writes=[H2T])
        c.op("pool", lambda: nc.gpsimd.memset(acc[:], 0.0), writes=[acc])
        for e in range(32):
            w2e = w2b[ei % 2]
            b1e = b1t[ei % 2]
            b2e = b2b[ei % 2]
            ei += 1
            for kc in range(8):
                s_ = stg[si % 2]
                c.dma("sp", s_[:], w1[e, kc * 128:(kc + 1) * 128, :], writes=[s_])
                sv2 = s_[:, :].rearrange("p (n two) -> p n two", two=2)
                c.cp("act", w1g, w1g[:, kc, :], s_, sv2[:, :, 0])
                c.cp("dve", w1l, w1l[:, kc, :], s_, sv2[:, :, 1])
                si += 1
            c.dma("sp", b1e[:], b1[e].rearrange("(c p two) -> p c two", p=128, two=2), writes=[b1e])
            c.dma("sp", b2f[:], b2[e:e + 1, :], writes=[b2f])
            c.cp("dve", b2e, b2e[:], b2f, b2f[:])
            for kc2 in range(4):
                s_ = stg[si % 2]
                c.dma("sp", s_[:].rearrange("p (a n) -> p a n", a=2),
                      w2[e, kc2 * 256:(kc2 + 1) * 256, :].rearrange("(a p) n -> p a n", p=128), writes=[s_])
                c.cp(("act", "dve")[kc2 % 2], w2e, w2e[:, kc2 * 2:kc2 * 2 + 2, :].rearrange("p a n -> p (a n)"), s_, s_[:])
                si += 1
            for blk0 in range(0, npass, 4):
                nb = min(4, npass - blk0)
                n = nb * 128
                tok0 = blk0 * 128
                for j in range(8):
                    ug, ul = ugp[jj % 2], ulp[jj % 2]
                    gl, sg_, ln = glu[jj % 2], sig[jj % 2], lin[jj % 2]
                    jj += 1
                    for kc in range(8):
                        c.mm(ug, ug[:, 0:n], w1g, w1g[:, kc, j * 128:(j + 1) * 128], H2T, H2T[:, kc, tok0:tok0 + n],
                             start=(kc == 0), stop=(kc == 7))
                    for kc in range(8):
                        c.mm(ul, ul[:, 0:n], w1l, w1l[:, kc, j * 128:(j + 1) * 128], H2T, H2T[:, kc, tok0:tok0 + n],
                             start=(kc == 0), stop=(kc == 7))
                    c.ts("dve", gl, gl[:, 0:n], ug, ug[:, 0:n], b1e[:, j, 0:1], 7.0, ALU.add, ALU.min, extra_reads=[b1e])
                    c.act(sg_, sg_[:, 0:n], gl, gl[:, 0:n], AF.Sigmoid, scale=1.702)
                    c.ts("dve", ln, ln[:, 0:n], ul, ul[:, 0:n], b1e[:, j, 1:2], 7.0, ALU.add, ALU.min, extra_reads=[b1e])
                    c.ts("pool", ln, ln[:, 0:n], ln, ln[:, 0:n], -7.0, 1.0, ALU.max, ALU.add)
                    c.tt("dve", gl, gl[:, 0:n], gl, gl[:, 0:n], sg_, sg_[:, 0:n], ALU.mult)
                    c.tt("dve", aT, aT[:, j, tok0:tok0 + n], gl, gl[:, 0:n], ln, ln[:, 0:n], ALU.mult)
            for tl in range(npass):
                for nh in range(2):
                    y_ = ypp[yi % 4]
                    yi += 1
                    cs_ = slice(nh * 512, (nh + 1) * 512)
                    c.mm(y_, y_[:], onesb, onesb[0:1, :], b2e, b2e[0:1, cs_], start=True, stop=False)
                    for j in range(8):
                        c.mm(y_, y_[:], aT, aT[:, j, tl * 128:(tl + 1) * 128], w2e, w2e[:, j, cs_],
                             start=False, stop=(j == 7))
                    c.stt("dve", acc, acc[:, tl, cs_], y_, y_[:], gates[:, tbase + tl, e:e + 1], acc, acc[:, tl, cs_],
                          ALU.mult, ALU.add, extra_reads=[gates])
        for tl, t in enumerate(tiles):
            seg = 0 if t < nlat else 1
            rows = slice(t * 128, (t + 1) * 128)
            xr_ = xr[t % 2]
            c.dma("sp", xr_[:], xo[rows, :], reads=[xo_db[t]], writes=[xr_])
            c.tt("pool", acc, acc[:, tl, :], acc, acc[:, tl, :], gate5[seg], gate5[seg][:], ALU.mult)
            c.tt("dve", xr_, xr_[:], xr_, xr_[:], acc, acc[:, tl, :], ALU.add)
            if last:
                c.act(fsq, fsq[:], xr_, xr_[:], AF.Square, accum=(fss, fss[:, 0:1]))
                c.ts("dve", fss, fss[:], fss, fss[:], 1.0 / D, 1e-6, ALU.mult, ALU.add)
                c.op("act", lambda: nc.scalar.sqrt(fss[:], fss[:]), reads=[fss], writes=[fss])
                c.op("dve", lambda: nc.vector.reciprocal(fss[:], fss[:]), reads=[fss], writes=[fss])
                c.stt("dve", xr_, xr_[:], xr_, xr_[:], fss[:, 0:1], fgt, fgt[:], ALU.mult, ALU.mult, extra_reads=[fss])
            c.dma("sp", xo[rows, :], xr_[:], reads=[xr_], writes=[xo_db[t]])
        tbase += npass
    c.pop()
    c.finish()
    return nc


CH = 64
GDN_F32R = False


def gdn_consts():
    i = np.arange(64)
    tri = (i[:, None] <= i[None, :]).astype(np.float32)
    sel = np.zeros((64, 128), np.float32)
    sel[63, :] = 1.0
    mincl = (i[:, None] >= i[None, :]).astype(np.float32)
    negm = (mincl - 1.0) * 30000.0
    mstrict = (i[:, None] > i[None, :]).astype(np.float32)
    return {"tri": tri, "sel": sel, "mincl": mincl, "negm": negm.astype(np.float32), "mstrict": mstrict,
            "identf": np.eye(128, dtype=np.float32)}


def build_gdn(nlat_groups=32, gch=8, nseq=8):
    nc = new_nc()
    nctx_ch = 4
    nchunks = nctx_ch + nlat_groups * gch
    ntok = nchunks * CH
    nlat = nlat_groups * gch * CH
    TP = 2 + 256 + 2 + 2 + nlat + 2
    NX = nchunks * nseq
    nqk = max(2, nseq // 2) if nseq > 1 else 1
    qk_pre = dram_in(nc, "qk_pre", [nqk, 2, 128, TP])
    v_pre = dram_in(nc, "v_pre", [nseq, 128, TP])
    cw_qk = dram_in(nc, "cw_qk", [nqk, 2, 128, 5])
    cw_v = dram_in(nc, "cw_v", [nseq, 128, 5])
    a_x = dram_in(nc, "a_x", [64, nchunks, nseq])
    b_x = dram_in(nc, "b_x", [64, nchunks, nseq])
    alog_x = dram_in(nc, "alog_x", [64, nseq])
    dtb_x = dram_in(nc, "dtb_x", [64, nseq])
    cst = {k: dram_in(nc, k, list(v.shape)) for k, v in gdn_consts().items()}
    o_out = dram_out(nc, "o", [nseq, ntok, 128])
    gts = nc.dram_tensor("gts", [NX, 64], F32, kind="Internal").ap()
    gts_db = DB()
    c = Ctx(nc)
    c.F32R = GDN_F32R

    def ldc(name, shape):
        t = c.sb(shape, F32, name + "_c")
        c.dma("sp", t[:], cst[name][:, :], writes=[t])
        return t
    tri = ldc("tri", [64, 64])
    sel = ldc("sel", [64, 128])
    mincl = ldc("mincl", [64, 64])
    negm = ldc("negm", [64, 64])
    mstrict = ldc("mstrict", [64, 64])
    idf = ldc("identf", [128, 128])
    ones = c.sb([128, 128], F32, "ones")
    c.op("dve", lambda: nc.vector.memset(ones[:], 1.0), writes=[ones])
    pb = [c.ps([128, 512], F32, f"pb{i}") for i in range(8)]
    pi = [0]

    def nextp():
        p = pb[pi[0] % 8]
        pi[0] += 1
        return p

    c.push()
    ax = c.sb([64, NX], F32, "ax")
    bx = c.sb([64, NX], F32, "bx")
    c.dma("sp", ax[:], a_x.rearrange("p c s -> p (c s)"), writes=[ax])
    c.dma("sp", bx[:], b_x.rearrange("p c s -> p (c s)"), writes=[bx])
    al = c.sb([64, nseq], F32, "al")
    dtb = c.sb([64, nseq], F32, "dtb")
    c.dma("sp", al[:], alog_x[:, :], writes=[al])
    c.dma("sp", dtb[:], dtb_x[:, :], writes=[dtb])
    c.act(al, al[:], al, al[:], AF.Exp)
    c.ts("dve", al, al[:], al, al[:], -1.0, None, ALU.mult)

    def xv(t):
        return t[:, :].rearrange("p (c s) -> p c s", s=nseq)
    bc8 = lambda t: t[:, :].unsqueeze(1).broadcast_to([64, nchunks, nseq])
    c.tt("dve", ax, xv(ax), ax, xv(ax), dtb, bc8(dtb), ALU.add)
    c.act(ax, ax[:], ax, ax[:], AF.Exp)
    c.act(ax, ax[:], ax, ax[:], AF.Ln, bias=1.0)
    c.tt("dve", ax, xv(ax), ax, xv(ax), al, bc8(al), ALU.mult)
    c.pop()
    beta = c.sb([64, NX], F32, "beta")
    G = c.sb([64, NX], F32, "G")
    eG = c.sb([64, NX], F32, "eG")
    bk = c.sb([64, NX], F32, "bk")
    kts = c.sb([64, NX], F32, "kts")
    eGe = c.sb([128, NX], F32, "eGe")
    c.push()
    gx = c.sb([64, NX], F32, "gx")
    bx2 = c.sb([64, NX], F32, "bx2")
    c.dma("sp", gx[:], a_x.rearrange("p c s -> p (c s)"), writes=[gx])
    c.dma("sp", bx2[:], b_x.rearrange("p c s -> p (c s)"), writes=[bx2])
    al2 = c.sb([64, nseq], F32, "al2")
    dtb2 = c.sb([64, nseq], F32, "dtb2")
    c.dma("sp", al2[:], alog_x[:, :], writes=[al2])
    c.dma("sp", dtb2[:], dtb_x[:, :], writes=[dtb2])
    c.act(al2, al2[:], al2, al2[:], AF.Exp)
    c.ts("dve", al2, al2[:], al2, al2[:], -1.0, None, ALU.mult)
    c.tt("dve", gx, xv(gx), gx, xv(gx), dtb2, bc8(dtb2), ALU.add)
    c.act(gx, gx[:], gx, gx[:], AF.Exp)
    c.act(gx, gx[:], gx, gx[:], AF.Ln, bias=1.0)
    c.tt("dve", gx, xv(gx), gx, xv(gx), al2, bc8(al2), ALU.mult)
    c.act(beta, beta[:], bx2, bx2[:], AF.Sigmoid)
    Ge = c.sb([128, NX], F32, "Ge")
    for n0 in range(0, NX, 512):
        n1 = min(NX, n0 + 512)
        p = nextp()
        c.mm(p, p[0:64, 0:n1 - n0], tri, tri[:, :], gx, gx[:, n0:n1])
        c.cp("act", G, G[:, n0:n1], p, p[0:64, 0:n1 - n0])
        p2 = nextp()
        c.mm(p2, p2[:, 0:n1 - n0], sel, sel[:, :], G, G[:, n0:n1])
        c.cp("dve", Ge, Ge[:, n0:n1], p2, p2[:, 0:n1 - n0])
    c.act(eG, eG[:], G, G[:], AF.Exp)
    c.act(eGe, eGe[:], Ge, Ge[:], AF.Exp)
    c.tt("dve", kts, kts[:], Ge, Ge[0:64, :], G, G[:], ALU.subtract)
    c.act(kts, kts[:], kts, kts[:], AF.Exp)
    c.tt("dve", bk, bk[:], beta, beta[:], eG, eG[:], ALU.mult)
    gtt = [c.sb([128, 64], F32, f"gtt{i}") for i in range(2)]
    for i_, n0 in enumerate(range(0, NX, 128)):
        n1 = min(NX, n0 + 128)
        p = nextp()
        c.tr(p, p[0:n1 - n0, 0:64], G, G[:, n0:n1], idf_v := c.view(idf))
        g_ = gtt[i_ % 2]
        c.cp("act", g_, g_[0:n1 - n0, :], p, p[0:n1 - n0, 0:64])
        c.dma("sp", gts[n0:n1, :], g_[0:n1 - n0, :], reads=[g_], writes=[gts_db])
    c.pop()

    NT = gch * CH
    S = [c.sb([128, 128], F32, f"S{s}") for s in range(nseq)]
    for s in range(nseq):
        c.op("pool", lambda s=s: nc.gpsimd.memset(S[s][:], 0.0), writes=[S[s]])
    NSLOT = 2

    def mkslot(k):
        B = {}
        B["xin"] = [c.sb([128, NT + 4], F32, f"xin{k}_{i}") for i in range(2)]
        B["cwt"] = c.sb([128, 3, 5], F32, f"cwt{k}")
        for nm in ("qT", "kT", "vT", "sq", "rs"):
            B[nm] = c.sb([128, NT], F32, f"{nm}{k}")
        B["ktail"] = c.sb([64, gch, 128], F32, f"ktail{k}")
        B["R"] = c.sb([64, gch, 256], F32, f"R{k}")
        for nm in ("L", "LT", "L2", "LT2", "intraT", "dec", "tmp64"):
            B[nm] = c.sb([64, gch, 64], F32, f"{nm}{k}")
        B["wT"] = c.sb([128, gch, 64], F32, f"wT{k}")
        B["grow"] = c.sb([1, NT], F32, f"grow{k}")
        B["ngrow"] = c.sb([1, NT], F32, f"ngrow{k}")
        B["vnew"] = [c.sb([64, 128], F32, f"vnew{k}_{i}") for i in range(2)]
        B["o2s"] = [c.sb([64, 128], F32, f"o2s{k}_{i}") for i in range(2)]
        B["osb"] = [c.sb([64, gch, 128], F32, f"osb{k}_{i}") for i in range(2)]
        B["gi"] = 0
        B["xi"] = 0
        return B
    slots = [mkslot(k) for k in range(NSLOT)]
    gts_v = gts.rearrange("(c s) j -> s c j", s=nseq)
    groups = [(0, nctx_ch, 0)] + [(nctx_ch + g * gch, gch, 260 + g * NT) for g in range(nlat_groups)]

    def seq_body(s, c0, ncg, col0, SL):
        n = ncg * CH
        xin, cwt, qT, kT, vT, sq, rs = SL['xin'], SL['cwt'], SL['qT'], SL['kT'], SL['vT'], SL['sq'], SL['rs']
        ktail, R, L, LT, L2, LT2 = SL['ktail'], SL['R'], SL['L'], SL['LT'], SL['L2'], SL['LT2']
        intraT, dec, tmp64, wT, grow, ngrow = SL['intraT'], SL['dec'], SL['tmp64'], SL['wT'], SL['grow'], SL['ngrow']
        vnew, o2s, osb = SL['vnew'], SL['o2s'], SL['osb']
        qki = s // 4 * 2 + (s % 2) if nseq > 1 else 0
        c.dma("pool", cwt[:, 0, :], cw_qk[qki, 0, :, :], writes=[cwt])
        c.dma("pool", cwt[:, 1, :], cw_qk[qki, 1, :, :], writes=[cwt])
        c.dma("pool", cwt[:, 2, :], cw_v[s, :, :], writes=[cwt])
        for wi, (src_ap, dst) in enumerate(((qk_pre[qki, 0], qT), (qk_pre[qki, 1], kT), (v_pre[s], vT))):
            xi_ = xin[SL['xi'] % 2]
            SL['xi'] += 1
            c.dma("sp", xi_[:, 0:n + 4], src_ap[:, col0:col0 + n + 4], writes=[xi_])
            c.ts("dve", dst, dst[:, 0:n], xi_, xi_[:, 0:n], cwt[:, wi, 0:1], None, ALU.mult, extra_reads=[cwt])
            for j in range(1, 5):
                c.stt("dve", dst, dst[:, 0:n], xi_, xi_[:, j:j + n], cwt[:, wi, j:j + 1], dst, dst[:, 0:n],
                      ALU.mult, ALU.add, extra_reads=[cwt])
            c.act(dst, dst[:, 0:n], dst, dst[:, 0:n], AF.Silu)
            yield
            if wi < 2:
                c.tt("pool", sq, sq[:, 0:n], dst, dst[:, 0:n], dst, dst[:, 0:n], ALU.mult)
                for n0 in range(0, n, 512):
                    n1 = min(n, n0 + 512)
                    p = nextp()
                    c.mm(p, p[:, 0:n1 - n0], ones, ones[:, :], sq, sq[:, n0:n1])
                    c.ts("dve", rs, rs[:, n0:n1], p, p[:, 0:n1 - n0], 1e-6, None, ALU.add)
                c.op("act", lambda: nc.scalar.sqrt(rs[:, 0:n], rs[:, 0:n]), reads=[rs], writes=[rs])
                c.op("dve", lambda: nc.vector.reciprocal(rs[:, 0:n], rs[:, 0:n]), reads=[rs], writes=[rs])
                if wi == 0:
                    c.stt("dve", dst, dst[:, 0:n], dst, dst[:, 0:n], 128.0 ** -0.5, rs, rs[:, 0:n], ALU.mult, ALU.mult)
                else:
                    c.tt("dve", dst, dst[:, 0:n], dst, dst[:, 0:n], rs, rs[:, 0:n], ALU.mult)
        def X(t):
            return t[:, :].rearrange("p (c s) -> p c s", s=nseq)[:, c0:c0 + ncg, s]

        def Xb(t, w):
            return X(t).unsqueeze(2).broadcast_to([64, ncg, w])
        for cc0 in range(0, ncg, 4):
            pk = nextp()
            pv = nextp()
            for u in range(4):
                cc = cc0 + u
                c.tr(pk, pk[0:64, u * 128:(u + 1) * 128], kT, kT[:, cc * 64:(cc + 1) * 64], idf)
                c.tr(pv, pv[0:64, u * 128:(u + 1) * 128], vT, vT[:, cc * 64:(cc + 1) * 64], idf)
            pk3 = pk[0:64, :].rearrange("p (a b) -> p a b", b=128)
            pv3 = pv[0:64, :].rearrange("p (a b) -> p a b", b=128)
            sl = slice(cc0, cc0 + 4)
            c.tt("dve", R, R[:, sl, 0:128], pv, pv3, beta, Xb(beta, 128)[:, sl, :], ALU.mult)
            c.tt("dve", R, R[:, sl, 128:256], pk, pk3, bk, Xb(bk, 128)[:, sl, :], ALU.mult)
            c.tt("dve", ktail, ktail[:, sl, :], pk, pk3, kts, Xb(kts, 128)[:, sl, :], ALU.mult)
            yield
        c.dma("pool", grow[0:1, 0:n].rearrange("p (c j) -> p c j", j=64), gts_v[s:s + 1, c0:c0 + ncg, :],
              reads=[gts_db], writes=[grow])
        c.ts("dve", ngrow, ngrow[0:1, 0:n], grow, grow[0:1, 0:n], -1.0, None, ALU.mult)
        bm = lambda t: t[:, :].unsqueeze(1).broadcast_to([64, 8, 64])
        for cc0 in range(0, ncg, 8):
            nb = min(8, ncg - cc0)
            sl = slice(cc0, cc0 + nb)
            pd = nextp()
            pkk = nextp()
            pqk = nextp()
            for u in range(nb):
                cc = cc0 + u
                cs_ = slice(cc * 64, (cc + 1) * 64)
                us_ = slice(u * 64, (u + 1) * 64)
                c.mm(pd, pd[0:64, us_], grow, grow[0:1, cs_], ones, ones[0:1, 0:64], start=True, stop=False)
                c.mm(pd, pd[0:64, us_], ones, ones[0:1, 0:64], ngrow, ngrow[0:1, cs_], start=False, stop=True)
                c.mm(pkk, pkk[0:64, us_], kT, kT[:, cs_], kT, kT[:, cs_])
                c.mm(pqk, pqk[0:64, us_], qT, qT[:, cs_], kT, kT[:, cs_])
            v3 = lambda p: p[0:64, 0:nb * 64].rearrange("p (a b) -> p a b", b=64)
            mb = lambda t: t[:, :].unsqueeze(1).broadcast_to([64, nb, 64])
            c.tt("dve", dec, dec[:, sl, :], pd, v3(pd), mincl, mb(mincl), ALU.mult)
            c.tt("pool", dec, dec[:, sl, :], dec, dec[:, sl, :], negm, mb(negm), ALU.add)
            c.act(dec, dec[:, sl, :], dec, dec[:, sl, :], AF.Exp)
            c.tt("dve", L, L[:, sl, :], pkk, v3(pkk), dec, dec[:, sl, :], ALU.mult)
            c.tt("pool", L, L[:, sl, :], L, L[:, sl, :], mstrict, mb(mstrict), ALU.mult)
            c.tt("pool", L, L[:, sl, :], L, L[:, sl, :], beta, Xb(beta, 64)[:, sl, :], ALU.mult)
            c.tt("dve", tmp64, tmp64[:, sl, :], pqk, v3(pqk), dec, dec[:, sl, :], ALU.mult)
            pl_ = nextp()
            pi_ = nextp()
            for u in range(nb):
                cc = cc0 + u
                us_ = slice(u * 64, (u + 1) * 64)
                c.tr(pl_, pl_[0:64, us_], L, L[:, cc, :], idf_v)
                c.tr(pi_, pi_[0:64, us_], tmp64, tmp64[:, cc, :], idf_v)
            c.cp("act", LT, LT[:, sl, :], pl_, v3(pl_))
            c.cp("act", intraT, intraT[:, sl, :], pi_, v3(pi_))
            yield
        A, AT, B, BT = L, LT, L2, LT2
        for lev in range(6):
            for cc0 in range(0, ncg, 2):
                p = nextp()
                for u in range(2):
                    cc = cc0 + u
                    c.mm(p, p[0:64, u * 256:(u + 1) * 256], AT, AT[:, cc, :], R, R[:, cc, :])
                p3 = p[0:64, :].rearrange("p (a b) -> p a b", b=256)
                sl = slice(cc0, cc0 + 2)
                c.tt(("dve", "pool")[0], R, R[:, sl, :], R, R[:, sl, :], p, p3, ALU.subtract if lev == 0 else ALU.add)
            yield
            if lev < 5:
                for cc0 in range(0, ncg, 8):
                    nb = min(8, ncg - cc0)
                    sl = slice(cc0, cc0 + nb)
                    pa = nextp()
                    pt = nextp()
                    for u in range(nb):
                        cc = cc0 + u
                        us_ = slice(u * 64, (u + 1) * 64)
                        c.mm(pa, pa[0:64, us_], AT, AT[:, cc, :], A, A[:, cc, :])
                        c.mm(pt, pt[0:64, us_], A, A[:, cc, :], AT, AT[:, cc, :])
                    v3 = lambda p: p[0:64, 0:nb * 64].rearrange("p (a b) -> p a b", b=64)
                    c.cp("act", B, B[:, sl, :], pa, v3(pa))
                    c.cp("act", BT, BT[:, sl, :], pt, v3(pt))
                yield
                A, AT, B, BT = B, BT, A, AT
        for cc0 in range(0, ncg, 8):
            nb = min(8, ncg - cc0)
            p = nextp()
            for u in range(nb):
                cc = cc0 + u
                c.tr(p, p[:, u * 64:(u + 1) * 64], R, R[:, cc, 128:256], idf_v)
            c.cp("act", wT, wT[:, cc0:cc0 + nb, :], p, p[:, 0:nb * 64].rearrange("p (a b) -> p a b", b=64))
        ob_ = osb[SL['gi'] % 2]
        SL['gi'] += 1
        St = S[s]
        for cc in range(ncg):
            col = (c0 + cc) * nseq + s
            cs_ = slice(cc * 64, (cc + 1) * 64)
            pv_ = nextp()
            c.mm(pv_, pv_[0:64, 0:128], wT, wT[:, cc, :], St, St[:, :])
            vn = vnew[cc % 2]
            c.tt("dve", vn, vn[:], R, R[:, cc, 0:128], pv_, pv_[0:64, 0:128], ALU.subtract)
            po1 = nextp()
            c.mm(po1, po1[0:64, 0:128], qT, qT[:, cs_], St, St[:, :])
            po2 = nextp()
            c.mm(po2, po2[0:64, 0:128], intraT, intraT[:, cc, :], vn, vn[:])
            ps_ = nextp()
            c.mm(ps_, ps_[:, 0:128], ktail, ktail[:, cc, :], vn, vn[:])
            o2 = o2s[cc % 2]
            c.cp("act", o2, o2[:], po2, po2[0:64, 0:128])
            c.stt("dve", ob_, ob_[:, cc, :], po1, po1[0:64, 0:128], eG[:, col:col + 1], o2, o2[:], ALU.mult, ALU.add,
                  extra_reads=[eG])
            c.stt("dve", St, St[:, :], St, St[:, :], eGe[:, col:col + 1], ps_, ps_[:, 0:128], ALU.mult, ALU.add,
                  extra_reads=[eGe])
            yield
        c.dma("sp", o_out[s, c0 * 64:(c0 + ncg) * 64, :].rearrange("(c p) e -> p c e", p=64), ob_[:, 0:ncg, :], reads=[ob_])


    for (c0, ncg, col0) in groups:
        for s0 in range(0, nseq, NSLOT):
            gens = [seq_body(s0 + i, c0, ncg, col0, slots[i]) for i in range(NSLOT) if s0 + i < nseq]
            while gens:
                for g_ in list(gens):
                    try:
                        next(g_)
                    except StopIteration:
                        gens.remove(g_)
    c.finish()
    return nc


ODD_IN = 6208


def run_l1proj(inp, mod, x_lat, x_ctx):
    ident = np.eye(128, dtype=np.float32)
    maps = []
    for k in range(NCORES):
        b, r = k // 4, k % 4
        xs = np.zeros((L0_T, D), np.float32)
        xs[:4096] = x_lat[b, r * 4096:(r + 1) * 4096]
        xs[4096:4160] = x_ctx[b, r * 64:(r + 1) * 64]
        m = np.stack([mod[1, b].reshape(6, D)[0:2], mod[1, 2].reshape(6, D)[0:2]])
        maps.append({"x": xs, "g": inp["norm1_g"][1], "mod": np.ascontiguousarray(m), "w": inp["od_w_in"][0], "ident": ident})
    res = run(build_l0proj(N=ODD_IN, rope=False), maps)
    p_lat = np.zeros((2, 16384, ODD_IN), np.float32)
    p_ctx = np.zeros((2, 256, ODD_IN), np.float32)
    for k in range(NCORES):
        b, r = k // 4, k % 4
        p_lat[b, r * 4096:(r + 1) * 4096] = res[k]["p"][:4096]
        p_ctx[b, r * 64:(r + 1) * 64] = res[k]["p"][4096:4160]
    return p_lat, p_ctx


def run_post0(inp, mod, o_lat, o_ctx, f_lat, f_ctx):
    ident = np.eye(128, dtype=np.float32)
    maps = []
    for k in range(NCORES):
        b, r = k // 4, k % 4

        def sh(lat, ctx, w):
            a = np.zeros((L0_T, w), np.float32)
            a[:4096] = lat[b, r * 4096:(r + 1) * 4096].reshape(4096, w)
            a[4096:4160] = ctx[b, r * 64:(r + 1) * 64].reshape(64, w)
            return a
        m = np.stack([mod[0, b].reshape(6, D), mod[0, 2].reshape(6, D)])
        maps.append({"x": sh(inp["x"], inp["ctx"], D), "o": sh(o_lat, o_ctx, 768), "fo": sh(f_lat, f_ctx, 256),
                     "sg": inp["ev_subln_g"][0], "g2": inp["norm2_g"][0], "mod": np.ascontiguousarray(m),
                     "wout": inp["ev_w_out"][0], "wr": inp["moe_w_router"][0], "br": inp["moe_b_router"][0],
                     "w1": inp["moe_w1"][0], "b1": inp["moe_b1"][0], "w2": inp["moe_w2"][0], "b2": inp["moe_b2"][0],
                     "ident": ident})
    res = run(build_post(0, 33, 32, False, [11, 11, 11], lam_init=lam_init_of(0)), maps)
    x_lat = np.zeros((2, 16384, D), np.float32)
    x_ctx = np.zeros((2, 256, D), np.float32)
    for k in range(NCORES):
        b, r = k // 4, k % 4
        x_lat[b, r * 4096:(r + 1) * 4096] = res[k]["xo"][:4096]
        x_ctx[b, r * 64:(r + 1) * 64] = res[k]["xo"][4096:4160]
    return x_lat, x_ctx


def run_gdn(inp, p_lat, p_ctx):
    consts = gdn_consts()
    conv_w = inp["od_conv_w"][0]
    TP = 2 + 256 + 2 + 2 + 16384 + 2
    maps = []
    for k in range(NCORES):
        b, r = k // 4, k % 4

        def seq(cols, d):
            c_ = p_ctx[b][:, cols]
            l_ = p_lat[b][:, cols]
            if d == 1:
                c_ = c_[::-1]
                l_ = l_[::-1]
            out = np.zeros((cols.stop - cols.start, TP), np.float32)
            out[:, 2:258] = c_.T
            out[:, 262:262 + 16384] = l_.T
            return out

        def taps(cols, d):
            w = conv_w[:, cols].T
            return np.ascontiguousarray(w[:, ::-1] if d == 1 else w)
        qk_pre = np.zeros((4, 2, 128, TP), np.float32)
        cw_qk = np.zeros((4, 2, 128, 5), np.float32)
        for khl in range(2):
            kh = 2 * r + khl
            for d in range(2):
                for j, base in enumerate((0, 1024)):
                    cols = slice(base + kh * 128, base + (kh + 1) * 128)
                    qk_pre[khl * 2 + d, j] = seq(cols, d)
                    cw_qk[khl * 2 + d, j] = taps(cols, d)
        v_pre = np.zeros((8, 128, TP), np.float32)
        cw_v = np.zeros((8, 128, 5), np.float32)
        a_x = np.zeros((64, 260, 8), np.float32)
        b_x = np.zeros((64, 260, 8), np.float32)
        alog_x = np.zeros((64, 8), np.float32)
        dtb_x = np.zeros((64, 8), np.float32)
        for hvl in range(4):
            hv = 4 * r + hvl
            for d in range(2):
                s = hvl * 2 + d
                cols = slice(2048 + hv * 128, 2048 + (hv + 1) * 128)
                v_pre[s] = seq(cols, d)
                cw_v[s] = taps(cols, d)
                for arr, ab in ((a_x, 0), (b_x, 1)):
                    col = 6144 + ab * 32 + d * 16 + hv
                    c_ = p_ctx[b][:, col]
                    l_ = p_lat[b][:, col]
                    if d == 1:
                        c_ = c_[::-1]
                        l_ = l_[::-1]
                    arr[:, :, s] = np.concatenate([c_, l_]).reshape(260, 64).T
                alog_x[:, s] = inp["od_a_log"][0, d, hv]
                dtb_x[:, s] = inp["od_dt_bias"][0, d, hv]
        m = {"qk_pre": qk_pre, "v_pre": v_pre, "cw_qk": cw_qk, "cw_v": cw_v, "a_x": a_x, "b_x": b_x,
             "alog_x": alog_x, "dtb_x": dtb_x}
        m.update(consts)
        maps.append(m)
    res = run(build_gdn(), maps)
    o_f = np.zeros((2, 16384, 16, 128), np.float32)
    o_b = np.zeros((2, 16384, 16, 128), np.float32)
    for k in range(NCORES):
        b, r = k // 4, k % 4
        o = res[k]["o"]
        for hvl in range(4):
            hv = 4 * r + hvl
            o_f[b, :, hv] = o[hvl * 2, 256:]
            o_b[b, :, hv] = o[hvl * 2 + 1, 256:][::-1]
    return o_f.reshape(2, 16384, 2048), o_b.reshape(2, 16384, 2048)


def run_post1(inp, mod, x_lat, o_f, o_b, p_lat):
    ident = np.eye(128, dtype=np.float32)
    maps = []
    for k in range(NCORES):
        b, r = k // 4, k % 4
        rs = slice(r * 4096, (r + 1) * 4096)
        m = np.stack([mod[1, b].reshape(6, D), mod[1, 2].reshape(6, D)])
        maps.append({"x": np.ascontiguousarray(x_lat[b, rs]), "of": np.ascontiguousarray(o_f[b, rs]),
                     "ob": np.ascontiguousarray(o_b[b, rs]), "z": np.ascontiguousarray(p_lat[b, rs, 4096:6144]),
                     "sg": inp["od_norm_g"][0], "g2": inp["norm2_g"][1], "mod": np.ascontiguousarray(m),
                     "wout": inp["od_w_out"][0], "wr": inp["moe_w_router"][1], "br": inp["moe_b_router"][1],
                     "w1": inp["moe_w1"][1], "b1": inp["moe_b1"][1], "w2": inp["moe_w2"][1], "b2": inp["moe_b2"][1],
                     "ident": ident, "fg": inp["final_g"]})
    res = run(build_post(1, 32, 32, True, [11, 11, 10]), maps)
    out = np.zeros((2, 16384, D), np.float32)
    for k in range(NCORES):
        b, r = k // 4, k % 4
        out[b, r * 4096:(r + 1) * 4096] = res[k]["xo"]
    return out


def kernel(**inputs):
    inp = {k: np.ascontiguousarray(np.asarray(v, dtype=np.float32)) for k, v in inputs.items()}
    mod = run_mod(inp)
    p_lat, p_ctx = run_l0proj(inp, mod)
    o_lat, o_ctx = run_attn(inp, p_lat, p_ctx)
    f_lat, f_ctx = run_fourier(p_lat, p_ctx)
    del p_lat, p_ctx
    x_lat, x_ctx = run_post0(inp, mod, o_lat, o_ctx, f_lat, f_ctx)
    del o_lat, o_ctx, f_lat, f_ctx
    p1_lat, p1_ctx = run_l1proj(inp, mod, x_lat, x_ctx)
    o_f, o_b = run_gdn(inp, p1_lat, p1_ctx)
    out = run_post1(inp, mod, x_lat, o_f, o_b, p1_lat)
    return out
```

```python
import contextlib
import math
import numpy as np
import concourse.bass as bass
import concourse.mybir as mybir
from concourse.bass_utils import run_bass_kernel_spmd

F32 = mybir.dt.float32
BF16 = mybir.dt.bfloat16
I32 = mybir.dt.int32
AF = mybir.ActivationFunctionType
ALU = mybir.AluOpType
AX = mybir.AxisListType

NCORES = 8
D = 1024


class Buf:
    __slots__ = ("t", "last_w", "reads", "name")

    def __init__(self, t, name=""):
        self.t = t
        self.last_w = None
        self.reads = {}
        self.name = name

    def __getitem__(self, k):
        return self.t[k]


class Ctx:
    NDMA = 12

    def __init__(self, nc):
        self.nc = nc
        self.es = contextlib.ExitStack()
        self.eng = {"pe": nc.tensor, "act": nc.scalar, "dve": nc.vector, "pool": nc.gpsimd, "sp": nc.sync}
        self.sem = {}
        self.cnt = {}
        self.waited = {}
        for e in self.eng:
            self.sem[e] = self.es.enter_context(nc.semaphore("s_" + e))
            self.cnt[e] = 0
            self.waited[e] = {}
        self.dsem = {}
        self.dn = {}
        for q in ("sp", "pool", "act"):
            self.dsem[q] = [self.es.enter_context(nc.semaphore(f"d_{q}{i}")) for i in range(self.NDMA)]
            self.dn[q] = 0
        self.nbuf = 0

    def sb(self, shape, dt, name=None):
        self.nbuf += 1
        name = name or f"sb{self.nbuf}"
        return Buf(self.es.enter_context(self.nc.sbuf_tensor(name, list(shape), dt)), name)

    def ps(self, shape, dt, name=None):
        self.nbuf += 1
        name = name or f"ps{self.nbuf}"
        return Buf(self.es.enter_context(self.nc.psum_tensor(name, list(shape), dt)), name)

    def view(self, buf):
        return Buf(buf.t, buf.name + "_v")

    def push(self):
        self.stack = getattr(self, "stack", [])
        self.stack.append(self.es)
        self.es = contextlib.ExitStack()

    def pop(self):
        self.barrier()
        self.es.close()
        self.es = self.stack.pop()

    def barrier(self):
        evs = []
        for f in ("pe", "act", "dve", "pool"):
            if self.cnt[f]:
                evs.append((f, self.sem[f], self.cnt[f]))
        for q in self.dsem:
            n = self.dn[q]
            for s in range(min(n, self.NDMA)):
                last = ((n - 1 - s) // self.NDMA) * self.NDMA + s
                evs.append((("d", q, s), self.dsem[q][s], 16 * (last // self.NDMA + 1)))
        for e in self.eng:
            for ev in evs:
                if ev[0] != e:
                    self._wait(e, ev)

    def _wait(self, e, ev):
        key, sem, val = ev
        if key == e and e == "pe":
            return
        w = self.waited[e]
        if w.get(key, 0) >= val:
            return
        self.eng[e].wait_ge(sem, val)
        w[key] = val

    def _deps(self, e, reads, writes):
        for b in reads:
            if b.last_w is not None:
                self._wait(e, b.last_w)
        for b in writes:
            if b.last_w is not None:
                self._wait(e, b.last_w)
            for ev in b.reads.values():
                self._wait(e, ev)

    def _mark(self, ev, reads, writes):
        for b in writes:
            b.last_w = ev
            b.reads = {}
        for b in reads:
            if b not in writes:
                b.reads[ev[0]] = ev

    def op(self, e, ins_fn, reads=(), writes=()):
        self._deps(e, reads, writes)
        ins = ins_fn()
        self.cnt[e] += 1
        ins.then_inc(self.sem[e], 1)
        self._mark((e, self.sem[e], self.cnt[e]), reads, writes)

    def dma(self, q, out, in_, reads=(), writes=(), **kw):
        self._deps(q, reads, writes)
        j = self.dn[q]
        P = self.NDMA
        sem = self.dsem[q][j % P]
        key = ("d", q, j % P)
        if j >= P:
            self._wait(q, (key, sem, 16 * (j // P)))
        ins = self.eng[q].dma_start(out=out, in_=in_, **kw)
        ins.then_inc(sem, 16)
        self.dn[q] = j + 1
        self._mark((key, sem, 16 * (j // P + 1)), reads, writes)

    def finish(self):
        self.barrier()
        self.es.close()

    F32R = False

    def mm(self, out, out_ap, lhsT, lhsT_ap, rhs, rhs_ap, start=True, stop=True):
        if self.F32R and lhsT_ap.dtype == F32 and rhs_ap.dtype == F32:
            lhsT_ap = lhsT_ap.bitcast(mybir.dt.float32r)
            rhs_ap = rhs_ap.bitcast(mybir.dt.float32r)
        self.op("pe", lambda: self.nc.tensor.matmul(out_ap, lhsT_ap, rhs_ap, start=start, stop=stop),
                reads=[lhsT, rhs], writes=[out])

    def tr(self, out, out_ap, in_, in_ap, ident):
        k = in_ap.shape[0]
        self.op("pe", lambda: self.nc.tensor.transpose(out_ap, in_ap, ident.t[0:k, 0:k]), reads=[in_, ident], writes=[out])

    def act(self, out, out_ap, in_, in_ap, func, bias=None, scale=None, accum=None, eng="act", extra_reads=()):
        kw = {}
        rd = [in_] + list(extra_reads)
        wr = [out]
        if bias is not None:
            kw["bias"] = bias
        if scale is not None:
            kw["scale"] = scale
        if accum is not None:
            kw["accum_out"] = accum[1]
            wr.append(accum[0])
        self.op("act", lambda: self.nc.scalar.activation(out_ap, in_ap, func, **kw), reads=rd, writes=wr)

    def cp(self, e, out, out_ap, in_, in_ap):
        if e == "act":
            self.op("act", lambda: self.nc.scalar.copy(out_ap, in_ap), reads=[in_], writes=[out])
        else:
            self.op(e, lambda: self.eng[e].tensor_copy(out_ap, in_ap), reads=[in_], writes=[out])

    def tt(self, e, out, out_ap, a, a_ap, b, b_ap, op):
        self.op(e, lambda: self.eng[e].tensor_tensor(out_ap, a_ap, b_ap, op), reads=[a, b], writes=[out])

    def ts(self, e, out, out_ap, a, a_ap, s1, s2, op0, op1=None, extra_reads=()):
        if op1 is None:
            f = lambda: self.eng[e].tensor_scalar(out_ap, a_ap, s1, None, op0)
        else:
            f = lambda: self.eng[e].tensor_scalar(out_ap, a_ap, s1, s2, op0, op1)
        self.op(e, f, reads=[a] + list(extra_reads), writes=[out])

    def stt(self, e, out, out_ap, a, a_ap, scalar, b, b_ap, op0, op1, extra_reads=()):
        self.op(e, lambda: self.eng[e].scalar_tensor_tensor(out_ap, a_ap, scalar, b_ap, op0, op1),
                reads=[a, b] + list(extra_reads), writes=[out])


def new_nc():
    return bass.Bass("TRN2", target_bir_lowering=False)


def dram_in(nc, name, shape, dt=F32):
    return nc.dram_tensor(name, list(shape), dt, kind="ExternalInput").ap()


def dram_out(nc, name, shape, dt=F32):
    return nc.dram_tensor(name, list(shape), dt, kind="ExternalOutput").ap()


def run(nc, in_maps):
    res = run_bass_kernel_spmd(nc, in_maps, core_ids=list(range(NCORES)))
    return res.results


def load_weight_bf16(c, w_ap, K, N, name, nchunk_cols=None):
    kc_n = K // 128
    wb = c.sb([128, kc_n, N], BF16, name)
    cw = min(N, 2048)
    st = [c.sb([128, cw], F32, f"{name}_st{i}") for i in range(2)]
    i = 0
    engs = ["act", "dve", "pool"]
    for kc in range(kc_n):
        for n0 in range(0, N, cw):
            n1 = min(N, n0 + cw)
            s = st[i % 2]
            c.dma("sp", s[:, 0:n1 - n0], w_ap[kc * 128:(kc + 1) * 128, n0:n1], writes=[s])
            c.cp(engs[i % 3], wb, wb[:, kc, n0:n1], s, s[:, 0:n1 - n0])
            i += 1
    return wb


def load_ident(c, ident_ap):
    idf = c.sb([128, 128], F32, "identf")
    idb = c.sb([128, 128], BF16, "identb")
    c.dma("sp", idf[:], ident_ap[:, :], writes=[idf])
    c.cp("dve", idb, idb[:], idf, idf[:])
    return idf, idb


def make_mod_tiles(c, g_ap, mod_ap, seg, i_shift, i_scale, name):
    gb = c.sb([128, D], F32, name + "_G")
    sh = c.sb([128, D], F32, name + "_S")
    tmp = c.sb([128, D], F32, name + "_t")
    c.dma("sp", tmp[:], g_ap.partition_broadcast(128), writes=[tmp])
    c.dma("sp", gb[:], mod_ap[seg, i_scale:i_scale + 1, :].partition_broadcast(128), writes=[gb])
    c.dma("sp", sh[:], mod_ap[seg, i_shift:i_shift + 1, :].partition_broadcast(128), writes=[sh])
    c.stt("dve", gb, gb[:], gb, gb[:], 1.0, tmp, tmp[:], ALU.add, ALU.mult)
    return gb, sh


def build_mod():
    nc = new_nc()
    cT = dram_in(nc, "cT", [D, 3])
    wm = dram_in(nc, "wm", [2, D, 768])
    bm = dram_in(nc, "bm", [2, 768])
    out = dram_out(nc, "mod", [2, 3, 768])
    c = Ctx(nc)
    sT = c.sb([128, 8, 3], F32, "sT")
    c.dma("sp", sT[:], cT.rearrange("(kc p) s -> p kc s", p=128), writes=[sT])
    c.act(sT, sT[:], sT, sT[:], AF.Silu)
    wt = [c.sb([128, 768], F32, f"wt{i}") for i in range(3)]
    pt = [c.ps([128, 512], F32, f"pm{i}") for i in range(4)]
    bt = c.sb([3, 2, 768], F32, "bt")
    for l in range(2):
        c.dma("sp", bt[:, l, :], bm[l:l + 1, :].partition_broadcast(3), writes=[bt])
    ot = c.sb([3, 2, 768], F32, "ot")
    i = 0
    for l in range(2):
        pa, pb = pt[2 * l], pt[2 * l + 1]
        for kc in range(8):
            w = wt[i % 3]
            i += 1
            c.dma("sp", w[:], wm[l, kc * 128:(kc + 1) * 128, :], writes=[w])
            c.mm(pa, pa[0:3, 0:512], sT, sT[:, kc, :], w, w[:, 0:512], start=(kc == 0), stop=(kc == 7))
            c.mm(pb, pb[0:3, 0:256], sT, sT[:, kc, :], w, w[:, 512:768], start=(kc == 0), stop=(kc == 7))
        c.tt("dve", ot, ot[:, l, 0:512], pa, pa[0:3, 0:512], bt, bt[:, l, 0:512], ALU.add)
        c.tt("dve", ot, ot[:, l, 512:768], pb, pb[0:3, 0:256], bt, bt[:, l, 512:768], ALU.add)
    c.dma("sp", out.rearrange("l s n -> s l n"), ot[:], reads=[ot])
    c.finish()
    return nc


def run_mod(inp):
    cT = np.ascontiguousarray(np.concatenate([inp["c"], inp["c_ctx"][None]], 0).T)
    maps = []
    for k in range(NCORES):
        maps.append({"cT": cT,
                     "wm": np.ascontiguousarray(inp["w_mod"][:, :, 768 * k:768 * (k + 1)]),
                     "bm": np.ascontiguousarray(inp["b_mod"][:, 768 * k:768 * (k + 1)])})
    res = run(build_mod(), maps)
    return np.concatenate([r["mod"] for r in res], axis=2)


class NormT:
    def __init__(self, c, idb, eps=1e-6):
        self.c = c
        self.idb = idb
        self.eps = eps
        self.xt = [c.sb([128, D], F32, f"nx{i}") for i in range(2)]
        self.sq = c.sb([128, D], F32, "nsq")
        self.ss = [c.sb([128, 1], F32, f"nss{i}") for i in range(2)]
        self.tmp = c.sb([128, D], F32, "ntmp")
        self.hb = [c.sb([128, D], BF16, f"nhb{i}") for i in range(2)]
        self.pT = [c.ps([128, 8, 128], BF16, f"npT{i}") for i in range(2)]
        self.hT = [c.sb([128, 8, 128], BF16, f"nhT{i}") for i in range(2)]
        self.i = 0

    def run(self, x_ap, G, Sh, xt=None, hf=None):
        c = self.c
        i = self.i
        self.i += 1
        if xt is None:
            xt = self.xt[i % 2]
            c.dma("sp", xt[:], x_ap, writes=[xt])
        ss = self.ss[i % 2]
        c.act(self.sq, self.sq[:], xt, xt[:], AF.Square, accum=(ss, ss[:, 0:1]))
        c.ts("dve", ss, ss[:], ss, ss[:], 1.0 / D, self.eps, ALU.mult, ALU.add)
        c.op("act", lambda: c.nc.scalar.sqrt(ss[:], ss[:]), reads=[ss], writes=[ss])
        c.op("dve", lambda: c.nc.vector.reciprocal(ss[:], ss[:]), reads=[ss], writes=[ss])
        c.stt("dve", self.tmp, self.tmp[:], xt, xt[:], ss[:, 0:1], G, G[:], ALU.mult, ALU.mult, extra_reads=[ss])
        hb = self.hb[i % 2]
        if hf is None:
            c.tt("pool", hb, hb[:], self.tmp, self.tmp[:], Sh, Sh[:], ALU.add)
        else:
            c.tt("pool", hf, hf[:], self.tmp, self.tmp[:], Sh, Sh[:], ALU.add)
            c.cp("pool", hb, hb[:], hf, hf[:])
        pT = self.pT[i % 2]
        for kc in range(8):
            c.tr(pT, pT[:, kc, :], hb, hb[:, kc * 128:(kc + 1) * 128], self.idb)
        hT = self.hT[i % 2]
        c.cp("act", hT, hT[:], pT, pT[:])
        return hT


L0_T = 4224
EVEN_IN = 2560


def build_l0proj(N=EVEN_IN, rope=True):
    nc = new_nc()
    x = dram_in(nc, "x", [L0_T, D])
    g = dram_in(nc, "g", [D])
    mod = dram_in(nc, "mod", [2, 2, D])
    w = dram_in(nc, "w", [D, N])
    if rope:
        cs = dram_in(nc, "cs", [L0_T, 2, 32])
    ident = dram_in(nc, "ident", [128, 128])
    out = dram_out(nc, "p", [L0_T, N])
    c = Ctx(nc)
    idf, idb = load_ident(c, ident)
    wb = load_weight_bf16(c, w, D, N, "wb")
    mods = []
    for s_ in range(2):
        gb = bcast_tile(c, mod[s_, 1:2, :], f"mG{s_}")
        gg = bcast_tile(c, g, f"mg{s_}") if s_ == 0 else mods[0][2]
        sh = bcast_tile(c, mod[s_, 0:1, :], f"mS{s_}")
        c.stt("dve", gb, gb[:], gb, gb[:], 1.0, gg, gg[:], ALU.add, ALU.mult)
        mods.append((gb, sh, gg))
    nt = NormT(c, idb)
    po = [c.ps([128, 512], F32, f"po{i}") for i in range(4)]
    ot = [c.sb([128, N], F32, f"ot{i}") for i in range(2 if N <= 4096 else 1)]
    if rope:
        cst = [c.sb([128, 2, 32], F32, f"cs{i}") for i in range(2)]
        r1 = c.sb([128, 24, 32], F32, "r1")
        r2 = c.sb([128, 24, 32], F32, "r2")
        r3 = c.sb([128, 24, 32], F32, "r3")
        r4 = c.sb([128, 24, 32], F32, "r4")
    nt_tiles = L0_T // 128
    j = 0
    for t in range(nt_tiles):
        G, Sh, _ = mods[0] if t < 32 else mods[1]
        hT = nt.run(x[t * 128:(t + 1) * 128, :], G, Sh)
        o = ot[t % len(ot)]
        if rope:
            cs_t = cst[t % 2]
            c.dma("pool", cs_t[:], cs[t * 128:(t + 1) * 128, :, :], writes=[cs_t])
        for n, n0 in enumerate(range(0, N, 512)):
            n1 = min(N, n0 + 512)
            p = po[j % 4]
            j += 1
            for kc in range(8):
                c.mm(p, p[:, 0:n1 - n0], hT, hT[:, kc, :], wb, wb[:, kc, n0:n1], start=(kc == 0), stop=(kc == 7))
            c.cp("act" if n % 2 == 0 else "dve", o, o[:, n0:n1], p, p[:, 0:n1 - n0])
        if not rope:
            c.dma("sp", out[t * 128:(t + 1) * 128, :], o[:], reads=[o])
            continue
        qk = o[:, 256:1792].rearrange("p (a h e) -> p a h e", a=24, h=2)
        x1 = qk[:, :, 0, :]
        x2 = qk[:, :, 1, :]
        cb = cs_t[:, 0:1, :].broadcast_to([128, 24, 32])
        sb_ = cs_t[:, 1:2, :].broadcast_to([128, 24, 32])
        c.tt("dve", r1, r1[:], o, x1, cs_t, cb, ALU.mult)
        c.tt("pool", r2, r2[:], o, x2, cs_t, sb_, ALU.mult)
        c.tt("dve", r3, r3[:], o, x2, cs_t, cb, ALU.mult)
        c.tt("pool", r4, r4[:], o, x1, cs_t, sb_, ALU.mult)
        c.tt("dve", o, x1, r1, r1[:], r2, r2[:], ALU.subtract)
        c.tt("pool", o, x2, r3, r3[:], r4, r4[:], ALU.add)
        c.dma("sp", out[t * 128:(t + 1) * 128, :], o[:], reads=[o])
    c.finish()
    return nc


def rope_tables():
    t = np.arange(16384)
    row = (t // 64).astype(np.float32)
    col = (t % 64).astype(np.float32)
    inv = (np.float32(10000.0) ** (-np.arange(0, 32, 2, dtype=np.float32) / np.float32(32))).astype(np.float32)
    ang = np.concatenate([row[:, None] * inv, col[:, None] * inv], axis=-1).astype(np.float32)
    return np.cos(ang).astype(np.float32), np.sin(ang).astype(np.float32)


def run_l0proj(inp, mod):
    cos, sin = rope_tables()
    ident = np.eye(128, dtype=np.float32)
    maps = []
    for k in range(NCORES):
        b, r = k // 4, k % 4
        xs = np.zeros((L0_T, D), np.float32)
        xs[:4096] = inp["x"][b, r * 4096:(r + 1) * 4096]
        xs[4096:4160] = inp["ctx"][b, r * 64:(r + 1) * 64]
        cs = np.zeros((L0_T, 2, 32), np.float32)
        cs[:4096, 0] = cos[r * 4096:(r + 1) * 4096]
        cs[:4096, 1] = sin[r * 4096:(r + 1) * 4096]
        cs[4096:, 0] = 1.0
        m = np.stack([mod[0, b].reshape(6, D)[0:2], mod[0, 2].reshape(6, D)[0:2]])
        maps.append({"x": xs, "g": inp["norm1_g"][0], "mod": np.ascontiguousarray(m), "w": inp["ev_w_in"][0],
                     "cs": cs, "ident": ident})
    res = run(build_l0proj(), maps)
    p_lat = np.zeros((2, 16384, EVEN_IN), np.float32)
    p_ctx = np.zeros((2, 256, EVEN_IN), np.float32)
    for k in range(NCORES):
        b, r = k // 4, k % 4
        p_lat[b, r * 4096:(r + 1) * 4096] = res[k]["p"][:4096]
        p_ctx[b, r * 64:(r + 1) * 64] = res[k]["p"][4096:4160]
    return p_lat, p_ctx


NKEY = 16640
NKT = NKEY // 128
NQ = 4160


def lam_tile(c, lam_ap, lam_init, neg=True):
    lt = c.sb([128, 4, 64], F32, "lam_in")
    c.dma("sp", lt[:], lam_ap.rearrange("a d -> (a d)").partition_broadcast(128), writes=[lt])
    pr = c.sb([128, 2, 64], F32, "lam_pr")
    c.tt("dve", pr, pr[:, 0, :], lt, lt[:, 0, :], lt, lt[:, 1, :], ALU.mult)
    c.tt("dve", pr, pr[:, 1, :], lt, lt[:, 2, :], lt, lt[:, 3, :], ALU.mult)
    sm = c.sb([128, 2], F32, "lam_sm")
    c.op("dve", lambda: c.nc.vector.reduce_sum(sm[:], pr[:], AX.X), reads=[pr], writes=[sm])
    c.act(sm, sm[:], sm, sm[:], AF.Exp)
    lam = c.sb([128, 1], F32, "lam_sb")
    c.tt("dve", lam, lam[:], sm, sm[:, 0:1], sm, sm[:, 1:2], ALU.subtract)
    if neg:
        c.ts("dve", lam, lam[:], lam, lam[:], lam_init, -1.0, ALU.add, ALU.mult)
    else:
        c.ts("dve", lam, lam[:], lam, lam[:], lam_init, None, ALU.add)
    return lam


def build_attn(nheads=6, nq=NQ, nkt=NKT, lam_init=0.2, dbg=False):
    nc = new_nc()
    if dbg:
        dbg_out = dram_out(nc, "dbg", [6, 128, 512])
    nkey = nkt * 128
    qT = dram_in(nc, "qT", [nheads, 128, nq])
    kT = dram_in(nc, "kT", [nheads, 128, nkey])
    v = dram_in(nc, "v", [nkey, nheads, 128])
    lam_ap = dram_in(nc, "lam", [4, 64])
    out = dram_out(nc, "oT", [nheads, 128, nq])
    c = Ctx(nc)
    nlam = lam_tile(c, lam_ap, lam_init)
    ones = c.sb([128, 128], BF16, "ones")
    c.op("dve", lambda: nc.vector.memset(ones[:], 1.0), writes=[ones])
    onesf = c.sb([1, 128], F32, "onesf")
    c.op("dve", lambda: nc.vector.memset(onesf[:], 1.0), writes=[onesf])
    KT = c.sb([128, nkey], BF16, "KT")
    V = c.sb([128, nkt, 128], BF16, "V")
    QT = c.sb([128, nq], BF16, "QT")
    CH = 2048
    st = [c.sb([128, CH], F32, f"st{i}") for i in range(2)]
    S = [[c.ps([128, 512], F32, f"S{m}{i}") for i in range(2)] for m in range(2)]
    OT = [c.ps([128, 512], F32, f"OT{m}") for m in range(2)]
    SUM = [c.ps([1, 512], F32, f"SUM{m}") for m in range(2)]
    P = [[c.sb([128, 512], BF16, f"P{m}{i}") for i in range(2)] for m in range(2)]
    rc = [c.sb([1, 512], F32, f"rc{m}") for m in range(2)]
    o0 = c.sb([128, 512], F32, "o0")
    o1 = c.sb([128, 512], F32, "o1")
    ob = [c.sb([128, 512], F32, f"ob{i}") for i in range(2)]
    si = 0
    nblk = 0
    vv = v.rearrange("(t p) h e -> p t h e", p=128)
    for h in range(nheads):
        for (dst, src, n) in ((KT, kT, nkey), (QT, qT, nq)):
            for c0 in range(0, n, CH):
                c1 = min(n, c0 + CH)
                s = st[si % 2]
                c.dma("sp", s[:, 0:c1 - c0], src[h, :, c0:c1], writes=[s])
                c.cp(("dve", "pool")[si % 2], dst, dst[:, c0:c1], s, s[:, 0:c1 - c0])
                si += 1
        for t0 in range(0, nkt, 16):
            t1 = min(nkt, t0 + 16)
            s = st[si % 2]
            sv = s[:, 0:(t1 - t0) * 128].rearrange("p (t e) -> p t e", e=128)
            c.dma("sp", sv, vv[:, t0:t1, h, :], writes=[s])
            c.cp(("dve", "pool")[si % 2], V, V[:, t0:t1, :], s, sv)
            si += 1
        blocks = [(q0, min(512, nq - 64 - q0), 0, nkt) for q0 in range(0, nq - 64, 512)]
        blocks.append((nq - 64, 64, nkt - 2, nkt))
        for (q0, n, kt0, kt1) in blocks:
            for kt in range(kt0, kt1):
                for m in range(2):
                    s_ = S[m][kt % 2]
                    p_ = P[m][kt % 2]
                    c.mm(s_, s_[:, 0:n], KT, KT[m * 64:(m + 1) * 64, kt * 128:(kt + 1) * 128],
                         QT, QT[m * 64:(m + 1) * 64, q0:q0 + n])
                    c.act(p_, p_[:, 0:n], s_, s_[:, 0:n], AF.Exp, scale=0.125)
                    c.mm(OT[m], OT[m][:, 0:n], V, V[:, kt, :], p_, p_[:, 0:n], start=(kt == kt0), stop=(kt == kt1 - 1))
                    c.mm(SUM[m], SUM[m][0:1, 0:n], ones, ones[:, 0:1], p_, p_[:, 0:n], start=(kt == kt0), stop=(kt == kt1 - 1))
            for m in range(2):
                c.op("dve", lambda m=m: nc.vector.reciprocal(rc[m][0:1, 0:n], SUM[m][0:1, 0:n]), reads=[SUM[m]], writes=[rc[m]])
                bc = S[m][0]
                c.mm(bc, bc[:, 0:n], onesf, onesf[0:1, :], rc[m], rc[m][0:1, 0:n])
                dst = o0 if m == 0 else o1
                c.cp("act", dst, dst[:, 0:n], OT[m], OT[m][:, 0:n])
                c.tt("dve", dst, dst[:, 0:n], dst, dst[:, 0:n], bc, bc[:, 0:n], ALU.mult)
            o = ob[nblk % 2]
            nblk += 1
            c.stt("dve", o, o[:, 0:n], o1, o1[:, 0:n], nlam[:, 0:1], o0, o0[:, 0:n], ALU.mult, ALU.add, extra_reads=[nlam])
            c.dma("pool", out[h, :, q0:q0 + n], o[:, 0:n], reads=[o])
    if dbg:
        d = c.sb([128, 512], F32, "dbgt")
        for i, (src, np_) in enumerate(((P[0][0], 128), (P[1][1], 128), (o0, 128), (o1, 128), (rc[0], 1), (nlam, 128))):
            w = 1 if src is nlam else 512
            c.cp("dve", d, d[0:np_, 0:w], src, src[0:np_, 0:w])
            c.dma("sp", dbg_out[i, 0:np_, 0:w], d[0:np_, 0:w], reads=[d], allow_slow_non_contiguous=True)
    c.finish()
    return nc


def lam_init_of(l):
    return 0.8 - 0.6 * math.exp(-0.3 * l)


def run_attn(inp, p_lat, p_ctx):
    maps = []
    for k in range(NCORES):
        b, r = k // 4, k % 4
        pk = np.concatenate([p_lat[b], p_ctx[b]], axis=0)
        pq = np.concatenate([p_lat[b, r * 4096:(r + 1) * 4096], p_ctx[b, r * 64:(r + 1) * 64]], axis=0)
        qT = np.ascontiguousarray(pq[:, 256:1024].reshape(NQ, 6, 128).transpose(1, 2, 0))
        kT = np.ascontiguousarray(pk[:, 1024:1792].reshape(NKEY, 6, 128).transpose(1, 2, 0))
        v = np.ascontiguousarray(pk[:, 1792:2560].reshape(NKEY, 6, 128))
        maps.append({"qT": qT, "kT": kT, "v": v, "lam": inp["ev_lam"][0]})
    res = run(build_attn(lam_init=lam_init_of(0)), maps)
    o_lat = np.zeros((2, 16384, 6, 128), np.float32)
    o_ctx = np.zeros((2, 256, 6, 128), np.float32)
    for k in range(NCORES):
        b, r = k // 4, k % 4
        o = res[k]["oT"].transpose(2, 0, 1)
        o_lat[b, r * 4096:(r + 1) * 4096] = o[:4096]
        o_ctx[b, r * 64:(r + 1) * 64] = o[4096:]
    return o_lat, o_ctx


def fourier_tables():
    c64 = np.arange(64)
    a64 = 2 * np.pi * np.outer(c64, c64) / 64
    cs64 = np.concatenate([np.cos(a64), -np.sin(a64)], 1).astype(np.float32)
    n = np.arange(128)
    a = 2 * np.pi * np.outer(n, n) / 128
    cs128 = np.concatenate([np.cos(a), -np.sin(a)], 1).astype(np.float32)
    sc128 = np.concatenate([np.sin(a), np.cos(a)], 1).astype(np.float32)
    tw = 2 * np.pi * np.outer(n, n) / 16384.0
    twt = np.stack([np.cos(tw), np.sin(tw)], 0).astype(np.float32)
    m = np.arange(256)
    a256 = 2 * np.pi * np.outer(m, m) / 256
    cs256 = np.stack([np.cos(a256), np.sin(a256)], 0).astype(np.float32)
    return {"cs64": cs64, "cs128": cs128, "sc128": sc128, "tw": twt, "cs256": cs256}


def build_fourier():
    nc = new_nc()
    fT = dram_in(nc, "fT", [64, 16384])
    fcT = dram_in(nc, "fcT", [64, 256])
    cs64 = dram_in(nc, "cs64", [64, 128])
    cs128 = dram_in(nc, "cs128", [128, 256])
    sc128 = dram_in(nc, "sc128", [128, 256])
    tw = dram_in(nc, "tw", [2, 128, 128])
    cs256 = dram_in(nc, "cs256", [2, 256, 256])
    out = dram_out(nc, "fo", [128, 64, 128])
    outc = dram_out(nc, "foc", [256, 64])
    c = Ctx(nc)

    def ld_bf(ap, shape, name, q="sp"):
        f = c.sb(shape, F32, name + "_f")
        b = c.sb(shape, BF16, name)
        c.dma(q, f[:], ap, writes=[f])
        c.cp("dve", b, b[:], f, f[:])
        return b
    FT = c.sb([64, 16384], BF16, "FT")
    stg = [c.sb([64, 4096], F32, f"fst{i}") for i in range(2)]
    for i in range(4):
        s = stg[i % 2]
        c.dma("sp", s[:], fT[:, i * 4096:(i + 1) * 4096], writes=[s])
        c.cp(("dve", "pool")[i % 2], FT, FT[:, i * 4096:(i + 1) * 4096], s, s[:])
    CS64 = ld_bf(cs64[:, :], [64, 128], "CS64")
    CS128 = ld_bf(cs128[:, :], [128, 256], "CS128")
    SC128 = ld_bf(sc128[:, :], [128, 256], "SC128")
    TW = c.sb([128, 2, 128], F32, "TW")
    c.dma("sp", TW[:], tw.rearrange("a p k -> p a k"), writes=[TW])
    ps = [c.ps([128, 512], F32, f"fp{i}") for i in range(4)]
    G = c.sb([128, 128, 128], BF16, "G")
    FTv = FT[:, :].rearrange("c (a b) -> c a b", b=128)
    for j in range(32):
        p = ps[j % 4]
        for u in range(4):
            t2 = j * 4 + u
            c.mm(p, p[:, u * 128:(u + 1) * 128], FT, FTv[:, :, t2], CS64, CS64[:, :])
        c.cp(("act", "dve")[j % 2], G, G[:, j * 4:(j + 1) * 4, :].rearrange("p a b -> p (a b)"), p, p[:])
    Br = c.sb([128, 64, 128], BF16, "Br")
    Bi = c.sb([128, 64, 128], BF16, "Bi")
    tmp = [c.sb([128, 2, 256], F32, f"ftmp{i}") for i in range(2)]
    t1_ = [c.sb([128, 2, 128], F32, f"ft1{i}") for i in range(2)]
    t2_ = [c.sb([128, 2, 128], F32, f"ft2{i}") for i in range(2)]
    for j in range(32):
        p = ps[j % 4]
        for u in range(2):
            cp_ = j * 2 + u
            c.mm(p, p[:, u * 256:(u + 1) * 256], G, G[:, :, cp_], CS128, CS128[:, :], start=True, stop=False)
            c.mm(p, p[:, u * 256:(u + 1) * 256], G, G[:, :, 64 + cp_], SC128, SC128[:, :], start=False, stop=True)
        A = tmp[j % 2]
        c.cp("act", A, A[:].rearrange("p a b -> p (a b)"), p, p[:])
        Ar = A[:, :, 0:128]
        Ai = A[:, :, 128:256]
        cw = TW[:, 0:1, :].broadcast_to([128, 2, 128])
        sw = TW[:, 1:2, :].broadcast_to([128, 2, 128])
        a1, a2 = t1_[j % 2], t2_[j % 2]
        c.tt("dve", a1, a1[:], A, Ar, TW, cw, ALU.mult)
        c.tt("pool", a2, a2[:], A, Ai, TW, sw, ALU.mult)
        c.tt("dve", Br, Br[:, j * 2:(j + 1) * 2, :], a1, a1[:], a2, a2[:], ALU.add)
        c.tt("pool", a2, a2[:], A, Ai, TW, cw, ALU.mult)
        c.tt("dve", a1, a1[:], A, Ar, TW, sw, ALU.mult)
        c.tt("pool", Bi, Bi[:, j * 2:(j + 1) * 2, :], a2, a2[:], a1, a1[:], ALU.subtract)
    ot = [c.sb([128, 512], F32, f"fot{i}") for i in range(2)]
    Brv = Br[:].rearrange("p a b -> p (a b)")
    Biv = Bi[:].rearrange("p a b -> p (a b)")
    outv = out.rearrange("k c j -> k (c j)")
    for j in range(16):
        p = ps[j % 4]
        c.mm(p, p[:], CS128, CS128[:, 0:128], Br, Brv[:, j * 512:(j + 1) * 512], start=True, stop=False)
        c.mm(p, p[:], SC128, SC128[:, 0:128], Bi, Biv[:, j * 512:(j + 1) * 512], start=False, stop=True)
        o = ot[j % 2]
        c.act(o, o[:], p, p[:], AF.Copy, scale=1.0 / 1024.0)
        c.dma("sp", outv[:, j * 512:(j + 1) * 512], o[:], reads=[o])
    FC = ld_bf(fcT[:, :], [64, 256], "FC")
    C256 = ld_bf(cs256.rearrange("a (kc p) k -> p a kc k", p=128), [128, 2, 2, 256], "C256")
    Gc = c.sb([128, 2, 128], BF16, "Gc")
    p = ps[0]
    for tcn in range(2):
        c.mm(p, p[:, tcn * 128:(tcn + 1) * 128], FC, FC[:, tcn * 128:(tcn + 1) * 128], CS64, CS64[:, :])
    c.cp("act", Gc, Gc[:].rearrange("p a b -> p (a b)"), p, p[:, 0:256])
    oc = c.sb([128, 2, 64], F32, "foc_t")
    p = ps[1]
    for kc in range(2):
        n = 0
        for tcn in range(2):
            c.mm(p, p[:, kc * 64:(kc + 1) * 64], C256, C256[:, 0, tcn, kc * 128:(kc + 1) * 128], Gc, Gc[:, tcn, 0:64], start=(n == 0), stop=False)
            n += 1
            c.mm(p, p[:, kc * 64:(kc + 1) * 64], C256, C256[:, 1, tcn, kc * 128:(kc + 1) * 128], Gc, Gc[:, tcn, 64:128], start=False, stop=(tcn == 1))
    c.act(oc, oc[:].rearrange("p a b -> p (a b)"), p, p[:, 0:128], AF.Copy, scale=1.0 / 128.0)
    c.dma("sp", outc.rearrange("(kc p) c -> p kc c", p=128), oc[:], reads=[oc])
    c.finish()
    return nc


def run_fourier(p_lat, p_ctx):
    tabs = fourier_tables()
    maps = []
    for k in range(NCORES):
        b, g = k // 4, k % 4
        m = dict(tabs)
        m["fT"] = np.ascontiguousarray(p_lat[b, :, g * 64:(g + 1) * 64].T)
        m["fcT"] = np.ascontiguousarray(p_ctx[b, :, g * 64:(g + 1) * 64].T)
        maps.append(m)
    res = run(build_fourier(), maps)
    fl = np.zeros((2, 16384, 256), np.float32)
    fc = np.zeros((2, 256, 256), np.float32)
    for k in range(NCORES):
        b, g = k // 4, k % 4
        fo = res[k]["fo"]
        fl[b, :, g * 64:(g + 1) * 64] = fo.transpose(0, 2, 1).reshape(16384, 64)
        fc[b, :, g * 64:(g + 1) * 64] = res[k]["foc"]
    return fl, fc


class DB:
    def __init__(self):
        self.last_w = None
        self.reads = {}


def bcast_tile(c, ap_row, name, n=D, q="sp"):
    t = c.sb([128, n], F32, name)
    c.dma(q, t[:], ap_row.partition_broadcast(128), writes=[t])
    return t


def build_post(layer, ntiles, nlat, last, passes, lam_init=0.2):
    nc = new_nc()
    T = ntiles * 128
    Kmix = 1024 if layer == 0 else 2048
    KC = Kmix // 128
    x = dram_in(nc, "x", [T, D])
    if layer == 0:
        o_in = dram_in(nc, "o", [T, 768])
        f_in = dram_in(nc, "fo", [T, 256])
    else:
        of_in = dram_in(nc, "of", [T, 2048])
        ob_in = dram_in(nc, "ob", [T, 2048])
        z_in = dram_in(nc, "z", [T, 2048])
    sg = dram_in(nc, "sg", [128])
    g2 = dram_in(nc, "g2", [D])
    mod = dram_in(nc, "mod", [2, 6, D])
    wout = dram_in(nc, "wout", [Kmix, D])
    wr = dram_in(nc, "wr", [D, 32])
    br = dram_in(nc, "br", [32])
    w1 = dram_in(nc, "w1", [32, D, 2048])
    b1 = dram_in(nc, "b1", [32, 2048])
    w2 = dram_in(nc, "w2", [32, D, D])
    b2 = dram_in(nc, "b2", [32, D])
    ident = dram_in(nc, "ident", [128, 128])
    if last:
        fg = dram_in(nc, "fg", [D])
    xo = dram_out(nc, "xo", [T, D])
    h2s = nc.dram_tensor("h2s", [128, 8, T], BF16, kind="Internal").ap()
    c = Ctx(nc)
    idf, idb = load_ident(c, ident)
    gates = c.sb([128, ntiles, 32], F32, "gates")
    xo_db = [DB() for _ in range(ntiles)]
    h2_db = [DB() for _ in range(ntiles)]
    nseg = 1 if nlat == ntiles else 2

    c.push()
    woutb = load_weight_bf16(c, wout, Kmix, D, "woutb")
    wrf = c.sb([128, 8, 32], F32, "wrf")
    c.dma("sp", wrf[:], wr.rearrange("(kc p) n -> p kc n", p=128), writes=[wrf])
    brt = bcast_tile(c, br, "brt", 32)
    sgt = bcast_tile(c, sg, "sgt", 128)
    if layer == 0:
        c.ts("dve", sgt, sgt[:], sgt, sgt[:], 1.0 - lam_init, None, ALU.mult)
    mods2 = [make_mod_tiles(c, g2, mod, s_, 3, 4, f"m2{s_}") for s_ in range(nseg)]
    gate_m = [bcast_tile(c, mod[s_, 2:3, :], f"gm{s_}") for s_ in range(nseg)]
    nt = NormT(c, idb)
    mixp = [c.ps([128, 8, 128], BF16, "mixp0")]
    yp = [c.ps([128, 512], F32, f"yp{i}") for i in range(2)]
    hfp = [c.ps([128, 4, 128], F32, f"hfp{i}") for i in range(2)]
    lgp = c.ps([128, 32], F32, "lgp")
    mixed = c.sb([128, Kmix], BF16, "mixed")
    mixT = c.sb([128, KC, 128], BF16, "mixT")
    x1 = [c.sb([128, D], F32, f"x1_{i}") for i in range(2)]
    hf = c.sb([128, D], F32, "hf")
    hTf = c.sb([128, 8, 128], F32, "hTf")
    lg = c.sb([128, 32], F32, "lg")
    mx8 = c.sb([128, 8], F32, "mx8")
    msk = c.sb([128, 32], F32, "msk")
    ex = c.sb([128, 32], F32, "ex")
    sm = c.sb([128, 1], F32, "gsm")
    ytmp = c.sb([128, D], F32, "ytmp")
    if layer == 0:
        ot = [c.sb([128, 768], F32, f"o_t{i}") for i in range(2)]
        ft = [c.sb([128, 256], F32, f"f_t{i}") for i in range(2)]
        osq = c.sb([128, 6, 128], F32, "osq")
        oss = c.sb([128, 6], F32, "oss")
        nh_, eps_h = 6, 1e-5
    else:
        oft = [c.sb([128, 2048], F32, f"of_t{i}") for i in range(2)]
        obt = [c.sb([128, 2048], F32, f"ob_t{i}") for i in range(2)]
        zt = [c.sb([128, 2048], F32, f"z_t{i}") for i in range(2)]
        osq = c.sb([128, 16, 128], F32, "osq")
        oss = c.sb([128, 16], F32, "oss")
        nh_, eps_h = 16, 1e-6
    for t in range(ntiles):
        seg = 0 if t < nlat else 1
        rows = slice(t * 128, (t + 1) * 128)
        xt = nt.xt[t % 2]
        c.dma("sp", xt[:], x[rows, :], writes=[xt])
        if layer == 0:
            o_ = ot[t % 2]
            f_ = ft[t % 2]
            c.dma("pool", o_[:], o_in[rows, :], writes=[o_])
            c.dma("pool", f_[:], f_in[rows, :], writes=[f_])
            src = o_
            c.cp("act", mixed, mixed[:, 0:256], f_, f_[:])
            moff = 256
        else:
            a_, b_, z_ = oft[t % 2], obt[t % 2], zt[t % 2]
            c.dma("pool", a_[:], of_in[rows, :], writes=[a_])
            c.dma("pool", b_[:], ob_in[rows, :], writes=[b_])
            c.dma("sp", z_[:], z_in[rows, :], writes=[z_])
            c.tt("pool", a_, a_[:], a_, a_[:], b_, b_[:], ALU.add)
            c.act(z_, z_[:], z_, z_[:], AF.Silu)
            src = a_
            moff = 0
        sv_ = src[:, :].rearrange("p (h e) -> p h e", e=128)
        c.tt("pool", osq, osq[:], src, sv_, src, sv_, ALU.mult)
        c.op("dve", lambda: nc.vector.reduce_sum(oss[:], osq[:], AX.X), reads=[osq], writes=[oss])
        c.ts("dve", oss, oss[:], oss, oss[:], 1.0 / 128, eps_h, ALU.mult, ALU.add)
        c.op("act", lambda: nc.scalar.sqrt(oss[:], oss[:]), reads=[oss], writes=[oss])
        c.op("dve", lambda: nc.vector.reciprocal(oss[:], oss[:]), reads=[oss], writes=[oss])
        c.tt("dve", osq, osq[:], src, sv_, oss, oss[:].unsqueeze(2).broadcast_to([128, nh_, 128]), ALU.mult)
        mv = mixed[:, moff:Kmix].rearrange("p (h e) -> p h e", e=128)
        sgb = sgt[:].unsqueeze(1).broadcast_to([128, nh_, 128])
        if layer == 0:
            c.tt("pool", mixed, mv, osq, osq[:], sgt, sgb, ALU.mult)
        else:
            c.tt("pool", osq, osq[:], osq, osq[:], sgt, sgb, ALU.mult)
            c.tt("dve", mixed, mv, osq, osq[:], z_, z_[:, :].rearrange("p (h e) -> p h e", e=128), ALU.mult)
        for i_ in range(KC // 8):
            mp = mixp[0]
            for kc in range(i_ * 8, (i_ + 1) * 8):
                c.tr(mp, mp[:, kc % 8, :], mixed, mixed[:, kc * 128:(kc + 1) * 128], idb)
            c.cp(("act", "dve")[i_ % 2], mixT, mixT[:, i_ * 8:(i_ + 1) * 8, :], mp, mp[:])
        x1t = x1[t % 2]
        for n in range(2):
            for kc in range(KC):
                c.mm(yp[n], yp[n][:], mixT, mixT[:, kc, :], woutb, woutb[:, kc, n * 512:(n + 1) * 512],
                     start=(kc == 0), stop=(kc == KC - 1))
            cs_ = slice(n * 512, (n + 1) * 512)
            c.tt("dve", ytmp, ytmp[:, cs_], yp[n], yp[n][:], gate_m[seg], gate_m[seg][:, cs_], ALU.mult)
            c.tt("pool", x1t, x1t[:, cs_], ytmp, ytmp[:, cs_], xt, xt[:, cs_], ALU.add)
        c.dma("sp", xo[rows, :], x1t[:], reads=[x1t], writes=[xo_db[t]])
        G2, S2 = mods2[seg]
        hT = nt.run(None, G2, S2, xt=x1t, hf=hf)
        c.dma("pool", h2s[:, :, rows], hT[:], reads=[hT], writes=[h2_db[t]])
        for kc in range(8):
            hp = hfp[kc // 4]
            c.tr(hp, hp[:, kc % 4, :], hf, hf[:, kc * 128:(kc + 1) * 128], idf)
        for i_ in range(2):
            c.cp(("act", "dve")[i_], hTf, hTf[:, i_ * 4:(i_ + 1) * 4, :], hfp[i_], hfp[i_][:])
        for kc in range(8):
            c.mm(lgp, lgp[:], hTf, hTf[:, kc, :], wrf, wrf[:, kc, :], start=(kc == 0), stop=(kc == 7))
        c.tt("dve", lg, lg[:], lgp, lgp[:], brt, brt[:], ALU.add)
        c.op("dve", lambda: nc.vector.max(out=mx8[:], in_=lg[:]), reads=[lg], writes=[mx8])
        c.ts("dve", msk, msk[:], lg, lg[:], mx8[:, 3:4], None, ALU.is_ge, extra_reads=[mx8])
        c.ts("dve", mx8, mx8[:, 0:1], mx8, mx8[:, 0:1], -1.0, None, ALU.mult)
        c.act(ex, ex[:], lg, lg[:], AF.Exp, bias=mx8[:, 0:1], extra_reads=[mx8])
        c.tt("dve", ex, ex[:], ex, ex[:], msk, msk[:], ALU.mult)
        c.op("dve", lambda: nc.vector.reduce_sum(sm[:], ex[:], AX.X), reads=[ex], writes=[sm])
        c.op("dve", lambda: nc.vector.reciprocal(sm[:], sm[:]), reads=[sm], writes=[sm])
        c.ts("dve", gates, gates[:, t, :], ex, ex[:], sm[:, 0:1], None, ALU.mult, extra_reads=[sm])
    c.pop()

    c.push()
    maxp = max(passes)
    H2T = c.sb([128, 8, maxp * 128], BF16, "H2T")
    acc = c.sb([128, maxp, D], F32, "acc")
    w1g = c.sb([128, 8, 1024], BF16, "w1g")
    w1l = c.sb([128, 8, 1024], BF16, "w1l")
    w2b = [c.sb([128, 8, 1024], BF16, f"w2b{i}") for i in range(2)]
    stg = [c.sb([128, 2048], F32, f"wst{i}") for i in range(2)]
    b1t = [c.sb([128, 8, 2], F32, f"b1t{i}") for i in range(2)]
    b2f = c.sb([1, D], F32, "b2f")
    b2b = [c.sb([1, D], BF16, f"b2b{i}") for i in range(2)]
    onesb = c.sb([1, 128], BF16, "onesb")
    c.op("dve", lambda: nc.vector.memset(onesb[:], 1.0), writes=[onesb])
    c119 = c.sb([128, 1], F32, "c119")
    c.op("dve", lambda: nc.vector.memset(c119[:], 1.702 * 7.0), writes=[c119])
    c14 = c.sb([128, 1], F32, "c14")
    c.op("dve", lambda: nc.vector.memset(c14[:], 14.0), writes=[c14])
    aT = c.sb([128, 8, maxp * 128], BF16, "aT")
    glu = [c.sb([128, 512], F32, f"glu{i}") for i in range(2)]
    sig = [c.sb([128, 512], F32, "sig0")] * 2
    lin = [c.sb([128, 512], F32, f"lin{i}") for i in range(2)]
    ugp = [c.ps([128, 512], F32, f"ugp{i}") for i in range(2)]
    ulp = [c.ps([128, 512], F32, f"ulp{i}") for i in range(2)]
    ypp = [c.ps([128, 512], F32, f"ypp{i}") for i in range(4)]
    gate5 = [bcast_tile(c, mod[s_, 5:6, :], f"g5{s_}") for s_ in range(nseg)]
    xr = [c.sb([128, D], F32, "xr0")] * 2
    if last:
        fgt = bcast_tile(c, fg, "fgt")
        fsq = c.sb([128, D], F32, "fsq")
        fss = c.sb([128, 1], F32, "fss")
    si = 0
    jj = 0
    yi = 0
    ei = 0
    tbase = 0
    for npass in passes:
        tiles = list(range(tbase, tbase + npass))
        ntok = npass * 128
        c.dma("sp", H2T[:, :, 0:ntok], h2s[:, :, tbase * 128:tbase * 128 + ntok], reads=[h2_db[t] for t in tiles], writes=[H2T])
        c.op("pool", lambda: nc.gpsimd.memset(acc[:], 0.0), writes=[acc])
        for e in range(32):
            w2e = w2b[ei % 2]
            b1e = b1t[ei % 2]
            b2e = b2b[ei % 2]
            ei += 1
            for kc in range(8):
                s_ = stg[si % 2]
                c.dma("sp", s_[:], w1[e, kc * 128:(kc + 1) * 128, :], writes=[s_])
                sv2 = s_[:, :].rearrange("p (n two) -> p n two", two=2)
                c.cp("act", w1g, w1g[:, kc, :], s_, sv2[:, :, 0])
                c.cp("dve", w1l, w1l[:, kc, :], s_, sv2[:, :, 1])
                si += 1
            c.dma("sp", b1e[:], b1[e].rearrange("(c p two) -> p c two", p=128, two=2), writes=[b1e])
            c.ts("dve", b1e, b1e[:, :, 0:1], b1e, b1e[:, :, 0:1], -1.0, 7.0, ALU.mult, ALU.add)
            c.ts("dve", b1e, b1e[:, :, 1:2], b1e, b1e[:, :, 1:2], 7.0, None, ALU.add)
            c.dma("sp", b2f[:], b2[e:e + 1, :], writes=[b2f])
            c.cp("dve", b2e, b2e[:], b2f, b2f[:])
            for kc2 in range(4):
                s_ = stg[si % 2]
                c.dma("sp", s_[:].rearrange("p (a n) -> p a n", a=2),
                      w2[e, kc2 * 256:(kc2 + 1) * 256, :].rearrange("(a p) n -> p a n", p=128), writes=[s_])
                c.cp(("act", "dve")[kc2 % 2], w2e, w2e[:, kc2 * 2:kc2 * 2 + 2, :].rearrange("p a n -> p (a n)"), s_, s_[:])
                si += 1
            for blk0 in range(0, npass, 4):
                nb = min(4, npass - blk0)
                n = nb * 128
                tok0 = blk0 * 128
                for j in range(8):
                    ug, ul = ugp[jj % 2], ulp[jj % 2]
                    gl, sg_, ln = glu[jj % 2], sig[jj % 2], lin[jj % 2]
                    jj += 1
                    for kc in range(8):
                        c.mm(ug, ug[:, 0:n], w1g, w1g[:, kc, j * 128:(j + 1) * 128], H2T, H2T[:, kc, tok0:tok0 + n],
                             start=(kc == 0), stop=(kc == 7))
                    for kc in range(8):
                        c.mm(ul, ul[:, 0:n], w1l, w1l[:, kc, j * 128:(j + 1) * 128], H2T, H2T[:, kc, tok0:tok0 + n],
                             start=(kc == 0), stop=(kc == 7))
                    c.act(gl, gl[:, 0:n], ug, ug[:, 0:n], AF.Relu, bias=b1e[:, j, 0:1], scale=-1.0, extra_reads=[b1e])
                    c.act(sg_, sg_[:, 0:n], gl, gl[:, 0:n], AF.Sigmoid, bias=c119[:, 0:1], scale=-1.702, extra_reads=[c119])
                    c.act(ln, ln[:, 0:n], ul, ul[:, 0:n], AF.Relu, bias=b1e[:, j, 1:2], scale=1.0, extra_reads=[b1e])
                    c.act(ln, ln[:, 0:n], ln, ln[:, 0:n], AF.Relu, bias=c14[:, 0:1], scale=-1.0, extra_reads=[c14])
                    c.stt("dve", gl, gl[:, 0:n], gl, gl[:, 0:n], 7.0, sg_, sg_[:, 0:n], ALU.subtract, ALU.mult)
                    c.stt("dve", aT, aT[:, j, tok0:tok0 + n], ln, ln[:, 0:n], 8.0, gl, gl[:, 0:n], ALU.subtract, ALU.mult)
            for tl in range(npass):
                for nh in range(2):
                    y_ = ypp[yi % 4]
                    yi += 1
                    cs_ = slice(nh * 512, (nh + 1) * 512)
                    c.mm(y_, y_[:], onesb, onesb[0:1, :], b2e, b2e[0:1, cs_], start=True, stop=False)
                    for j in range(8):
                        c.mm(y_, y_[:], aT, aT[:, j, tl * 128:(tl + 1) * 128], w2e, w2e[:, j, cs_],
                             start=False, stop=(j == 7))
                    c.stt("dve", acc, acc[:, tl, cs_], y_, y_[:], gates[:, tbase + tl, e:e + 1], acc, acc[:, tl, cs_],
                          ALU.mult, ALU.add, extra_reads=[gates])
        for tl, t in enumerate(tiles):
            seg = 0 if t < nlat else 1
            rows = slice(t * 128, (t + 1) * 128)
            xr_ = xr[t % 2]
            c.dma("sp", xr_[:], xo[rows, :], reads=[xo_db[t]], writes=[xr_])
            c.tt("pool", acc, acc[:, tl, :], acc, acc[:, tl, :], gate5[seg], gate5[seg][:], ALU.mult)
            c.tt("dve", xr_, xr_[:], xr_, xr_[:], acc, acc[:, tl, :], ALU.add)
            if last:
                c.act(fsq, fsq[:], xr_, xr_[:], AF.Square, accum=(fss, fss[:, 0:1]))
                c.ts("dve", fss, fss[:], fss, fss[:], 1.0 / D, 1e-6, ALU.mult, ALU.add)
                c.op("act", lambda: nc.scalar.sqrt(fss[:], fss[:]), reads=[fss], writes=[fss])
                c.op("dve", lambda: nc.vector.reciprocal(fss[:], fss[:]), reads=[fss], writes=[fss])
                c.stt("dve", xr_, xr_[:], xr_, xr_[:], fss[:, 0:1], fgt, fgt[:], ALU.mult, ALU.mult, extra_reads=[fss])
            c.dma("sp", xo[rows, :], xr_[:], reads=[xr_], writes=[xo_db[t]])
        tbase += npass
    c.pop()
    c.finish()
    return nc


CH = 64
GDN_F32R = False


def gdn_consts():
    i = np.arange(64)
    tri = (i[:, None] <= i[None, :]).astype(np.float32)
    sel = np.zeros((64, 128), np.float32)
    sel[63, :] = 1.0
    mincl = (i[:, None] >= i[None, :]).astype(np.float32)
    negm = (mincl - 1.0) * 30000.0
    mstrict = (i[:, None] > i[None, :]).astype(np.float32)
    return {"tri": tri, "sel": sel, "mincl": mincl, "negm": negm.astype(np.float32), "mstrict": mstrict,
            "identf": np.eye(128, dtype=np.float32)}


def build_gdn(nlat_groups=32, gch=8, nseq=8):
    nc = new_nc()
    nctx_ch = 4
    nchunks = nctx_ch + nlat_groups * gch
    ntok = nchunks * CH
    nlat = nlat_groups * gch * CH
    TP = 2 + 256 + 2 + 2 + nlat + 2
    NX = nchunks * nseq
    nqk = max(2, nseq // 2) if nseq > 1 else 1
    qk_pre = dram_in(nc, "qk_pre", [nqk, 2, 128, TP])
    v_pre = dram_in(nc, "v_pre", [nseq, 128, TP])
    cw_qk = dram_in(nc, "cw_qk", [nqk, 2, 128, 5])
    cw_v = dram_in(nc, "cw_v", [nseq, 128, 5])
    a_x = dram_in(nc, "a_x", [64, nchunks, nseq])
    b_x = dram_in(nc, "b_x", [64, nchunks, nseq])
    alog_x = dram_in(nc, "alog_x", [64, nseq])
    dtb_x = dram_in(nc, "dtb_x", [64, nseq])
    cst = {k: dram_in(nc, k, list(v.shape)) for k, v in gdn_consts().items()}
    o_out = dram_out(nc, "o", [nseq, ntok, 128])
    gts = nc.dram_tensor("gts", [NX, 64], F32, kind="Internal").ap()
    gts_db = DB()
    c = Ctx(nc)
    c.F32R = GDN_F32R

    def ldc(name, shape):
        t = c.sb(shape, F32, name + "_c")
        c.dma("sp", t[:], cst[name][:, :], writes=[t])
        return t
    tri = ldc("tri", [64, 64])
    sel = ldc("sel", [64, 128])
    mincl = ldc("mincl", [64, 64])
    negm = ldc("negm", [64, 64])
    mstrict = ldc("mstrict", [64, 64])
    idf = ldc("identf", [128, 128])
    ones = c.sb([128, 128], F32, "ones")
    c.op("dve", lambda: nc.vector.memset(ones[:], 1.0), writes=[ones])
    pb = [c.ps([128, 512], F32, f"pb{i}") for i in range(8)]
    pi = [0]

    def nextp():
        p = pb[pi[0] % 8]
        pi[0] += 1
        return p

    c.push()
    ax = c.sb([64, NX], F32, "ax")
    bx = c.sb([64, NX], F32, "bx")
    c.dma("sp", ax[:], a_x.rearrange("p c s -> p (c s)"), writes=[ax])
    c.dma("sp", bx[:], b_x.rearrange("p c s -> p (c s)"), writes=[bx])
    al = c.sb([64, nseq], F32, "al")
    dtb = c.sb([64, nseq], F32, "dtb")
    c.dma("sp", al[:], alog_x[:, :], writes=[al])
    c.dma("sp", dtb[:], dtb_x[:, :], writes=[dtb])
    c.act(al, al[:], al, al[:], AF.Exp)
    c.ts("dve", al, al[:], al, al[:], -1.0, None, ALU.mult)

    def xv(t):
        return t[:, :].rearrange("p (c s) -> p c s", s=nseq)
    bc8 = lambda t: t[:, :].unsqueeze(1).broadcast_to([64, nchunks, nseq])
    c.tt("dve", ax, xv(ax), ax, xv(ax), dtb, bc8(dtb), ALU.add)
    c.act(ax, ax[:], ax, ax[:], AF.Exp)
    c.act(ax, ax[:], ax, ax[:], AF.Ln, bias=1.0)
    c.tt("dve", ax, xv(ax), ax, xv(ax), al, bc8(al), ALU.mult)
    c.pop()
    beta = c.sb([64, NX], F32, "beta")
    G = c.sb([64, NX], F32, "G")
    eG = c.sb([64, NX], F32, "eG")
    bk = c.sb([64, NX], F32, "bk")
    kts = c.sb([64, NX], F32, "kts")
    eGe = c.sb([128, NX], F32, "eGe")
    c.push()
    gx = c.sb([64, NX], F32, "gx")
    bx2 = c.sb([64, NX], F32, "bx2")
    c.dma("sp", gx[:], a_x.rearrange("p c s -> p (c s)"), writes=[gx])
    c.dma("sp", bx2[:], b_x.rearrange("p c s -> p (c s)"), writes=[bx2])
    al2 = c.sb([64, nseq], F32, "al2")
    dtb2 = c.sb([64, nseq], F32, "dtb2")
    c.dma("sp", al2[:], alog_x[:, :], writes=[al2])
    c.dma("sp", dtb2[:], dtb_x[:, :], writes=[dtb2])
    c.act(al2, al2[:], al2, al2[:], AF.Exp)
    c.ts("dve", al2, al2[:], al2, al2[:], -1.0, None, ALU.mult)
    c.tt("dve", gx, xv(gx), gx, xv(gx), dtb2, bc8(dtb2), ALU.add)
    c.act(gx, gx[:], gx, gx[:], AF.Exp)
    c.act(gx, gx[:], gx, gx[:], AF.Ln, bias=1.0)
    c.tt("dve", gx, xv(gx), gx, xv(gx), al2, bc8(al2), ALU.mult)
    c.act(beta, beta[:], bx2, bx2[:], AF.Sigmoid)
    Ge = c.sb([128, NX], F32, "Ge")
    for n0 in range(0, NX, 512):
        n1 = min(NX, n0 + 512)
        p = nextp()
        c.mm(p, p[0:64, 0:n1 - n0], tri, tri[:, :], gx, gx[:, n0:n1])
        c.cp("act", G, G[:, n0:n1], p, p[0:64, 0:n1 - n0])
        p2 = nextp()
        c.mm(p2, p2[:, 0:n1 - n0], sel, sel[:, :], G, G[:, n0:n1])
        c.cp("dve", Ge, Ge[:, n0:n1], p2, p2[:, 0:n1 - n0])
    c.act(eG, eG[:], G, G[:], AF.Exp)
    c.act(eGe, eGe[:], Ge, Ge[:], AF.Exp)
    c.tt("dve", kts, kts[:], Ge, Ge[0:64, :], G, G[:], ALU.subtract)
    c.act(kts, kts[:], kts, kts[:], AF.Exp)
    c.tt("dve", bk, bk[:], beta, beta[:], eG, eG[:], ALU.mult)
    gtt = [c.sb([128, 64], F32, f"gtt{i}") for i in range(2)]
    for i_, n0 in enumerate(range(0, NX, 128)):
        n1 = min(NX, n0 + 128)
        p = nextp()
        c.tr(p, p[0:n1 - n0, 0:64], G, G[:, n0:n1], idf_v := c.view(idf))
        g_ = gtt[i_ % 2]
        c.cp("act", g_, g_[0:n1 - n0, :], p, p[0:n1 - n0, 0:64])
        c.dma("sp", gts[n0:n1, :], g_[0:n1 - n0, :], reads=[g_], writes=[gts_db])
    c.pop()

    NT = gch * CH
    S = [c.sb([128, 128], F32, f"S{s}") for s in range(nseq)]
    for s in range(nseq):
        c.op("pool", lambda s=s: nc.gpsimd.memset(S[s][:], 0.0), writes=[S[s]])
    NSLOT = 2

    def mkslot(k):
        B = {}
        B["xin"] = [c.sb([128, NT + 4], F32, f"xin{k}_{i}") for i in range(2)]
        B["cwt"] = c.sb([128, 3, 5], F32, f"cwt{k}")
        for nm in ("qT", "kT", "vT", "sq", "rs"):
            B[nm] = c.sb([128, NT], F32, f"{nm}{k}")
        B["ktail"] = c.sb([64, gch, 128], F32, f"ktail{k}")
        B["R"] = c.sb([64, gch, 256], F32, f"R{k}")
        for nm in ("L", "LT", "L2", "LT2", "intraT", "dec", "tmp64"):
            B[nm] = c.sb([64, gch, 64], F32, f"{nm}{k}")
        B["wT"] = c.sb([128, gch, 64], F32, f"wT{k}")
        B["grow"] = c.sb([1, NT], F32, f"grow{k}")
        B["ngrow"] = c.sb([1, NT], F32, f"ngrow{k}")
        B["vnew"] = [c.sb([64, 128], F32, f"vnew{k}_{i}") for i in range(2)]
        B["o2s"] = [c.sb([64, 128], F32, f"o2s{k}_{i}") for i in range(2)]
        B["osb"] = [c.sb([64, gch, 128], F32, f"osb{k}_{i}") for i in range(2)]
        B["gi"] = 0
        B["xi"] = 0
        return B
    slots = [mkslot(k) for k in range(NSLOT)]
    gts_v = gts.rearrange("(c s) j -> s c j", s=nseq)
    groups = [(0, nctx_ch, 0)] + [(nctx_ch + g * gch, gch, 260 + g * NT) for g in range(nlat_groups)]

    def seq_body(s, c0, ncg, col0, SL):
        n = ncg * CH
        xin, cwt, qT, kT, vT, sq, rs = SL['xin'], SL['cwt'], SL['qT'], SL['kT'], SL['vT'], SL['sq'], SL['rs']
        ktail, R, L, LT, L2, LT2 = SL['ktail'], SL['R'], SL['L'], SL['LT'], SL['L2'], SL['LT2']
        intraT, dec, tmp64, wT, grow, ngrow = SL['intraT'], SL['dec'], SL['tmp64'], SL['wT'], SL['grow'], SL['ngrow']
        vnew, o2s, osb = SL['vnew'], SL['o2s'], SL['osb']
        qki = s // 4 * 2 + (s % 2) if nseq > 1 else 0
        c.dma("pool", cwt[:, 0, :], cw_qk[qki, 0, :, :], writes=[cwt])
        c.dma("pool", cwt[:, 1, :], cw_qk[qki, 1, :, :], writes=[cwt])
        c.dma("pool", cwt[:, 2, :], cw_v[s, :, :], writes=[cwt])
        for wi, (src_ap, dst) in enumerate(((qk_pre[qki, 0], qT), (qk_pre[qki, 1], kT), (v_pre[s], vT))):
            xi_ = xin[SL['xi'] % 2]
            SL['xi'] += 1
            c.dma("sp", xi_[:, 0:n + 4], src_ap[:, col0:col0 + n + 4], writes=[xi_])
            c.ts("dve", dst, dst[:, 0:n], xi_, xi_[:, 0:n], cwt[:, wi, 0:1], None, ALU.mult, extra_reads=[cwt])
            for j in range(1, 5):
                c.stt("dve", dst, dst[:, 0:n], xi_, xi_[:, j:j + n], cwt[:, wi, j:j + 1], dst, dst[:, 0:n],
                      ALU.mult, ALU.add, extra_reads=[cwt])
            c.act(dst, dst[:, 0:n], dst, dst[:, 0:n], AF.Silu)
            yield
            if wi < 2:
                c.tt("pool", sq, sq[:, 0:n], dst, dst[:, 0:n], dst, dst[:, 0:n], ALU.mult)
                for n0 in range(0, n, 512):
                    n1 = min(n, n0 + 512)
                    p = nextp()
                    c.mm(p, p[:, 0:n1 - n0], ones, ones[:, :], sq, sq[:, n0:n1])
                    c.ts("dve", rs, rs[:, n0:n1], p, p[:, 0:n1 - n0], 1e-6, None, ALU.add)
                c.op("act", lambda: nc.scalar.sqrt(rs[:, 0:n], rs[:, 0:n]), reads=[rs], writes=[rs])
                c.op("dve", lambda: nc.vector.reciprocal(rs[:, 0:n], rs[:, 0:n]), reads=[rs], writes=[rs])
                if wi == 0:
                    c.stt("dve", dst, dst[:, 0:n], dst, dst[:, 0:n], 128.0 ** -0.5, rs, rs[:, 0:n], ALU.mult, ALU.mult)
                else:
                    c.tt("dve", dst, dst[:, 0:n], dst, dst[:, 0:n], rs, rs[:, 0:n], ALU.mult)
        def X(t):
            return t[:, :].rearrange("p (c s) -> p c s", s=nseq)[:, c0:c0 + ncg, s]

        def Xb(t, w):
            return X(t).unsqueeze(2).broadcast_to([64, ncg, w])
        for cc0 in range(0, ncg, 4):
            pk = nextp()
            pv = nextp()
            for u in range(4):
                cc = cc0 + u
                c.tr(pk, pk[0:64, u * 128:(u + 1) * 128], kT, kT[:, cc * 64:(cc + 1) * 64], idf)
                c.tr(pv, pv[0:64, u * 128:(u + 1) * 128], vT, vT[:, cc * 64:(cc + 1) * 64], idf)
            pk3 = pk[0:64, :].rearrange("p (a b) -> p a b", b=128)
            pv3 = pv[0:64, :].rearrange("p (a b) -> p a b", b=128)
            sl = slice(cc0, cc0 + 4)
            c.tt("dve", R, R[:, sl, 0:128], pv, pv3, beta, Xb(beta, 128)[:, sl, :], ALU.mult)
            c.tt("dve", R, R[:, sl, 128:256], pk, pk3, bk, Xb(bk, 128)[:, sl, :], ALU.mult)
            c.tt("dve", ktail, ktail[:, sl, :], pk, pk3, kts, Xb(kts, 128)[:, sl, :], ALU.mult)
            yield
        c.dma("pool", grow[0:1, 0:n].rearrange("p (c j) -> p c j", j=64), gts_v[s:s + 1, c0:c0 + ncg, :],
              reads=[gts_db], writes=[grow])
        c.ts("dve", ngrow, ngrow[0:1, 0:n], grow, grow[0:1, 0:n], -1.0, None, ALU.mult)
        bm = lambda t: t[:, :].unsqueeze(1).broadcast_to([64, 8, 64])
        for cc0 in range(0, ncg, 8):
            nb = min(8, ncg - cc0)
            sl = slice(cc0, cc0 + nb)
            pd = nextp()
            pkk = nextp()
            pqk = nextp()
            for u in range(nb):
                cc = cc0 + u
                cs_ = slice(cc * 64, (cc + 1) * 64)
                us_ = slice(u * 64, (u + 1) * 64)
                c.mm(pd, pd[0:64, us_], grow, grow[0:1, cs_], ones, ones[0:1, 0:64], start=True, stop=False)
                c.mm(pd, pd[0:64, us_], ones, ones[0:1, 0:64], ngrow, ngrow[0:1, cs_], start=False, stop=True)
                c.mm(pkk, pkk[0:64, us_], kT, kT[:, cs_], kT, kT[:, cs_])
                c.mm(pqk, pqk[0:64, us_], qT, qT[:, cs_], kT, kT[:, cs_])
            v3 = lambda p: p[0:64, 0:nb * 64].rearrange("p (a b) -> p a b", b=64)
            mb = lambda t: t[:, :].unsqueeze(1).broadcast_to([64, nb, 64])
            c.tt("dve", dec, dec[:, sl, :], pd, v3(pd), mincl, mb(mincl), ALU.mult)
            c.tt("pool", dec, dec[:, sl, :], dec, dec[:, sl, :], negm, mb(negm), ALU.add)
            c.act(dec, dec[:, sl, :], dec, dec[:, sl, :], AF.Exp)
            c.tt("dve", L, L[:, sl, :], pkk, v3(pkk), dec, dec[:, sl, :], ALU.mult)
            c.tt("pool", L, L[:, sl, :], L, L[:, sl, :], mstrict, mb(mstrict), ALU.mult)
            c.tt("pool", L, L[:, sl, :], L, L[:, sl, :], beta, Xb(beta, 64)[:, sl, :], ALU.mult)
            c.tt("dve", tmp64, tmp64[:, sl, :], pqk, v3(pqk), dec, dec[:, sl, :], ALU.mult)
            pl_ = nextp()
            pi_ = nextp()
            for u in range(nb):
                cc = cc0 + u
                us_ = slice(u * 64, (u + 1) * 64)
                c.tr(pl_, pl_[0:64, us_], L, L[:, cc, :], idf_v)
                c.tr(pi_, pi_[0:64, us_], tmp64, tmp64[:, cc, :], idf_v)
            c.cp("act", LT, LT[:, sl, :], pl_, v3(pl_))
            c.cp("act", intraT, intraT[:, sl, :], pi_, v3(pi_))
            yield
        A, AT, B, BT = L, LT, L2, LT2
        for lev in range(6):
            for cc0 in range(0, ncg, 2):
                p = nextp()
                for u in range(2):
                    cc = cc0 + u
                    c.mm(p, p[0:64, u * 256:(u + 1) * 256], AT, AT[:, cc, :], R, R[:, cc, :])
                p3 = p[0:64, :].rearrange("p (a b) -> p a b", b=256)
                sl = slice(cc0, cc0 + 2)
                c.tt(("dve", "pool")[0], R, R[:, sl, :], R, R[:, sl, :], p, p3, ALU.subtract if lev == 0 else ALU.add)
            yield
            if lev < 5:
                for cc0 in range(0, ncg, 8):
                    nb = min(8, ncg - cc0)
                    sl = slice(cc0, cc0 + nb)
                    pa = nextp()
                    pt = nextp()
                    for u in range(nb):
                        cc = cc0 + u
                        us_ = slice(u * 64, (u + 1) * 64)
                        c.mm(pa, pa[0:64, us_], AT, AT[:, cc, :], A, A[:, cc, :])
                        c.mm(pt, pt[0:64, us_], A, A[:, cc, :], AT, AT[:, cc, :])
                    v3 = lambda p: p[0:64, 0:nb * 64].rearrange("p (a b) -> p a b", b=64)
                    c.cp("act", B, B[:, sl, :], pa, v3(pa))
                    c.cp("act", BT, BT[:, sl, :], pt, v3(pt))
                yield
                A, AT, B, BT = B, BT, A, AT
        for cc0 in range(0, ncg, 8):
            nb = min(8, ncg - cc0)
            p = nextp()
            for u in range(nb):
                cc = cc0 + u
                c.tr(p, p[:, u * 64:(u + 1) * 64], R, R[:, cc, 128:256], idf_v)
            c.cp("act", wT, wT[:, cc0:cc0 + nb, :], p, p[:, 0:nb * 64].rearrange("p (a b) -> p a b", b=64))
        ob_ = osb[SL['gi'] % 2]
        SL['gi'] += 1
        St = S[s]
        for cc in range(ncg):
            col = (c0 + cc) * nseq + s
            cs_ = slice(cc * 64, (cc + 1) * 64)
            pv_ = nextp()
            c.mm(pv_, pv_[0:64, 0:128], wT, wT[:, cc, :], St, St[:, :])
            vn = vnew[cc % 2]
            c.tt("dve", vn, vn[:], R, R[:, cc, 0:128], pv_, pv_[0:64, 0:128], ALU.subtract)
            po1 = nextp()
            c.mm(po1, po1[0:64, 0:128], qT, qT[:, cs_], St, St[:, :])
            po2 = nextp()
            c.mm(po2, po2[0:64, 0:128], intraT, intraT[:, cc, :], vn, vn[:])
            ps_ = nextp()
            c.mm(ps_, ps_[:, 0:128], ktail, ktail[:, cc, :], vn, vn[:])
            o2 = o2s[cc % 2]
            c.cp("act", o2, o2[:], po2, po2[0:64, 0:128])
            c.stt("dve", ob_, ob_[:, cc, :], po1, po1[0:64, 0:128], eG[:, col:col + 1], o2, o2[:], ALU.mult, ALU.add,
                  extra_reads=[eG])
            c.stt("dve", St, St[:, :], St, St[:, :], eGe[:, col:col + 1], ps_, ps_[:, 0:128], ALU.mult, ALU.add,
                  extra_reads=[eGe])
            yield
        c.dma("sp", o_out[s, c0 * 64:(c0 + ncg) * 64, :].rearrange("(c p) e -> p c e", p=64), ob_[:, 0:ncg, :], reads=[ob_])


    for (c0, ncg, col0) in groups:
        for s0 in range(0, nseq, NSLOT):
            gens = [seq_body(s0 + i, c0, ncg, col0, slots[i]) for i in range(NSLOT) if s0 + i < nseq]
            while gens:
                for g_ in list(gens):
                    try:
                        next(g_)
                    except StopIteration:
                        gens.remove(g_)
    c.finish()
    return nc


ODD_IN = 6208


def run_l1proj(inp, mod, x_lat, x_ctx):
    ident = np.eye(128, dtype=np.float32)
    maps = []
    for k in range(NCORES):
        b, r = k // 4, k % 4
        xs = np.zeros((L0_T, D), np.float32)
        xs[:4096] = x_lat[b, r * 4096:(r + 1) * 4096]
        xs[4096:4160] = x_ctx[b, r * 64:(r + 1) * 64]
        m = np.stack([mod[1, b].reshape(6, D)[0:2], mod[1, 2].reshape(6, D)[0:2]])
        maps.append({"x": xs, "g": inp["norm1_g"][1], "mod": np.ascontiguousarray(m), "w": inp["od_w_in"][0], "ident": ident})
    res = run(build_l0proj(N=ODD_IN, rope=False), maps)
    p_lat = np.zeros((2, 16384, ODD_IN), np.float32)
    p_ctx = np.zeros((2, 256, ODD_IN), np.float32)
    for k in range(NCORES):
        b, r = k // 4, k % 4
        p_lat[b, r * 4096:(r + 1) * 4096] = res[k]["p"][:4096]
        p_ctx[b, r * 64:(r + 1) * 64] = res[k]["p"][4096:4160]
    return p_lat, p_ctx


def run_post0(inp, mod, o_lat, o_ctx, f_lat, f_ctx):
    ident = np.eye(128, dtype=np.float32)
    maps = []
    for k in range(NCORES):
        b, r = k // 4, k % 4

        def sh(lat, ctx, w):
            a = np.zeros((L0_T, w), np.float32)
            a[:4096] = lat[b, r * 4096:(r + 1) * 4096].reshape(4096, w)
            a[4096:4160] = ctx[b, r * 64:(r + 1) * 64].reshape(64, w)
            return a
        m = np.stack([mod[0, b].reshape(6, D), mod[0, 2].reshape(6, D)])
        maps.append({"x": sh(inp["x"], inp["ctx"], D), "o": sh(o_lat, o_ctx, 768), "fo": sh(f_lat, f_ctx, 256),
                     "sg": inp["ev_subln_g"][0], "g2": inp["norm2_g"][0], "mod": np.ascontiguousarray(m),
                     "wout": inp["ev_w_out"][0], "wr": inp["moe_w_router"][0], "br": inp["moe_b_router"][0],
                     "w1": inp["moe_w1"][0], "b1": inp["moe_b1"][0], "w2": inp["moe_w2"][0], "b2": inp["moe_b2"][0],
                     "ident": ident})
    res = run(build_post(0, 33, 32, False, [11, 11, 11], lam_init=lam_init_of(0)), maps)
    x_lat = np.zeros((2, 16384, D), np.float32)
    x_ctx = np.zeros((2, 256, D), np.float32)
    for k in range(NCORES):
        b, r = k // 4, k % 4
        x_lat[b, r * 4096:(r + 1) * 4096] = res[k]["xo"][:4096]
        x_ctx[b, r * 64:(r + 1) * 64] = res[k]["xo"][4096:4160]
    return x_lat, x_ctx


def run_gdn(inp, p_lat, p_ctx):
    consts = gdn_consts()
    conv_w = inp["od_conv_w"][0]
    TP = 2 + 256 + 2 + 2 + 16384 + 2
    maps = []
    for k in range(NCORES):
        b, r = k // 4, k % 4

        def seq(cols, d):
            c_ = p_ctx[b][:, cols]
            l_ = p_lat[b][:, cols]
            if d == 1:
                c_ = c_[::-1]
                l_ = l_[::-1]
            out = np.zeros((cols.stop - cols.start, TP), np.float32)
            out[:, 2:258] = c_.T
            out[:, 262:262 + 16384] = l_.T
            return out

        def taps(cols, d):
            w = conv_w[:, cols].T
            return np.ascontiguousarray(w[:, ::-1] if d == 1 else w)
        qk_pre = np.zeros((4, 2, 128, TP), np.float32)
        cw_qk = np.zeros((4, 2, 128, 5), np.float32)
        for khl in range(2):
            kh = 2 * r + khl
            for d in range(2):
                for j, base in enumerate((0, 1024)):
                    cols = slice(base + kh * 128, base + (kh + 1) * 128)
                    qk_pre[khl * 2 + d, j] = seq(cols, d)
                    cw_qk[khl * 2 + d, j] = taps(cols, d)
        v_pre = np.zeros((8, 128, TP), np.float32)
        cw_v = np.zeros((8, 128, 5), np.float32)
        a_x = np.zeros((64, 260, 8), np.float32)
        b_x = np.zeros((64, 260, 8), np.float32)
        alog_x = np.zeros((64, 8), np.float32)
        dtb_x = np.zeros((64, 8), np.float32)
        for hvl in range(4):
            hv = 4 * r + hvl
            for d in range(2):
                s = hvl * 2 + d
                cols = slice(2048 + hv * 128, 2048 + (hv + 1) * 128)
                v_pre[s] = seq(cols, d)
                cw_v[s] = taps(cols, d)
                for arr, ab in ((a_x, 0), (b_x, 1)):
                    col = 6144 + ab * 32 + d * 16 + hv
                    c_ = p_ctx[b][:, col]
                    l_ = p_lat[b][:, col]
                    if d == 1:
                        c_ = c_[::-1]
                        l_ = l_[::-1]
                    arr[:, :, s] = np.concatenate([c_, l_]).reshape(260, 64).T
                alog_x[:, s] = inp["od_a_log"][0, d, hv]
                dtb_x[:, s] = inp["od_dt_bias"][0, d, hv]
        m = {"qk_pre": qk_pre, "v_pre": v_pre, "cw_qk": cw_qk, "cw_v": cw_v, "a_x": a_x, "b_x": b_x,
             "alog_x": alog_x, "dtb_x": dtb_x}
        m.update(consts)
        maps.append(m)
    res = run(build_gdn(), maps)
    o_f = np.zeros((2, 16384, 16, 128), np.float32)
    o_b = np.zeros((2, 16384, 16, 128), np.float32)
    for k in range(NCORES):
        b, r = k // 4, k % 4
        o = res[k]["o"]
        for hvl in range(4):
            hv = 4 * r + hvl
            o_f[b, :, hv] = o[hvl * 2, 256:]
            o_b[b, :, hv] = o[hvl * 2 + 1, 256:][::-1]
    return o_f.reshape(2, 16384, 2048), o_b.reshape(2, 16384, 2048)


def run_post1(inp, mod, x_lat, o_f, o_b, p_lat):
    ident = np.eye(128, dtype=np.float32)
    maps = []
    for k in range(NCORES):
        b, r = k // 4, k % 4
        rs = slice(r * 4096, (r + 1) * 4096)
        m = np.stack([mod[1, b].reshape(6, D), mod[1, 2].reshape(6, D)])
        maps.append({"x": np.ascontiguousarray(x_lat[b, rs]), "of": np.ascontiguousarray(o_f[b, rs]),
                     "ob": np.ascontiguousarray(o_b[b, rs]), "z": np.ascontiguousarray(p_lat[b, rs, 4096:6144]),
                     "sg": inp["od_norm_g"][0], "g2": inp["norm2_g"][1], "mod": np.ascontiguousarray(m),
                     "wout": inp["od_w_out"][0], "wr": inp["moe_w_router"][1], "br": inp["moe_b_router"][1],
                     "w1": inp["moe_w1"][1], "b1": inp["moe_b1"][1], "w2": inp["moe_w2"][1], "b2": inp["moe_b2"][1],
                     "ident": ident, "fg": inp["final_g"]})
    res = run(build_post(1, 32, 32, True, [11, 11, 10]), maps)
    out = np.zeros((2, 16384, D), np.float32)
    for k in range(NCORES):
        b, r = k // 4, k % 4
        out[b, r * 4096:(r + 1) * 4096] = res[k]["xo"]
    return out


def kernel(**inputs):
    inp = {k: np.ascontiguousarray(np.asarray(v, dtype=np.float32)) for k, v in inputs.items()}
    mod = run_mod(inp)
    p_lat, p_ctx = run_l0proj(inp, mod)
    o_lat, o_ctx = run_attn(inp, p_lat, p_ctx)
    f_lat, f_ctx = run_fourier(p_lat, p_ctx)
    del p_lat, p_ctx
    x_lat, x_ctx = run_post0(inp, mod, o_lat, o_ctx, f_lat, f_ctx)
    del o_lat, o_ctx, f_lat, f_ctx
    p1_lat, p1_ctx = run_l1proj(inp, mod, x_lat, x_ctx)
    o_f, o_b = run_gdn(inp, p1_lat, p1_ctx)
    out = run_post1(inp, mod, x_lat, o_f, o_b, p1_lat)
    return out
```
